# Optimizing a Trainium2 kernel written in Bass

```python
import math
import jax, jax.numpy as jnp
from jax import lax
import numpy as np

D_MODEL = 1024
BATCH = 2
SEQ = 8192
DEPTH = 2

CHUNK = 64
Q_BLOCK = 128
ROPE_THETA = 10000.0
EPS = 1e-6

A_HEADS = 4
A_HEAD_DIM = 64
A_WIDTH = A_HEADS * 2 * A_HEAD_DIM
B_HEADS = 8
B_HEAD_DIM = 64
B_WIDTH = B_HEADS * B_HEAD_DIM
IDX_HEADS = 4
IDX_DIM = 64
TOPK_MAX = 256
C_HEADS = 8
C_HEAD_DIM = 64
C_WIDTH = C_HEADS * C_HEAD_DIM
C_LEFT_CHUNKS = 8
REL_CLIP = 256
N_EXPERTS = 16
N_GROUPS = 4
EXPERTS_PER_GROUP = N_EXPERTS // N_GROUPS
TOP_K = 2
D_FF_EXPERT = 512
N_BRANCHES = 3

IN_SIZES = (
    A_WIDTH, A_WIDTH, A_WIDTH,
    B_WIDTH, B_HEAD_DIM, B_HEAD_DIM,
    IDX_HEADS * IDX_DIM, IDX_DIM, IDX_HEADS,
    C_WIDTH, C_WIDTH, C_WIDTH,
    N_BRANCHES * D_MODEL,
)
IN_COLS = sum(IN_SIZES)

kernel_name = "hybrid_gated_diff_dsa_chunkband_grouped_moe"


def rms_norm(x, g):
    xf = x.astype(jnp.float32)
    y = xf * lax.rsqrt(jnp.mean(xf * xf, axis=-1, keepdims=True) + EPS)
    return (y * g.astype(jnp.float32)).astype(x.dtype)


def rope(x, positions):
    half = x.shape[-1] // 2
    inv = ROPE_THETA ** (-jnp.arange(half, dtype=jnp.float32) / half)
    ang = positions.astype(jnp.float32)[..., None] * inv
    cos = jnp.cos(ang)[:, :, None, :].astype(x.dtype)
    sin = jnp.sin(ang)[:, :, None, :].astype(x.dtype)
    x1, x2 = x[..., :half], x[..., half:]
    return jnp.concatenate([x1 * cos - x2 * sin, x2 * cos + x1 * sin], axis=-1)


def split_cols(p, sizes):
    out, start = [], 0
    for s in sizes:
        out.append(p[..., start:start + s])
        start += s
    return out


def chunk_causal_mask(q_start, n_q, n_k):
    qc = (q_start + jnp.arange(n_q)) // CHUNK
    kc = jnp.arange(n_k) // CHUNK
    return kc[None, :] <= qc[:, None]


def diff_attention(q, k, v, lam, lam_init, norm_g):
    B, S, H, _, d = q.shape
    scale = d ** -0.5

    def block(i):
        qs = i * Q_BLOCK
        qb = lax.dynamic_slice_in_dim(q, qs, Q_BLOCK, axis=1)
        s = jnp.einsum('bqhmd,bkhmd->bhmqk', qb, k).astype(jnp.float32) * scale
        s = jnp.where(chunk_causal_mask(qs, Q_BLOCK, S), s, -jnp.inf)
        p = jax.nn.softmax(s, axis=-1)
        p = p[:, :, 0] - lam * p[:, :, 1]
        return jnp.einsum('bhqk,bkhe->bqhe', p.astype(v.dtype), v)

    o = lax.map(block, jnp.arange(S // Q_BLOCK))
    o = jnp.moveaxis(o, 0, 1).reshape(B, S, H, 2 * d)
    o = rms_norm(o, norm_g) * (1.0 - lam_init)
    return o.reshape(B, S, H * 2 * d)


def dsa_attention(q, k, v, iq, ik, iw):
    B, S, H, d = q.shape
    topk = min(TOPK_MAX, S // 4)
    scale = d ** -0.5
    b_idx = jnp.arange(B)[:, None, None]

    def block(i):
        qs = i * Q_BLOCK
        qb = lax.dynamic_slice_in_dim(q, qs, Q_BLOCK, axis=1)
        iqb = lax.dynamic_slice_in_dim(iq, qs, Q_BLOCK, axis=1)
        iwb = lax.dynamic_slice_in_dim(iw, qs, Q_BLOCK, axis=1)
        hs = jax.nn.relu(jnp.einsum('bqhe,bke->bqhk', iqb, ik))
        score = jnp.einsum('bqh,bqhk->bqk', iwb, hs).astype(jnp.float32)
        score = jnp.where(chunk_causal_mask(qs, Q_BLOCK, S)[None], score, -jnp.inf)
        _, sel = lax.top_k(score, topk)
        q_chunk = (qs + jnp.arange(Q_BLOCK)) // CHUNK
        valid = (sel // CHUNK) <= q_chunk[None, :, None]
        kg = k[b_idx, sel]
        vg = v[b_idx, sel]
        s = jnp.einsum('bqhd,bqkd->bhqk', qb, kg).astype(jnp.float32) * scale
        s = jnp.where(valid[:, None], s, -jnp.inf)
        p = jax.nn.softmax(s, axis=-1)
        return jnp.einsum('bhqk,bqkd->bqhd', p.astype(vg.dtype), vg)

    o = lax.map(block, jnp.arange(S // Q_BLOCK))
    return jnp.moveaxis(o, 0, 1).reshape(B, S, H * d)


def chunk_band_attention(q, k, v, rel_bias):
    B, S, H, d = q.shape
    nc = S // CHUNK
    band = C_LEFT_CHUNKS + 1
    qc = q.reshape(B, nc, CHUNK, H, d)
    pad = ((0, 0), (C_LEFT_CHUNKS * CHUNK, 0), (0, 0), (0, 0))
    kp = jnp.pad(k, pad).reshape(B, nc + C_LEFT_CHUNKS, CHUNK, H, d)
    vp = jnp.pad(v, pad).reshape(B, nc + C_LEFT_CHUNKS, CHUNK, H, d)
    band_idx = jnp.arange(nc)[:, None] + jnp.arange(band)[None, :]
    kb = kp[:, band_idx].reshape(B, nc, band * CHUNK, H, d)
    vb = vp[:, band_idx].reshape(B, nc, band * CHUNK, H, d)
    qi = jnp.arange(CHUNK)[:, None]
    kj = jnp.arange(band * CHUNK)[None, :]
    rel = C_LEFT_CHUNKS * CHUNK + qi - kj
    bias = rel_bias.astype(jnp.float32)[:, jnp.clip(rel, -REL_CLIP, REL_CLIP) + REL_CLIP]
    key_ok = (jnp.arange(nc)[:, None] - C_LEFT_CHUNKS + jnp.arange(band)[None, :]) >= 0
    key_ok = jnp.repeat(key_ok, CHUNK, axis=1)
    s = jnp.einsum('bnqhd,bnkhd->bnhqk', qc, kb).astype(jnp.float32) * (d ** -0.5)
    s = s + bias[None, None]
    s = jnp.where(key_ok[None, :, None, None, :], s, -jnp.inf)
    p = jax.nn.softmax(s, axis=-1)
    o = jnp.einsum('bnhqk,bnkhd->bnqhd', p.astype(vb.dtype), vb)
    return o.reshape(B, S, H * d)


def token_mixers(u, positions, w_in, lq1, lk1, lq2, lk2, a_norm_g, rel_bias,
                 wa, wb, wc, w_out, lam_init):
    B, S, _ = u.shape
    (aq, ak, av, bq, bk, bv, iq, ik, iw, cq, ck, cv, gates) = split_cols(
        jnp.einsum('bsd,dn->bsn', u, w_in), IN_SIZES)
    aq = rope(aq.reshape(B, S, A_HEADS * 2, A_HEAD_DIM), positions).reshape(B, S, A_HEADS, 2, A_HEAD_DIM)
    ak = rope(ak.reshape(B, S, A_HEADS * 2, A_HEAD_DIM), positions).reshape(B, S, A_HEADS, 2, A_HEAD_DIM)
    av = av.reshape(B, S, A_HEADS, 2 * A_HEAD_DIM)
    f32 = jnp.float32
    lam = (jnp.exp(jnp.sum(lq1.astype(f32) * lk1.astype(f32)))
           - jnp.exp(jnp.sum(lq2.astype(f32) * lk2.astype(f32))) + lam_init)
    ya = diff_attention(aq, ak, av, lam, lam_init, a_norm_g)
    bq = rope(bq.reshape(B, S, B_HEADS, B_HEAD_DIM), positions)
    bk = rope(bk[:, :, None, :], positions)[:, :, 0]
    iq = rope(iq.reshape(B, S, IDX_HEADS, IDX_DIM), positions)
    ik = rope(ik[:, :, None, :], positions)[:, :, 0]
    iw = iw * (IDX_HEADS ** -0.5 * IDX_DIM ** -0.5)
    yb = dsa_attention(bq, bk, bv, iq, ik, iw)
    yc = chunk_band_attention(cq.reshape(B, S, C_HEADS, C_HEAD_DIM),
                              ck.reshape(B, S, C_HEADS, C_HEAD_DIM),
                              cv.reshape(B, S, C_HEADS, C_HEAD_DIM), rel_bias)
    ga, gb, gc = jnp.split(jax.nn.sigmoid(gates), N_BRANCHES, axis=-1)
    merged = ga * (ya @ wa) + gb * (yb @ wb) + gc * (yc @ wc)
    return merged @ w_out


def grouped_moe(u, router_w, router_b, w1, w3, w2):
    B, S, D = u.shape
    t = u.reshape(B * S, D)
    aff = jax.nn.sigmoid(jnp.dot(t, router_w).astype(jnp.float32))
    sel = aff + router_b.astype(jnp.float32)
    grp_top = lax.top_k(sel.reshape(-1, N_GROUPS, EXPERTS_PER_GROUP), 2)[0]
    best_g = jnp.argmax(grp_top.sum(-1), axis=-1)
    in_group = (jnp.arange(N_EXPERTS) // EXPERTS_PER_GROUP)[None, :] == best_g[:, None]
    _, top_idx = lax.top_k(jnp.where(in_group, sel, -jnp.inf), TOP_K)
    top_w = jnp.take_along_axis(aff, top_idx, axis=-1)
    top_w = top_w / jnp.sum(top_w, axis=-1, keepdims=True)
    combine = jnp.einsum('nk,nke->ne', top_w, jax.nn.one_hot(top_idx, N_EXPERTS, dtype=jnp.float32))
    combine = combine.astype(t.dtype)
    y = jnp.zeros_like(t)
    for e in range(N_EXPERTS):
        h = jax.nn.silu(t @ w1[e]) * (t @ w3[e])
        y = y + combine[:, e:e + 1] * (h @ w2[e])
    return y.reshape(B, S, D)


def setup_inputs(seed: int = 0) -> dict:
    key = jax.random.key(seed)
    ks = jax.random.split(key, 32)
    f32 = jnp.float32

    def nrm(k, shape, fan_in, gain=1.0):
        return (gain * fan_in ** -0.5) * jax.random.normal(k, shape, f32)

    x = jax.random.normal(ks[0], (BATCH, SEQ, D_MODEL), f32)
    c = jax.random.normal(ks[1], (BATCH, D_MODEL), f32)
    positions = (jnp.arange(SEQ, dtype=jnp.int32)[None, :]
                 + jax.random.randint(ks[2], (BATCH, 1), 0, 1024, dtype=jnp.int32))
    return {
        "x": x,
        "c": c,
        "positions": positions,
        "norm1_g": 1.0 + 0.02 * jax.random.normal(ks[3], (DEPTH, D_MODEL), f32),
        "norm2_g": 1.0 + 0.02 * jax.random.normal(ks[4], (DEPTH, D_MODEL), f32),
        "w_mod": nrm(ks[5], (DEPTH, D_MODEL, 6 * D_MODEL), D_MODEL, 0.5),
        "b_mod": 0.02 * jax.random.normal(ks[6], (DEPTH, 6 * D_MODEL), f32),
        "w_in": nrm(ks[7], (DEPTH, D_MODEL, IN_COLS), D_MODEL),
        "lambda_q1": 0.1 * jax.random.normal(ks[8], (DEPTH, A_HEAD_DIM), f32),
        "lambda_k1": 0.1 * jax.random.normal(ks[9], (DEPTH, A_HEAD_DIM), f32),
        "lambda_q2": 0.1 * jax.random.normal(ks[10], (DEPTH, A_HEAD_DIM), f32),
        "lambda_k2": 0.1 * jax.random.normal(ks[11], (DEPTH, A_HEAD_DIM), f32),
        "a_norm_g": 1.0 + 0.02 * jax.random.normal(ks[12], (DEPTH, 2 * A_HEAD_DIM), f32),
        "c_rel_bias": 0.2 * jax.random.normal(ks[13], (DEPTH, C_HEADS, 2 * REL_CLIP + 1), f32),
        "w_branch_a": nrm(ks[14], (DEPTH, A_WIDTH, D_MODEL), A_WIDTH),
        "w_branch_b": nrm(ks[15], (DEPTH, B_WIDTH, D_MODEL), B_WIDTH),
        "w_branch_c": nrm(ks[16], (DEPTH, C_WIDTH, D_MODEL), C_WIDTH),
        "w_out": nrm(ks[17], (DEPTH, D_MODEL, D_MODEL), D_MODEL),
        "router_w": nrm(ks[18], (D_MODEL, N_EXPERTS), D_MODEL),
        "router_b": 0.01 * jax.random.normal(ks[19], (N_EXPERTS,), f32),
        "exp_w1": nrm(ks[20], (DEPTH, N_EXPERTS, D_MODEL, D_FF_EXPERT), D_MODEL),
        "exp_w3": nrm(ks[21], (DEPTH, N_EXPERTS, D_MODEL, D_FF_EXPERT), D_MODEL),
        "exp_w2": nrm(ks[22], (DEPTH, N_EXPERTS, D_FF_EXPERT, D_MODEL), D_FF_EXPERT),
        "final_g": 1.0 + 0.02 * jax.random.normal(ks[23], (D_MODEL,), f32),
    }


def reference(x, c, positions, norm1_g, norm2_g, w_mod, b_mod, w_in,
              lambda_q1, lambda_k1, lambda_q2, lambda_k2, a_norm_g, c_rel_bias,
              w_branch_a, w_branch_b, w_branch_c, w_out, router_w, router_b,
              exp_w1, exp_w3, exp_w2, final_g):
    c_act = jax.nn.silu(c)
    for layer in range(DEPTH):
        lam_init = 0.8 - 0.6 * math.exp(-0.3 * layer)
        mod = jnp.dot(c_act, w_mod[layer]) + b_mod[layer]
        sh1, sc1, g1, sh2, sc2, g2 = jnp.split(mod[:, None, :], 6, axis=-1)
        u = rms_norm(x, norm1_g[layer]) * (1.0 + sc1) + sh1
        x = x + g1 * token_mixers(u, positions, w_in[layer],
                                  lambda_q1[layer], lambda_k1[layer],
                                  lambda_q2[layer], lambda_k2[layer],
                                  a_norm_g[layer], c_rel_bias[layer],
                                  w_branch_a[layer], w_branch_b[layer], w_branch_c[layer],
                                  w_out[layer], lam_init)
        u = rms_norm(x, norm2_g[layer]) * (1.0 + sc2) + sh2
        x = x + g2 * grouped_moe(u, router_w, router_b,
                                 exp_w1[layer], exp_w3[layer], exp_w2[layer])
    return rms_norm(x, final_g)
```

```python
import math
from contextlib import ExitStack
import numpy as np
import ml_dtypes
import concourse.bass as bass
import concourse.mybir as mybir
from concourse.bass_utils import run_bass_kernel_spmd

F32 = mybir.dt.float32
BF16 = mybir.dt.bfloat16
I32 = mybir.dt.int32
ALU = mybir.AluOpType
AF = mybir.ActivationFunctionType
AX = mybir.AxisListType

ENGS = ("pe", "act", "dve", "pool", "sp")


class Op:
    __slots__ = ("eng", "fn", "deps", "is_dma", "sem", "ticket", "needs_inc", "idx")

    def __init__(self, eng, fn, is_dma, sem):
        self.eng = eng
        self.fn = fn
        self.deps = []
        self.is_dma = is_dma
        self.sem = sem
        self.ticket = None
        self.needs_inc = False


class Rec:
    def __init__(self, nc):
        self.nc = nc
        self.streams = {e: [] for e in ENGS}
        self.last_w = {}
        self.readers = {}
        self.dma_count = {}
        self.all_ops = []

    def _add(self, op, r, w):
        deps = []
        for k in r:
            lw = self.last_w.get(k)
            if lw is not None:
                deps.append(lw)
        for k in w:
            lw = self.last_w.get(k)
            if lw is not None:
                deps.append(lw)
            deps.extend(self.readers.get(k, ()))
        seen = set()
        for d in deps:
            if d is op or id(d) in seen:
                continue
            seen.add(id(d))
            if d.eng == "pe" and op.eng == "pe" and not d.is_dma and not op.is_dma:
                continue
            op.deps.append(d)
            d.needs_inc = True
        for k in w:
            self.last_w[k] = op
            self.readers[k] = []
        for k in r:
            self.readers.setdefault(k, []).append(op)
        self.streams[op.eng].append(op)
        self.all_ops.append(op)
        return op

    def op(self, eng, fn, r=(), w=()):
        return self._add(Op(eng, fn, False, None), r, w)

    def dma(self, eng, out, in_, sem, r=(), w=()):
        o = Op(eng, lambda e: e.dma_start(out=out, in_=in_), True, sem)
        self.dma_count[sem] = self.dma_count.get(sem, 0) + 1
        o.ticket = self.dma_count[sem]
        return self._add(o, r, w)

    def emit(self):
        nc = self.nc
        cnt = {e: 0 for e in ENGS}
        for e in ENGS:
            for o in self.streams[e]:
                if o.is_dma:
                    continue
                if o.needs_inc:
                    cnt[e] += 1
                    o.ticket = cnt[e]
        order = {id(o): i for i, o in enumerate(self.all_ops)}
        dma_hist = {}
        for i, o in enumerate(self.all_ops):
            if o.is_dma:
                dma_hist.setdefault(o.sem, []).append((i, o.ticket))
        import bisect
        dma_keys = sorted(self.dma_count.keys(), key=str)
        from contextlib import ExitStack
        esem = {e: nc.alloc_semaphore(name=nc.make_name("s_" + e, add_next_id=True)) for e in ENGS}
        dsem = {k: nc.alloc_semaphore(name=nc.make_name("d_%d" % i, add_next_id=True)) for i, k in enumerate(dma_keys)}
        with ExitStack() as st:
            block = st.enter_context(nc.Block())

            def run_stream(ename):
                def body(e):
                    waited = {}
                    for o in self.streams[ename]:
                        me = order[id(o)]
                        for d in o.deps:
                            if d.is_dma:
                                hist = dma_hist[d.sem]
                                j = bisect.bisect_left(hist, (me, 0)) - 1
                                val = 16 * hist[j][1]
                                sem = dsem[d.sem]
                                key = ("d", d.sem)
                            else:
                                val = d.ticket
                                sem = esem[d.eng]
                                key = ("e", d.eng)
                            if waited.get(key, 0) >= val:
                                continue
                            waited[key] = val
                            e.wait_ge(sem, val)
                        ins = o.fn(e)
                        if o.is_dma:
                            ins.then_inc(dsem[o.sem], 16)
                        elif o.needs_inc:
                            ins.then_inc(esem[ename], 1)
                    if ename == "sp":
                        for k in getattr(self, "final_wait", ()):
                            e.wait_ge(dsem[k], 16 * self.dma_count[k])
                return body

            block.tensor(run_stream("pe"))
            block.scalar(run_stream("act"))
            block.vector(run_stream("dve"))
            block.gpsimd(run_stream("pool"))
            block.sync(run_stream("sp"))
        nc.all_engine_barrier()
        nc.clear_and_free_semaphores(list(esem.values()) + list(dsem.values()))
        nc.all_engine_barrier()


def dram_ap(t, offset, pattern):
    return bass.AP(t, offset, pattern)


D = 1024
NT = 16
TOK = NT * 128
INC = 7108
C_AQ, C_AK, C_AV, C_BQ, C_BK, C_BV, C_IQ, C_IK, C_IW, C_CQ, C_CK, C_CV, C_G = (
    0, 512, 1024, 1536, 2048, 2112, 2176, 2432, 2496, 2500, 3012, 3524, 4036)
EPS = 1e-6
TWO_PI = 2.0 * math.pi


def emit_P(nc, T, pfx, nt=NT):
    tok = nt * 128
    di = lambda n, s, d=F32: T[n]
    do = lambda n, s, d=BF16: T[n]
    x = di("x", [tok, D]); pos = di("pos", [128, nt], I32); cvec = di("c", [128, 8])
    w_mod = di("w_mod", [D, 6144]); b_mod = di("b_mod", [1, 6144]); norm_g = di("norm_g", [1, D])
    w_in = di("w_in", [D, INC]); ident = di("ident", [128, 128]); ropeinv = di("ropeinv", [1, 32])
    aqt = do("aqt", [4, 128, tok]); akt = do("akt", [4, 128, tok]); av = do("av", [tok, 516])
    bqt = do("bqt", [nt, 64, 1024]); bkt = do("bkt", [64, tok]); bv = do("bv", [tok, 65])
    iw = do("iw", [tok, 4], F32)
    cqt = do("cqt", [4, 128, tok]); ckt = do("ckt", [4, 128, tok]); cv = do("cv", [tok, 520])
    gates = do("gates", [tok, 3072], F32)

    R = Rec(nc)
    with ExitStack() as st, nc.allow_low_precision("bf16 matmul operands, fp32 accumulation"):
        sb = lambda n, s, d=F32: st.enter_context(nc.sbuf_tensor(pfx + n, s, d))
        ps = lambda n, s, d=F32: st.enter_context(nc.psum_tensor(pfx + n, s, d))
        cs = sb("cs", [128, 8]); ca = sb("ca", [128, 8]); CA = sb("CA", [128, 8, 128])
        modbc = sb("modbc", [128, 2048]); gs = sb("gs", [128, D])
        wsb = sb("wsb", [128, 8, INC], BF16)
        idf = sb("idf", [128, 128]); idb = sb("idb", [128, 128], BF16)
        inv = sb("inv", [128, 32]); posi = sb("posi", [128, nt], I32); posf = sb("posf", [128, nt])
        xt = [sb("xt%d" % i, [128, D]) for i in range(2)]
        junk = sb("junk", [128, D], BF16); ss = sb("ss", [128, 1]); rstd = sb("rstd", [128, 1]); rt = sb("rt", [128, 1])
        tmp = sb("tmp", [128, D]); ub = sb("ub", [128, D], BF16); uT = sb("uT", [128, D], BF16)
        pj = sb("pj", [128, INC])
        ang = sb("ang", [128, nt, 32]); ang2 = sb("ang2", [128, nt, 32]); kf = sb("kf", [128, nt, 32]); ki = sb("ki", [128, nt, 32], I32)
        SN = sb("SN", [128, nt, 32]); CN = sb("CN", [128, nt, 32])
        t1 = sb("t1", [128, 512]); t2 = sb("t2", [128, 512])
        rb = sb("rb", [128, C_CV], BF16)
        avb = sb("avb", [128, 4, 129], BF16); bvb = sb("bvb", [128, 65], BF16); cvb = sb("cvb", [128, 8, 65], BF16)
        iwb = sb("iwb", [128, 4]); cst = sb("cst", [128, 2])
        wm = [pj[:, b * 2048:(b + 1) * 2048].rearrange("p (ch n) -> p ch n", n=256) for b in range(2)]
        WMK = [["pj%d" % i for i in range(4)], ["pj%d" % i for i in range(4, 8)]]
        bmb = pj[:, 4096:6144]; BMK = ["pj%d" % i for i in range(8, 12)]
        gbc = tmp
        tA = [sb("tA%d" % i, [128, 1024], BF16) for i in range(2)]
        pT = ps("pT", [128, 1024], BF16)
        pp = [ps("pp%d" % i, [128, 512]) for i in range(2)]
        pX = [ps("pX%d" % i, [128, 1024], BF16) for i in range(2)]

        R.dma("sp", cs[:], cvec.ap(), "c", w=["cs"])
        R.op("act", lambda e: e.activation(out=ca[:], in_=cs[:], func=AF.Silu), r=["cs"], w=["ca"])
        R.op("dve", lambda e: e.tensor_copy(out=CA[:], in_=ca[:].unsqueeze(2).broadcast_to([128, 8, 128])), r=["ca"], w=["CA"])
        R.dma("sp", bmb, b_mod.ap()[0:1, 0:2048].partition_broadcast(128), "c", w=BMK)
        R.dma("sp", gbc[:], norm_g.ap()[0:1, :].partition_broadcast(128), "c", w=["tmp"])
        R.dma("sp", idf[:], ident.ap(), "c", w=["idf"])
        R.dma("sp", inv[:], ropeinv.ap()[0:1, :].partition_broadcast(128), "c", w=["inv"])
        R.dma("sp", posi[:], pos.ap(), "c", w=["posi"])
        R.op("dve", lambda e: e.tensor_copy(out=idb[:], in_=idf[:]), r=["idf"], w=["idb"])
        R.op("dve", lambda e: e.tensor_copy(out=posf[:], in_=posi[:]), r=["posi"], w=["posf"])
        R.op("pool", lambda e: e.memset(cst[:, 0:1], EPS), w=["cst"])
        R.op("pool", lambda e: e.memset(cst[:, 1:2], math.pi), w=["cst"])
        R.op("pool", lambda e: e.memset(avb[:], 1.0), w=["avb"])
        R.op("pool", lambda e: e.memset(bvb[:], 1.0), w=["bvb"])
        R.op("pool", lambda e: e.memset(cvb[:], 1.0), w=["cvb"])
        R.op("dve", lambda e: e.tensor_tensor(out=ang[:], in0=inv[:].unsqueeze(1).broadcast_to([128, nt, 32]),
                                              in1=posf[:].unsqueeze(2).broadcast_to([128, nt, 32]), op=ALU.mult), r=["inv", "posf"], w=["ang"])
        R.op("dve", lambda e: e.tensor_scalar(out=ang2[:], in0=ang[:], scalar1=math.pi / 2, scalar2=None, op0=ALU.add), r=["ang"], w=["ang2"])
        for (src, dst, nm) in ((ang, SN, "SN"), (ang2, CN, "CN")):
            sk = "ang" if src is ang else "ang2"
            R.op("dve", lambda e, src=src: e.tensor_scalar(out=ki[:], in0=src[:], scalar1=1.0 / TWO_PI, scalar2=None, op0=ALU.mult), r=[sk], w=["ki"])
            R.op("dve", lambda e: e.tensor_copy(out=kf[:], in_=ki[:]), r=["ki"], w=["kf"])
            R.op("dve", lambda e, src=src: e.scalar_tensor_tensor(out=kf[:], in0=kf[:], scalar=-TWO_PI, in1=src[:], op0=ALU.mult, op1=ALU.add),
                 r=["kf", sk], w=["kf"])
            R.op("dve", lambda e: e.tensor_scalar(out=kf[:], in0=kf[:], scalar1=3.14159, scalar2=-3.14159, op0=ALU.min, op1=ALU.max), r=["kf"], w=["kf"])
            R.op("act", lambda e, dst=dst: e.activation(out=dst[:], in_=kf[:], func=AF.Sin), r=["kf"], w=[nm])
        wmv = w_mod.ap().rearrange("(ch p) n -> p ch n", p=128)
        for j in range(8):
            b = j % 2
            R.dma("sp", wm[b], wmv[:, :, j * 256:(j + 1) * 256], "wm%d" % b, w=WMK[b])
            for ch in range(8):
                R.op("pe", lambda e, ch=ch, b=b: e.matmul(pp[b][:, 0:256], lhsT=CA[:, ch, :], rhs=wm[b][:, ch, :],
                                                          start=(ch == 0), stop=(ch == 7)),
                     r=["CA"] + WMK[b], w=["pp%d" % b])
            R.op("dve", lambda e, j=j, b=b: e.tensor_tensor(out=modbc[:, j * 256:(j + 1) * 256], in0=pp[b][:, 0:256],
                                                            in1=bmb[:, j * 256:(j + 1) * 256], op=ALU.add),
                 r=["pp%d" % b] + BMK, w=["modbc"])
        R.op("dve", lambda e: e.scalar_tensor_tensor(out=gs[:], in0=modbc[:, 1024:2048], scalar=1.0, in1=gbc[:],
                                                     op0=ALU.add, op1=ALU.mult), r=["modbc", "tmp"], w=["gs"])
        wiv = w_in.ap().rearrange("(ch p) n -> p ch n", p=128)
        for ch in range(8):
            R.dma("pool", wsb[:, ch, :], wiv[:, ch, :], "wsb", w=["wsb%d" % ch])
        WS = ["wsb%d" % ch for ch in range(8)]

        for t in range(nt):
            xb = t % 2
            xk = "xt%d" % xb
            R.dma("sp", xt[xb][:], x.ap()[t * 128:(t + 1) * 128, :], xk, w=[xk])
            R.op("act", lambda e, xb=xb: e.activation(out=junk[:], in_=xt[xb][:], func=AF.Square, accum_out=ss[:]),
                 r=[xk], w=["junk", "ss"])
            R.op("act", lambda e: e.activation(out=rt[:], in_=ss[:], func=AF.Sqrt, scale=1.0 / D, bias=cst[:, 0:1]),
                 r=["ss", "cst"], w=["rt"])
            R.op("dve", lambda e: e.reciprocal(out=rstd[:], in_=rt[:]), r=["rt"], w=["rstd"])
            R.op("dve", lambda e, xb=xb: e.scalar_tensor_tensor(out=tmp[:], in0=xt[xb][:], scalar=rstd[:, 0:1], in1=gs[:],
                                                                op0=ALU.mult, op1=ALU.mult), r=[xk, "rstd", "gs"], w=["tmp"])
            R.op("dve", lambda e: e.tensor_tensor(out=ub[:], in0=tmp[:], in1=modbc[:, 0:1024], op=ALU.add),
                 r=["tmp", "modbc"], w=["ub"])
            for ch in range(8):
                R.op("pe", lambda e, ch=ch: e.transpose(out=pT[:, ch * 128:(ch + 1) * 128], in_=ub[:, ch * 128:(ch + 1) * 128],
                                                        identity=idb[:]), r=["ub", "idb"], w=["pT"])
            R.op("act", lambda e: e.copy(out=uT[:], in_=pT[:]), r=["pT"], w=["uT"])
            nchunks = (INC + 511) // 512
            for n in range(nchunks):
                n0, n1 = n * 512, min(INC, (n + 1) * 512)
                b = n % 2
                for ch in range(8):
                    R.op("pe", lambda e, ch=ch, b=b, n0=n0, n1=n1: e.matmul(pp[b][:, 0:n1 - n0], lhsT=uT[:, ch * 128:(ch + 1) * 128],
                                                                              rhs=wsb[:, ch, n0:n1], start=(ch == 0), stop=(ch == 7)),
                         r=["uT", WS[ch]], w=["pp%d" % b])
                eng = "act" if n % 2 == 0 else "dve"
                if eng == "act":
                    R.op("act", lambda e, b=b, n0=n0, n1=n1: e.copy(out=pj[:, n0:n1], in_=pp[b][:, 0:n1 - n0]),
                         r=["pp%d" % b], w=["pj%d" % n])
                else:
                    R.op("dve", lambda e, b=b, n0=n0, n1=n1: e.tensor_copy(out=pj[:, n0:n1], in_=pp[b][:, 0:n1 - n0]),
                         r=["pp%d" % b], w=["pj%d" % n])
            PJ = lambda c0, c1: ["pj%d" % n for n in range(c0 // 512, (c1 - 1) // 512 + 1)]
            for (c0, H) in ((C_AQ, 16), (C_BQ, 9), (C_IQ, 5)):
                c1 = c0 + 64 * H
                xv = pj[:, c0:c1].rearrange("p (h two d) -> p h two d", two=2, d=32)
                ov = rb[:, c0:c1].rearrange("p (h two d) -> p h two d", two=2, d=32)
                x1, x2 = xv[:, :, 0, :], xv[:, :, 1, :]
                o1, o2 = ov[:, :, 0, :], ov[:, :, 1, :]
                cb = CN[:, t:t + 1, :].broadcast_to([128, H, 32])
                sbv = SN[:, t:t + 1, :].broadcast_to([128, H, 32])
                a1 = t1[:, 0:32 * H].rearrange("p (h d) -> p h d", d=32)
                a2 = t2[:, 0:32 * H].rearrange("p (h d) -> p h d", d=32)
                rk = PJ(c0, c1)
                R.op("dve", lambda e, a1=a1, x1=x1, cb=cb: e.tensor_tensor(out=a1, in0=x1, in1=cb, op=ALU.mult), r=rk + ["CN"], w=["t1"])
                R.op("dve", lambda e, a2=a2, x2=x2, sbv=sbv: e.tensor_tensor(out=a2, in0=x2, in1=sbv, op=ALU.mult), r=rk + ["SN"], w=["t2"])
                R.op("dve", lambda e, o1=o1, a1=a1, a2=a2: e.tensor_tensor(out=o1, in0=a1, in1=a2, op=ALU.subtract), r=["t1", "t2"], w=["rb"])
                R.op("dve", lambda e, a1=a1, x2=x2, cb=cb: e.tensor_tensor(out=a1, in0=x2, in1=cb, op=ALU.mult), r=rk + ["CN"], w=["t1"])
                R.op("dve", lambda e, a2=a2, x1=x1, sbv=sbv: e.tensor_tensor(out=a2, in0=x1, in1=sbv, op=ALU.mult), r=rk + ["SN"], w=["t2"])
                R.op("dve", lambda e, o2=o2, a1=a1, a2=a2: e.tensor_tensor(out=o2, in0=a1, in1=a2, op=ALU.add), r=["t1", "t2"], w=["rb"])
            R.op("pool", lambda e: e.tensor_copy(out=rb[:, C_CQ:C_CV], in_=pj[:, C_CQ:C_CV]), r=PJ(C_CQ, C_CV), w=["rb"])
            R.op("pool", lambda e: e.tensor_copy(out=avb[:, :, 0:128], in_=pj[:, C_AV:C_BQ].rearrange("p (h d) -> p h d", d=128)),
                 r=PJ(C_AV, C_BQ), w=["avb"])
            R.op("pool", lambda e: e.tensor_copy(out=bvb[:, 0:64], in_=pj[:, C_BV:C_IQ]), r=PJ(C_BV, C_IQ), w=["bvb"])
            R.op("pool", lambda e: e.tensor_copy(out=cvb[:, :, 0:64], in_=pj[:, C_CV:C_G].rearrange("p (h d) -> p h d", d=64)),
                 r=PJ(C_CV, C_G), w=["cvb"])
            R.op("pool", lambda e: e.tensor_scalar(out=iwb[:], in0=pj[:, C_IW:C_CQ], scalar1=1.0 / 16.0, scalar2=None, op0=ALU.mult),
                 r=PJ(C_IW, C_CQ), w=["iwb"])
            R.op("act", lambda e: e.activation(out=pj[:, C_G:INC], in_=pj[:, C_G:INC], func=AF.Sigmoid), r=PJ(C_G, INC), w=PJ(C_G, INC))
            ts = slice(t * 128, (t + 1) * 128)
            R.dma("sp", av.ap()[ts, :], avb[:].rearrange("p h d -> p (h d)"), "out", r=["avb"])
            R.dma("sp", bv.ap()[ts, :], bvb[:], "out", r=["bvb"])
            R.dma("sp", cv.ap()[ts, :], cvb[:].rearrange("p h d -> p (h d)"), "out", r=["cvb"])
            R.dma("sp", iw.ap()[ts, :], iwb[:], "out", r=["iwb"])
            R.dma("sp", gates.ap()[ts, :], pj[:, C_G:INC], "out", r=PJ(C_G, INC))
            g = 0
            for blk in range(8):
                c0 = blk * 128
                R.op("pe", lambda e, blk=blk, c0=c0: e.transpose(out=pX[0][:, blk * 128:(blk + 1) * 128], in_=rb[:, c0:c0 + 128], identity=idb[:]),
                     r=["rb", "idb"], w=["pX0"])
            R.op("act", lambda e: e.copy(out=tA[0][:], in_=pX[0][:]), r=["pX0"], w=["tA0"])
            for m in range(2):
                R.dma("sp", bass.AP(aqt, m * tok + t * 128, [[2 * tok, 64], [64 * 2 * tok, 4], [1, 128]]),
                      tA[0][m * 64:(m + 1) * 64, 0:512].rearrange("p (h q) -> p h q", q=128), "out", r=["tA0"])
                R.dma("sp", bass.AP(akt, m * tok + t * 128, [[2 * tok, 64], [64 * 2 * tok, 4], [1, 128]]),
                      tA[0][m * 64:(m + 1) * 64, 512:1024].rearrange("p (h q) -> p h q", q=128), "out", r=["tA0"])
            for blk in range(8):
                c0 = C_CQ + blk * 128
                R.op("pe", lambda e, blk=blk, c0=c0: e.transpose(out=pX[1][:, blk * 128:(blk + 1) * 128], in_=rb[:, c0:c0 + 128], identity=idb[:]),
                     r=["rb", "idb"], w=["pX1"])
            R.op("dve", lambda e: e.tensor_copy(out=tA[1][:], in_=pX[1][:]), r=["pX1"], w=["tA1"])
            for hf in range(2):
                R.dma("sp", bass.AP(cqt, hf * tok + t * 128, [[8 * tok, 64], [2 * tok, 4], [1, 128]]),
                      tA[1][hf * 64:(hf + 1) * 64, 0:512].rearrange("p (h q) -> p h q", q=128), "out", r=["tA1"])
                R.dma("sp", bass.AP(ckt, hf * tok + t * 128, [[8 * tok, 64], [2 * tok, 4], [1, 128]]),
                      tA[1][hf * 64:(hf + 1) * 64, 512:1024].rearrange("p (h q) -> p h q", q=128), "out", r=["tA1"])
            for h in range(8):
                c0 = C_BQ + h * 64
                R.op("pe", lambda e, h=h, c0=c0: e.transpose(out=pX[0][0:64, h * 128:(h + 1) * 128], in_=rb[:, c0:c0 + 64], identity=idb[:]),
                     r=["rb", "idb"], w=["pX0"])
            R.op("act", lambda e: e.copy(out=tA[0][0:64, :], in_=pX[0][0:64, :]), r=["pX0"], w=["tA0"])
            R.dma("sp", bqt.ap()[t][:, 0:1024], tA[0][0:64, :], "out", r=["tA0"])
            srcs = [C_IQ + h * 64 for h in range(4)] + [C_BK, C_IK]
            for i, c0 in enumerate(srcs):
                R.op("pe", lambda e, i=i, c0=c0: e.transpose(out=pX[1][0:64, i * 128:(i + 1) * 128], in_=rb[:, c0:c0 + 64], identity=idb[:]),
                     r=["rb", "idb"], w=["pX1"])
            R.op("dve", lambda e: e.tensor_copy(out=tA[1][0:64, 0:768], in_=pX[1][0:64, 0:768]), r=["pX1"], w=["tA1"])
            R.dma("sp", bqt.ap()[t][:, 1024:1536], tA[1][0:64, 0:512], "out", r=["tA1"])
            R.dma("sp", bkt.ap()[:, t * 128:(t + 1) * 128], tA[1][0:64, 512:640], "out", r=["tA1"])
            R.dma("sp", bkt.ap()[:, tok + t * 128:tok + (t + 1) * 128], tA[1][0:64, 640:768], "out", r=["tA1"])
        R.final_wait = ["out"]
        R.emit()


D = 1024
BIG = 30000.0
NIT = 22
EPS = 1e-6


def emit_T(nc, T, pfx, nt=16, phases="0ABCM"):
    tok = nt * 128
    NKT = 4 * nt
    S = NKT * 128
    di = lambda n, s, d=F32: T[n]
    aqt = di("aqt", [4, 64, 2 * tok], BF16); bq_iq = di("bq_iq", [nt, 64, 1536], BF16); iw = di("iw", [tok, 4])
    cqt = di("cqt", [64, 8 * tok], BF16); gates = di("gates", [tok, 3072]); x = di("x", [tok, D])
    akt = di("akt", [4, 64, 2 * S], BF16); av = di("av", [4, 128, NKT * 129], BF16); bkik = di("bkik", [64, 2 * S], BF16)
    bv = di("bv", [128, NKT * 65], BF16); ckb = di("ckb", [nt, 64, 8 * 640], BF16); cvb = di("cvb", [nt, 128, 5 * 520], BF16)
    zmT = di("zmT", [128, 512], BF16); zq = di("zq", [128, 512]); cmaskT = di("cmaskT", [128, 8 * 128])
    wa = di("wa", [512, D]); wb = di("wb", [512, D]); wc = di("wc", [512, D]); w_out = di("w_out", [D, D])
    w_mod = di("w_mod", [D, 6144]); b_mod = di("b_mod", [1, 6144]); cvec = di("c", [128, 8])
    lam4 = di("lam4", [1, 256]); ang_in = di("a_norm_g", [1, 128]); rbext = di("rbext", [8, 1024])
    ident = di("ident", [128, 128]); aident = di("aident", [128, 128]); laminit = di("laminit", [1, 2]); p2tab = di("p2tab", [1, NIT])
    xo = T["xo"]

    with ExitStack() as st, nc.allow_low_precision("bf16 matmul operands, fp32 accumulation"):
        sbo = lambda n, s, d=F32: st.enter_context(nc.sbuf_tensor(pfx + n, s, d))
        pso = lambda n, s, d=F32: st.enter_context(nc.psum_tensor(pfx + n, s, d))
        ya = sbo("ya", [128, nt, 512], BF16); yb = sbo("yb", [128, nt, 512], BF16); yc = sbo("yc", [128, nt, 512], BF16)
        g1bc = sbo("g1bc", [128, D]); idf = sbo("idf", [128, 128]); idb = sbo("idb", [128, 128], BF16); jdb = sbo("jdb", [128, 128], BF16)
        bigi4 = sbo("bigi4", [128, 512], BF16); nlam = sbo("nlam", [128, 1]); gn = sbo("gn", [128, 128])
        cst = sbo("cst", [128, 2])
        psS = [pso("psS%d" % i, [128, 1024]) for i in range(2)]
        psO = [pso("psO%d" % i, [128, 1024]) for i in range(2)]

        R = Rec(nc)
        with ExitStack() as s0:
            sb = lambda n, s, d=F32: s0.enter_context(nc.sbuf_tensor(pfx + n, s, d))
            cs = sb("cs", [128, 8]); ca = sb("ca", [128, 8]); CA = sb("CA", [128, 8, 128])
            wm = [sb("wm%d" % i, [128, 8, 256]) for i in range(2)]
            bmb = sb("bmb", [128, D]); l4 = sb("l4", [128, 4, 64]); lt = sb("lt", [128, 2, 64]); ls = sb("ls", [128, 2]); le = sb("le", [128, 2])
            li = sb("li", [128, 2]); agb = sb("agb", [128, 128]); stg = sb("stg", [128, 8, 128])
            R.dma("sp", cs[:], cvec.ap(), "c", w=["cs"])
            R.dma("sp", idf[:], ident.ap(), "c", w=["idf"])
            R.dma("sp", bmb[:], b_mod.ap()[0:1, 2048:3072].partition_broadcast(128), "c", w=["bmb"])
            R.dma("sp", l4[:].rearrange("p a b -> p (a b)"), lam4.ap()[0:1, :].partition_broadcast(128), "c", w=["l4"])
            R.dma("sp", li[:], laminit.ap()[0:1, :].partition_broadcast(128), "c", w=["li"])
            R.dma("sp", agb[:], ang_in.ap()[0:1, :].partition_broadcast(128), "c", w=["agb"])
            R.op("pool", lambda e: e.memset(cst[:, 0:1], EPS), w=["cst"])
            R.op("act", lambda e: e.activation(out=ca[:], in_=cs[:], func=AF.Silu), r=["cs"], w=["ca"])
            R.op("dve", lambda e: e.tensor_copy(out=CA[:], in_=ca[:].unsqueeze(2).broadcast_to([128, 8, 128])), r=["ca"], w=["CA"])
            R.op("dve", lambda e: e.tensor_copy(out=idb[:], in_=idf[:]), r=["idf"], w=["idb"])
            R.dma("sp", stg[:, 0, :], aident.ap(), "c", w=["stg"])
            R.op("dve", lambda e: e.tensor_copy(out=jdb[:], in_=stg[:, 0, :]), r=["stg"], w=["jdb"])
            for k in range(4):
                R.op("dve", lambda e, k=k: e.tensor_scalar(out=bigi4[:, k * 128:(k + 1) * 128], in0=idf[:], scalar1=BIG, scalar2=None, op0=ALU.mult),
                     r=["idf"], w=["bigi4"])
            wmv = w_mod.ap().rearrange("(ch p) n -> p ch n", p=128)
            for j in range(4):
                b = j % 2
                R.dma("sp", wm[b][:], wmv[:, :, 2048 + j * 256:2048 + (j + 1) * 256], "wm%d" % b, w=["wm%d" % b])
                for ch in range(8):
                    R.op("pe", lambda e, ch=ch, b=b: e.matmul(psS[b][:, 0:256], lhsT=CA[:, ch, :], rhs=wm[b][:, ch, :], start=(ch == 0), stop=(ch == 7)),
                         r=["CA", "wm%d" % b], w=["psS%d" % b])
                R.op("dve", lambda e, j=j, b=b: e.tensor_tensor(out=g1bc[:, j * 256:(j + 1) * 256], in0=psS[b][:, 0:256], in1=bmb[:, j * 256:(j + 1) * 256], op=ALU.add),
                     r=["psS%d" % b, "bmb"], w=["g1bc"])
            R.op("dve", lambda e: e.tensor_tensor(out=lt[:, 0, :], in0=l4[:, 0, :], in1=l4[:, 1, :], op=ALU.mult), r=["l4"], w=["lt"])
            R.op("dve", lambda e: e.tensor_tensor(out=lt[:, 1, :], in0=l4[:, 2, :], in1=l4[:, 3, :], op=ALU.mult), r=["l4"], w=["lt"])
            R.op("dve", lambda e: e.tensor_reduce(out=ls[:], in_=lt[:], axis=AX.X, op=ALU.add), r=["lt"], w=["ls"])
            R.op("act", lambda e: e.activation(out=le[:], in_=ls[:], func=AF.Exp), r=["ls"], w=["le"])
            R.op("dve", lambda e: e.tensor_tensor(out=nlam[:], in0=le[:, 1:2], in1=le[:, 0:1], op=ALU.subtract), r=["le"], w=["nlam"])
            R.op("dve", lambda e: e.tensor_tensor(out=nlam[:], in0=nlam[:], in1=li[:, 0:1], op=ALU.subtract), r=["nlam", "li"], w=["nlam"])
            R.op("dve", lambda e: e.tensor_scalar(out=gn[:], in0=agb[:], scalar1=li[:, 1:2], scalar2=None, op0=ALU.mult), r=["agb", "li"], w=["gn"])
            R.final_wait = list(R.dma_count.keys())
            if "0" in phases:
                R.emit()
        nc.all_engine_barrier()

        def attn_epilogue_BC(R, po, pk, ydst, yk, rinv):
            pv = po[:].rearrange("p (a c) -> p a c", a=2)[:, :, 0:260].rearrange("p a (h e) -> p a h e", e=65)
            R.op("dve", lambda e: e.reciprocal(out=rinv[:].rearrange("p (a h) -> p a h", a=2), in_=pv[:, :, :, 64]), r=[pk], w=["rinv"])
            R.op("dve", lambda e: e.tensor_tensor(out=ydst.rearrange("p (a h e) -> p a h e", a=2, e=64), in0=pv[:, :, :, 0:64],
                                                  in1=rinv[:].rearrange("p (a h) -> p a h", a=2).unsqueeze(3).broadcast_to([128, 2, 4, 64]), op=ALU.mult),
                 r=[pk, "rinv"], w=[yk])

        def hoff(h):
            return (h // 4) * 512 + (h % 4) * 65

        R = Rec(nc)
        with ExitStack() as s1:
            sb = lambda n, s, d=F32: s1.enter_context(nc.sbuf_tensor(pfx + n, s, d))
            ktb = [sb("ktb%d" % i, [64, 2, S], BF16) for i in range(2)]
            avh = [sb("avh%d" % i, [128, NKT, 129], BF16) for i in range(2)]
            qh = [sb("qh%d" % i, [64, 2, tok], BF16) for i in range(2)]
            zm = sb("zm", [128, 4, 128], BF16)
            pt = [sb("pt%d" % i, [128, 4, 2, 128], BF16) for i in range(2)]
            r12 = sb("r12", [128, 2]); nr2 = sb("nr2", [128, 1]); d1 = sb("d1", [128, 128]); dd = sb("dd", [128, 128])
            jk = sb("jk", [128, 128]); ssq = sb("ssq", [128, 1]); lnv = sb("lnv", [128, 1]); rstd = sb("rstd", [128, 1])
            R.dma("sp", zm[:].rearrange("p a b -> p (a b)"), zmT.ap(), "c", w=["zm"])
            gi = 0
            for h in range(4):
                hb = h % 2
                for m_ in range(2):
                    for r_ in range(4):
                        R.dma("sp", ktb[hb][:, m_, :].rearrange("d (t r p) -> d t r p", r=4, p=128)[:, :, r_, :],
                              bass.AP(akt[h // 2], ((r_ * 2 + h % 2) * 64) * 2 * tok + m_ * tok, [[2 * tok, 64], [128, nt], [1, 128]]), "kt%d" % hb, w=["ktb%d" % hb])
                SL = min(4, nt)
                for c_ in range(nt // SL):
                    for r_ in range(4):
                        R.dma("sp", avh[hb][:].rearrange("p (t r) e -> p t r e", r=4)[:, c_ * SL:(c_ + 1) * SL, r_, :],
                              bass.AP(av[c_], r_ * SL * 128 * 516 + h * 129, [[516, 128], [128 * 516, SL], [1, 129]]), "av%d" % hb, w=["avh%d" % hb])
                R.dma("sp", qh[hb][:].rearrange("p a b -> p (a b)"), aqt.ap()[h], "qh%d" % hb, w=["qh%d" % hb])
                jobs = [(t, g) for t in range(nt) for g in range(t + 1)]

                def a_qk(t, g, b, hb=hb):
                    qs = slice(t * 128, (t + 1) * 128)
                    zone = (g == t)
                    for i in range(4):
                        kt = 4 * g + i
                        ks = slice(kt * 128, (kt + 1) * 128)
                        for m in range(2):
                            ps_out = psS[b][:, (i * 2 + m) * 128:(i * 2 + m + 1) * 128]
                            R.op("pe", lambda e, ps_out=ps_out, m=m, ks=ks, qs=qs, zone=zone: e.matmul(
                                ps_out, lhsT=ktb[hb][:, m, ks], rhs=qh[hb][:, m, qs], start=True, stop=not zone),
                                r=["ktb%d" % hb, "qh%d" % hb], w=["psS%d" % b])
                            if zone:
                                R.op("pe", lambda e, ps_out=ps_out, i=i: e.matmul(ps_out, lhsT=idb[:], rhs=zm[:, i, :], start=False, stop=True),
                                     r=["zm"], w=["psS%d" % b])

                def a_pv(t, g, b, hb=hb):
                    ob = t % 2
                    ok = "psO%d" % ob
                    R.op("act", lambda e: e.activation(out=pt[b][:].rearrange("p a m q -> p (a m q)"), in_=psS[b][:], func=AF.Exp, scale=0.125),
                         r=["psS%d" % b], w=["pt%d" % b])
                    for i in range(4):
                        kt = 4 * g + i
                        for m in range(2):
                            R.op("pe", lambda e, i=i, m=m, kt=kt: e.matmul(
                                psO[ob][:, m * 512:m * 512 + 129], lhsT=pt[b][:, i, m, :], rhs=avh[hb][:, kt, :],
                                start=(g == 0 and i == 0), stop=(g == t and i == 3)),
                                r=["pt%d" % b, "avh%d" % hb], w=[ok])
                    if g != t:
                        return
                    po = psO[ob]
                    R.op("dve", lambda e: e.reciprocal(out=r12[:, 0:1], in_=po[:, 128:129]), r=[ok], w=["r12"])
                    R.op("dve", lambda e: e.reciprocal(out=r12[:, 1:2], in_=po[:, 640:641]), r=[ok], w=["r12"])
                    R.op("dve", lambda e: e.tensor_tensor(out=nr2[:], in0=r12[:, 1:2], in1=nlam[:], op=ALU.mult), r=["r12"], w=["nr2"])
                    R.op("dve", lambda e: e.tensor_scalar(out=d1[:], in0=po[:, 0:128], scalar1=r12[:, 0:1], scalar2=None, op0=ALU.mult),
                         r=[ok, "r12"], w=["d1"])
                    R.op("dve", lambda e: e.scalar_tensor_tensor(out=dd[:], in0=po[:, 512:640], scalar=nr2[:, 0:1], in1=d1[:], op0=ALU.mult, op1=ALU.add),
                         r=[ok, "nr2", "d1"], w=["dd"])

                def a_norm(t, h=h):
                    R.op("act", lambda e: e.activation(out=jk[:], in_=dd[:], func=AF.Square, accum_out=ssq[:]), r=["dd"], w=["jk", "ssq"])
                    R.op("act", lambda e: e.activation(out=lnv[:], in_=ssq[:], func=AF.Ln, scale=1.0 / 128.0, bias=cst[:, 0:1]), r=["ssq"], w=["lnv"])
                    R.op("act", lambda e: e.activation(out=rstd[:], in_=lnv[:], func=AF.Exp, scale=-0.5), r=["lnv"], w=["rstd"])
                    R.op("dve", lambda e: e.scalar_tensor_tensor(out=ya[:, t, h * 128:(h + 1) * 128], in0=dd[:], scalar=rstd[:, 0:1], in1=gn[:],
                                                                 op0=ALU.mult, op1=ALU.mult), r=["dd", "rstd"], w=["ya"])

                pend = None
                a_qk(jobs[0][0], jobs[0][1], gi % 2)
                for ji, (t, g) in enumerate(jobs):
                    b = gi % 2
                    gi += 1
                    if ji + 1 < len(jobs):
                        a_qk(jobs[ji + 1][0], jobs[ji + 1][1], gi % 2)
                    a_pv(t, g, b)
                    if pend is not None:
                        a_norm(pend)
                        pend = None
                    if g == t:
                        pend = t
                if pend is not None:
                    a_norm(pend)
            R.final_wait = list(R.dma_count.keys())
            if "A" in phases:
                R.emit()
        nc.all_engine_barrier()

        R = Rec(nc)
        with ExitStack() as s2:
            sb = lambda n, s, d=F32: s2.enter_context(nc.sbuf_tensor(pfx + n, s, d))
            kk = sb("kk", [64, 2, S], BF16); bvs = sb("bvs", [128, NKT, 65], BF16)
            sc = sb("sc", [128, S]); cA = sb("cA", [128, S], BF16); cB = sb("cB", [128, S], BF16)
            mk = [sb("mk%d" % i, [128, S], BF16) for i in range(2)]
            bqi = [sb("bqi%d" % i, [64, 1536], BF16) for i in range(2)]
            iwt = [sb("iwt%d" % i, [128, 4]) for i in range(2)]
            pt = [sb("ptb%d" % i, [128, 1024], BF16) for i in range(2)]
            rl = [sb("rl%d" % i, [128, 512]) for i in range(2)] * 2
            zqs = sb("zqs", [128, 512]); p2 = sb("p2", [128, NIT]); WN = sb("WN", [128, NIT])
            rmin = sb("rmin", [128, 1]); rmax = sb("rmax", [128, 1]); w0 = sb("w0", [128, 1]); lo = sb("lo", [128, 1]); mid = sb("mid", [128, 1])
            cnt = sb("cnt", [128, 1]); dl = sb("dl", [128, 1]); hi = sb("hi", [128, 1]); chi = sb("chi", [128, 1]); mrem = sb("mrem", [128, 1])
            rinv = sb("rinv", [128, 8])
            for m_ in range(2):
                for r_ in range(4):
                    R.dma("sp", kk[:, m_, :].rearrange("d (t r p) -> d t r p", r=4, p=128)[:, :, r_, :],
                          bass.AP(bkik, r_ * 64 * 2 * tok + m_ * tok, [[2 * tok, 64], [128, nt], [1, 128]]), "c", w=["kk"])
            for r_ in range(4):
                R.dma("sp", bvs[:].rearrange("p (t r) e -> p t r e", r=4)[:, :, r_, :],
                      bass.AP(bv, r_ * tok * 65, [[65, 128], [128 * 65, nt], [1, 65]]), "c", w=["bvs"])
            R.dma("sp", zqs[:], zq.ap(), "c", w=["zqs"])
            R.dma("sp", p2[:], p2tab.ap()[0:1, :].partition_broadcast(128), "c", w=["p2"])
            psI = [psS[hh // 2][:, (hh % 2) * 512:(hh % 2) * 512 + 512] for hh in range(4)]
            def b_select(t):
                b = t % 2
                n = (4 * t + 4) * 128
                R.dma("sp", bqi[b][:], bq_iq.ap()[t], "bqi%d" % b, w=["bqi%d" % b])
                R.dma("sp", iwt[b][:], iw.ap()[t * 128:(t + 1) * 128, :], "bqi%d" % b, w=["iwt%d" % b])
                for g in range(t + 1):
                    gs_ = slice(g * 512, (g + 1) * 512)
                    for hh in range(4):
                        R.op("pe", lambda e, hh=hh, b=b, gs_=gs_: e.matmul(psI[hh], lhsT=bqi[b][:, 1024 + hh * 128:1024 + (hh + 1) * 128], rhs=kk[:, 1, gs_],
                                                                           start=True, stop=True), r=["bqi%d" % b, "kk"], w=["psS%d" % (hh // 2)])
                        R.op("act", lambda e, hh=hh: e.activation(out=rl[hh][:], in_=psI[hh], func=AF.Relu), r=["psS%d" % (hh // 2)], w=["rl%d" % (hh % 2)])
                        if hh == 0:
                            R.op("dve", lambda e, b=b, gs_=gs_: e.tensor_scalar(out=sc[:, gs_], in0=rl[0][:], scalar1=iwt[b][:, 0:1], scalar2=None, op0=ALU.mult),
                                 r=["rl0", "iwt%d" % b], w=["sc"])
                        else:
                            R.op("dve", lambda e, hh=hh, b=b, gs_=gs_: e.scalar_tensor_tensor(out=sc[:, gs_], in0=rl[hh][:], scalar=iwt[b][:, hh:hh + 1], in1=sc[:, gs_],
                                                                                               op0=ALU.mult, op1=ALU.add), r=["rl%d" % (hh % 2), "iwt%d" % b, "sc"], w=["sc"])
                R.op("dve", lambda e, n=n: e.tensor_reduce(out=rmin[:], in_=sc[:, 0:n], axis=AX.X, op=ALU.min), r=["sc"], w=["rmin"])
                R.op("dve", lambda e, t=t: e.tensor_tensor(out=sc[:, t * 512:(t + 1) * 512], in0=sc[:, t * 512:(t + 1) * 512], in1=zqs[:], op=ALU.add),
                     r=["sc", "zqs"], w=["sc"])
                R.op("dve", lambda e, n=n: e.tensor_reduce(out=rmax[:], in_=sc[:, 0:n], axis=AX.X, op=ALU.max), r=["sc"], w=["rmax"])
                R.op("dve", lambda e: e.tensor_tensor(out=w0[:], in0=rmax[:], in1=rmin[:], op=ALU.subtract), r=["rmax", "rmin"], w=["w0"])
                R.op("dve", lambda e: e.tensor_scalar(out=w0[:], in0=w0[:], scalar1=1.001, scalar2=1e-6, op0=ALU.mult, op1=ALU.add), r=["w0"], w=["w0"])
                R.op("dve", lambda e: e.tensor_scalar(out=WN[:], in0=p2[:], scalar1=w0[:, 0:1], scalar2=None, op0=ALU.mult), r=["p2", "w0"], w=["WN"])
                R.op("dve", lambda e: e.tensor_copy(out=lo[:], in_=rmin[:]), r=["rmin"], w=["lo"])
                for it in range(NIT):
                    R.op("dve", lambda e, it=it: e.tensor_tensor(out=mid[:], in0=lo[:], in1=WN[:, it:it + 1], op=ALU.add), r=["lo", "WN"], w=["mid"])
                    R.op("dve", lambda e, n=n: e.tensor_scalar(out=cA[:, 0:n], in0=sc[:, 0:n], scalar1=mid[:, 0:1], scalar2=None, op0=ALU.is_ge, op1=ALU.add,
                                                               accum_out=cnt[:]), r=["sc", "mid"], w=["cA", "cnt"])
                    R.op("dve", lambda e, it=it: e.tensor_scalar(out=dl[:], in0=cnt[:], scalar1=256.0, scalar2=WN[:, it:it + 1], op0=ALU.is_ge, op1=ALU.mult),
                         r=["cnt", "WN"], w=["dl"])
                    R.op("dve", lambda e: e.tensor_tensor(out=lo[:], in0=lo[:], in1=dl[:], op=ALU.add), r=["lo", "dl"], w=["lo"])
                R.op("dve", lambda e: e.tensor_tensor(out=hi[:], in0=lo[:], in1=WN[:, NIT - 1:NIT], op=ALU.add), r=["lo", "WN"], w=["hi"])
                R.op("dve", lambda e, n=n: e.tensor_scalar(out=cB[:, 0:n], in0=sc[:, 0:n], scalar1=hi[:, 0:1], scalar2=None, op0=ALU.is_ge, op1=ALU.add,
                                                           accum_out=chi[:]), r=["sc", "hi"], w=["cB", "chi"])
                R.op("dve", lambda e, n=n: e.tensor_scalar(out=cA[:, 0:n], in0=sc[:, 0:n], scalar1=lo[:, 0:1], scalar2=None, op0=ALU.is_ge), r=["sc", "lo"], w=["cA"])
                R.op("dve", lambda e, n=n: e.tensor_tensor(out=cA[:, 0:n], in0=cA[:, 0:n], in1=cB[:, 0:n], op=ALU.subtract), r=["cA", "cB"], w=["cA"])
                R.op("dve", lambda e: e.tensor_scalar(out=mrem[:], in0=chi[:], scalar1=-1.0, scalar2=256.0, op0=ALU.mult, op1=ALU.add), r=["chi"], w=["mrem"])
                R.op("dve", lambda e, n=n: e.tensor_tensor_scan(out=sc[:, 0:n], data0=cA[:, 0:n], data1=cA[:, 0:n], initial=0.0, op0=ALU.add, op1=ALU.max),
                     r=["cA"], w=["sc"])
                R.op("dve", lambda e, n=n: e.scalar_tensor_tensor(out=cA[:, 0:n], in0=sc[:, 0:n], scalar=mrem[:, 0:1], in1=cA[:, 0:n], op0=ALU.is_le, op1=ALU.mult),
                     r=["sc", "mrem", "cA"], w=["cA"])
                R.op("dve", lambda e, n=n, b=b: e.scalar_tensor_tensor(out=mk[b][:, 0:n], in0=cA[:, 0:n], scalar=-1.0, in1=cB[:, 0:n], op0=ALU.add, op1=ALU.add),
                     r=["cA", "cB"], w=["mk%d" % b])

            def b_attend(t):
                b = t % 2
                ob = t % 2
                ok = "psO%d" % ob
                for kt in range(4 * t + 4):
                    b2 = kt % 2
                    ks = slice(kt * 128, (kt + 1) * 128)
                    for half in range(2):
                        hs = slice(half * 512, (half + 1) * 512)
                        R.op("pe", lambda e, b2=b2, hs=hs, ks=ks, b=b: e.matmul(psS[b2][:, hs], lhsT=kk[:, 0, ks], rhs=bqi[b][:, hs], start=True, stop=False),
                             r=["kk", "bqi%d" % b], w=["psS%d" % b2])
                        R.op("pe", lambda e, b2=b2, hs=hs, ks=ks, b=b: e.matmul(psS[b2][:, hs], lhsT=mk[b][:, ks], rhs=bigi4[:], start=False, stop=True),
                             r=["mk%d" % b], w=["psS%d" % b2])
                    R.op("act", lambda e, b2=b2: e.activation(out=pt[b2][:], in_=psS[b2][:], func=AF.Exp, scale=0.125), r=["psS%d" % b2], w=["ptb%d" % b2])
                    for h in range(8):
                        R.op("pe", lambda e, h=h, b2=b2, kt=kt, ob=ob, t=t: e.matmul(psO[ob][:, hoff(h):hoff(h) + 65], lhsT=pt[b2][:, h * 128:(h + 1) * 128],
                                                                                    rhs=bvs[:, kt, :], start=(kt == 0 and h % 4 == 0), stop=(kt == 4 * t + 3 and h % 4 == 3)),
                             r=["ptb%d" % b2, "bvs"], w=[ok])
                attn_epilogue_BC(R, psO[ob], ok, yb[:, t, :], "yb", rinv)

            b_select(0)
            for t in range(1, nt):
                b_select(t)
                b_attend(t - 1)
            b_attend(nt - 1)
            R.final_wait = list(R.dma_count.keys())
            if "B" in phases:
                R.emit()
        nc.all_engine_barrier()

        R = Rec(nc)
        with ExitStack() as s3:
            sb = lambda n, s, d=F32: s3.enter_context(nc.sbuf_tensor(pfx + n, s, d))
            cqs = sb("cqs", [64, 8, tok], BF16)
            EBT = sb("EBT", [128, 8, 8, 128], BF16); stg = sb("stgc", [128, 8, 128]); cmk = sb("cmk", [128, 8, 128])
            R.dma("sp", cmk[:].rearrange("p a b -> p (a b)"), cmaskT.ap(), "c", w=["cmk"])
            for j in range(8):
                src = bass.AP(rbext, T["rb_off"] + 1665 - 128 * j, [[1, 128], [T["rb_w"], 8], [1, 128]])
                R.dma("sp", stg[:], src, "stg", w=["stg"])
                R.op("dve", lambda e, j=j: e.scalar_tensor_tensor(out=EBT[:, j, :, :], in0=stg[:], scalar=8.0,
                                                                   in1=cmk[:, j, :].unsqueeze(1).broadcast_to([128, 8, 128]),
                                                                   op0=ALU.mult, op1=ALU.add), r=["stg", "cmk"], w=["EBT"])
            ck = [sb("ck%d" % i, [64, 8, 8, 128], BF16) for i in range(2)]
            cvs = [sb("cvs%d" % i, [128, 8, 520], BF16) for i in range(2)]
            pt = [sb("ptc%d" % i, [128, 1024], BF16) for i in range(2)]
            rinv = sb("rinvc", [128, 8])
            R.dma("sp", cqs[:].rearrange("p a b -> p (a b)"), cqt.ap(), "c", w=["cqs"])
            gi = 0
            for t in range(nt):
                b = t % 2
                for si, s_ in enumerate((t - 1, t)):
                    if s_ < 0:
                        R.op("pool", lambda e, b=b: e.memset(ck[b][:, :, 0:4, :], 0.0), w=["ck%d" % b])
                        R.op("pool", lambda e, b=b: e.memset(cvs[b][:, 0:4, :], 0.0), w=["cvs%d" % b])
                        continue
                    for r_ in range(4):
                        for c_ in range(2):
                            R.dma("sp", ck[b][c_ * 32:(c_ + 1) * 32, :, si * 4 + r_, :],
                                  bass.AP(ckb[c_], r_ * 32 * 8 * tok + s_ * 128, [[8 * tok, 32], [tok, 8], [1, 128]]), "ck%d" % b, w=["ck%d" % b])
                    SLc = min(4, nt)
                    R.dma("sp", cvs[b][:, si * 4:(si + 1) * 4, :], bass.AP(cvb[s_ // SLc], (s_ % SLc) * 128 * 520, [[520, 128], [SLc * 128 * 520, 4], [1, 520]]),
                          "ck%d" % b, w=["cvs%d" % b])
                ob = t % 2
                ok = "psO%d" % ob
                for j in range(8):
                    b2 = gi % 2
                    gi += 1
                    for h in range(8):
                        R.op("pe", lambda e, h=h, b=b, b2=b2, j=j, t=t: e.matmul(
                            psS[b2][:, h * 128:(h + 1) * 128], lhsT=ck[b][:, h, j, :],
                            rhs=cqs[:, h, t * 128:(t + 1) * 128], start=(h % 4 == 0), stop=False), r=["ck%d" % b, "cqs"], w=["psS%d" % b2])
                    for half in range(2):
                        R.op("pe", lambda e, half=half, b2=b2, j=j: e.matmul(psS[b2][:, half * 512:(half + 1) * 512], lhsT=jdb[:],
                                                                             rhs=EBT[:, j, half * 4:(half + 1) * 4, :].rearrange("p h q -> p (h q)"),
                                                                             start=False, stop=True), r=["EBT"], w=["psS%d" % b2])
                    R.op("act", lambda e, b2=b2: e.activation(out=pt[b2][:], in_=psS[b2][:], func=AF.Exp, scale=0.125), r=["psS%d" % b2], w=["ptc%d" % b2])
                    for h in range(8):
                        R.op("pe", lambda e, h=h, b2=b2, j=j, b=b, ob=ob: e.matmul(psO[ob][:, hoff(h):hoff(h) + 65], lhsT=pt[b2][:, h * 128:(h + 1) * 128],
                                                                                  rhs=cvs[b][:, j, h * 65:(h + 1) * 65], start=(j == 0 and h % 4 == 0), stop=(j == 7 and h % 4 == 3)),
                             r=["ptc%d" % b2, "cvs%d" % b], w=[ok])
                attn_epilogue_BC(R, psO[ob], ok, yc[:, t, :], "yc", rinv)
            R.final_wait = list(R.dma_count.keys())
            if "C" in phases:
                R.emit()
        nc.all_engine_barrier()

        R = Rec(nc)
        with ExitStack() as s4:
            sb = lambda n, s, d=F32: s4.enter_context(nc.sbuf_tensor(pfx + n, s, d))
            wbr = [sb("wbr%d" % i, [128, 4, D], BF16) for i in range(3)]
            wo = sb("wo", [128, 8, D], BF16)
            gt = [sb("gt%d" % i, [128, 3072]) for i in range(2)]
            xt = [sb("xt%d" % i, [128, D]) for i in range(2)]
            yT = sb("yT", [128, 512], BF16); mg = sb("mg", [128, D]); tt = sb("tt", [128, 512]); mgb = sb("mgb", [128, D], BF16)
            mT = sb("mT", [128, D], BF16); xot = [sb("xot%d" % i, [128, D]) for i in range(2)]
            for i, wsrc in enumerate((wa, wb, wc)):
                R.dma("pool", wbr[i][:], wsrc.ap().rearrange("(ch p) n -> p ch n", p=128), "w", w=["wbr%d" % i])
            R.dma("pool", wo[:], w_out.ap().rearrange("(ch p) n -> p ch n", p=128), "w", w=["wo"])
            pXt = psO[0][:, 0:512].bitcast(BF16)
            assert tuple(pXt.shape) == (128, 1024), pXt.shape
            for t in range(nt):
                b = t % 2
                R.dma("sp", gt[b][:], gates.ap()[t * 128:(t + 1) * 128, :], "gx%d" % b, w=["gt%d" % b])
                R.dma("sp", xt[b][:], x.ap()[t * 128:(t + 1) * 128, :], "gx%d" % b, w=["xt%d" % b])
                for bi, (ysrc, yk) in enumerate(((ya, "ya"), (yb, "yb"), (yc, "yc"))):
                    for ch in range(4):
                        R.op("pe", lambda e, ysrc=ysrc, ch=ch, t=t: e.transpose(out=pXt[:, ch * 128:(ch + 1) * 128], in_=ysrc[:, t, ch * 128:(ch + 1) * 128], identity=idb[:]),
                             r=[], w=["pXt"])
                    R.op("act", lambda e: e.copy(out=yT[:], in_=pXt[:, 0:512]), r=["pXt"], w=["yT"])
                    for half in range(2):
                        hs = slice(half * 512, (half + 1) * 512)
                        for ch in range(4):
                            R.op("pe", lambda e, ch=ch, bi=bi, half=half, hs=hs: e.matmul(psS[half][:, 0:512], lhsT=yT[:, ch * 128:(ch + 1) * 128], rhs=wbr[bi][:, ch, hs],
                                                                                          start=(ch == 0), stop=(ch == 3)), r=["yT", "wbr%d" % bi], w=["psS%d" % half])
                        gsl = gt[b][:, bi * 1024 + half * 512: bi * 1024 + (half + 1) * 512]
                        if bi == 0:
                            R.op("dve", lambda e, half=half, hs=hs, gsl=gsl: e.tensor_tensor(out=mg[:, hs], in0=psS[half][:, 0:512], in1=gsl, op=ALU.mult),
                                 r=["psS%d" % half, "gt%d" % b], w=["mg%d" % half])
                        else:
                            R.op("dve", lambda e, half=half, gsl=gsl: e.tensor_tensor(out=tt[:], in0=psS[half][:, 0:512], in1=gsl, op=ALU.mult),
                                 r=["psS%d" % half, "gt%d" % b], w=["tt"])
                            dst = mg if bi == 1 else mgb
                            R.op("dve", lambda e, hs=hs, dst=dst: e.tensor_tensor(out=dst[:, hs], in0=mg[:, hs], in1=tt[:], op=ALU.add),
                                 r=["tt", "mg%d" % half], w=["mg%d" % half if bi == 1 else "mgb%d" % half])
                for ch in range(8):
                    R.op("pe", lambda e, ch=ch: e.transpose(out=pXt[:, ch * 128:(ch + 1) * 128], in_=mgb[:, ch * 128:(ch + 1) * 128], identity=idb[:]),
                         r=["mgb0", "mgb1"], w=["pXt"])
                R.op("act", lambda e: e.copy(out=mT[:], in_=pXt[:]), r=["pXt"], w=["mT"])
                for half in range(2):
                    hs = slice(half * 512, (half + 1) * 512)
                    for ch in range(8):
                        R.op("pe", lambda e, ch=ch, half=half, hs=hs: e.matmul(psS[half][:, 0:512], lhsT=mT[:, ch * 128:(ch + 1) * 128], rhs=wo[:, ch, hs],
                                                                               start=(ch == 0), stop=(ch == 7)), r=["mT", "wo"], w=["psS%d" % half])
                    R.op("dve", lambda e, half=half, hs=hs: e.tensor_tensor(out=tt[:], in0=psS[half][:, 0:512], in1=g1bc[:, hs], op=ALU.mult),
                         r=["psS%d" % half], w=["tt"])
                    R.op("dve", lambda e, hs=hs, b=b: e.tensor_tensor(out=xot[b][:, hs], in0=tt[:], in1=xt[b][:, hs], op=ALU.add),
                         r=["tt", "xt%d" % b], w=["xot%d_%d" % (b, half)])
                R.dma("sp", xo.ap()[t * 128:(t + 1) * 128, :], xot[b][:], "out%d" % b, r=["xot%d_0" % b, "xot%d_1" % b])
            R.final_wait = list(R.dma_count.keys())
            if "M" in phases:
                R.emit()


D = 1024
EPS = 1e-6
NE = 16
FF = 512
BIGR = 1.0e4


def emit_M(nc, T, pfx, nt=16, final=False, ne=NE, phases="123", cut=9):
    tok = nt * 128
    di = lambda n, s, d=F32: T[n]
    x = di("x", [tok, D]); cvec = di("c", [128, 8]); w_mod = di("w_mod", [D, 6144]); b_mod = di("b_mod", [1, 6144])
    norm_g = di("norm_g", [1, D]); router_w = di("router_w", [D, 16]); router_b = di("router_b", [1, 16])
    w1 = di("w1", [NE, D, FF]); w3 = di("w3", [NE, D, FF]); w2 = di("w2", [NE, FF, D]); ident = di("ident", [128, 128])
    final_g = di("final_g", [1, D])
    xo = T["xo"]
    TG = (nt + 3) // 4

    with ExitStack() as st, nc.allow_low_precision("bf16 matmul operands, fp32 accumulation"):
        sbo = lambda n, s, d=F32: st.enter_context(nc.sbuf_tensor(pfx + n, s, d))
        pso = lambda n, s, d=F32: st.enter_context(nc.psum_tensor(pfx + n, s, d))
        modbc = sbo("modbc", [128, 3 * D]); gs = sbo("gs", [128, D]); idf = sbo("idf", [128, 128])
        uT = sbo("uT", [128, 8, tok], BF16); comb = sbo("comb", [128, nt, 16]); yacc = sbo("yacc", [128, nt, D])
        cst = sbo("cst", [128, 2]); fgb = sbo("fgb", [128, D])
        PS = [pso("PS%d" % i, [128, 512]) for i in range(8)]

        R = Rec(nc)
        with ExitStack() as s0:
            sb = lambda n, s, d=F32: s0.enter_context(nc.sbuf_tensor(pfx + n, s, d))
            cs = sb("cs", [128, 8]); ca = sb("ca", [128, 8]); CA = sb("CA", [128, 8, 128])
            wm = [sb("wm%d" % i, [128, 8, 256]) for i in range(2)]
            bmb = sb("bmb", [128, 3 * D]); gbc = sb("gbc", [128, D]); rw = sb("rw", [128, 8, 16]); rbb = sb("rbb", [128, 16])
            xt = [sb("xt%d" % i, [128, D]) for i in range(2)]
            junk = sb("junk", [128, D], BF16); ss = sb("ss", [128, 1]); rt = sb("rt", [128, 1]); rstd = sb("rstd", [128, 1])
            tmp = sb("tmp", [128, D]); u2 = sb("u2", [128, D]); uh = sb("uh", [128, D], BF16); ul = sb("ul", [128, D], BF16)
            ulT = sb("ulT", [128, D], BF16); idb = sb("idb", [128, 128], BF16); rwh = sb("rwh", [128, 8, 16], BF16); rwl = sb("rwl", [128, 8, 16], BF16)
            pXh = PS[2][:].bitcast(BF16); pXl = PS[3][:].bitcast(BF16)
            aff = sb("aff", [128, 16]); sel = sb("sel", [128, 16]); m1 = sb("m1", [128, 4]); eq = sb("eq", [128, 16]); s2 = sb("s2", [128, 16])
            m2 = sb("m2", [128, 4]); gsum = sb("gsum", [128, 4]); gmax = sb("gmax", [128, 1]); ing = sb("ing", [128, 4]); pen = sb("pen", [128, 4])
            selm = sb("selm", [128, 16]); t1 = sb("t1", [128, 1]); e1 = sb("e1", [128, 16]); t2 = sb("t2", [128, 1]); e2 = sb("e2", [128, 16])
            den = sb("den", [128, 1]); rden = sb("rden", [128, 1])
            R.dma("sp", cs[:], cvec.ap(), "c", w=["cs"])
            R.dma("sp", idf[:], ident.ap(), "c", w=["idf"])
            R.dma("sp", bmb[:], b_mod.ap()[0:1, 3072:6144].partition_broadcast(128), "c", w=["bmb"])
            R.dma("sp", gbc[:], norm_g.ap()[0:1, :].partition_broadcast(128), "c", w=["gbc"])
            R.dma("sp", fgb[:], final_g.ap()[0:1, :].partition_broadcast(128), "c", w=["fgb"])
            R.dma("sp", rbb[:], router_b.ap()[0:1, :].partition_broadcast(128), "c", w=["rbb"])
            R.dma("sp", rw[:], router_w.ap().rearrange("(ch p) n -> p ch n", p=128), "c", w=["rw"])
            R.op("pool", lambda e: e.memset(cst[:, 0:1], EPS), w=["cst"])
            R.op("dve", lambda e: e.tensor_copy(out=idb[:], in_=idf[:]), r=["idf"], w=["idb"])
            R.op("dve", lambda e: e.tensor_copy(out=rwh[:], in_=rw[:]), r=["rw"], w=["rwh"])
            R.op("dve", lambda e: e.tensor_tensor(out=rwl[:], in0=rw[:], in1=rwh[:], op=ALU.subtract), r=["rw", "rwh"], w=["rwl"])
            R.op("act", lambda e: e.activation(out=ca[:], in_=cs[:], func=AF.Silu), r=["cs"], w=["ca"])
            R.op("dve", lambda e: e.tensor_copy(out=CA[:], in_=ca[:].unsqueeze(2).broadcast_to([128, 8, 128])), r=["ca"], w=["CA"])
            wmv = w_mod.ap().rearrange("(ch p) n -> p ch n", p=128)
            for j in range(12):
                b = j % 2
                R.dma("sp", wm[b][:], wmv[:, :, 3072 + j * 256:3072 + (j + 1) * 256], "wm%d" % b, w=["wm%d" % b])
                for ch in range(8):
                    R.op("pe", lambda e, ch=ch, b=b: e.matmul(PS[b][:, 0:256], lhsT=CA[:, ch, :], rhs=wm[b][:, ch, :], start=(ch == 0), stop=(ch == 7)),
                         r=["CA", "wm%d" % b], w=["PS%d" % b])
                R.op("dve", lambda e, j=j, b=b: e.tensor_tensor(out=modbc[:, j * 256:(j + 1) * 256], in0=PS[b][:, 0:256], in1=bmb[:, j * 256:(j + 1) * 256], op=ALU.add),
                     r=["PS%d" % b, "bmb"], w=["modbc"])
            R.op("dve", lambda e: e.scalar_tensor_tensor(out=gs[:], in0=modbc[:, D:2 * D], scalar=1.0, in1=gbc[:], op0=ALU.add, op1=ALU.mult),
                 r=["modbc", "gbc"], w=["gs"])
            for t in range(nt):
                b = t % 2
                xk = "xt%d" % b
                R.dma("sp", xt[b][:], x.ap()[t * 128:(t + 1) * 128, :], xk, w=[xk])
                R.op("act", lambda e, b=b: e.activation(out=junk[:], in_=xt[b][:], func=AF.Square, accum_out=ss[:]), r=[xk], w=["junk", "ss"])
                R.op("act", lambda e: e.activation(out=rt[:], in_=ss[:], func=AF.Sqrt, scale=1.0 / D, bias=cst[:, 0:1]), r=["ss", "cst"], w=["rt"])
                R.op("dve", lambda e: e.reciprocal(out=rstd[:], in_=rt[:]), r=["rt"], w=["rstd"])
                R.op("dve", lambda e, b=b: e.scalar_tensor_tensor(out=tmp[:], in0=xt[b][:], scalar=rstd[:, 0:1], in1=gs[:], op0=ALU.mult, op1=ALU.mult),
                     r=[xk, "rstd", "gs"], w=["tmp"])
                R.op("dve", lambda e: e.tensor_tensor(out=u2[:], in0=tmp[:], in1=modbc[:, 0:D], op=ALU.add), r=["tmp", "modbc"], w=["u2"])
                if cut < 2:
                    continue
                R.op("dve", lambda e: e.tensor_copy(out=uh[:], in_=u2[:]), r=["u2"], w=["uh"])
                R.op("dve", lambda e: e.tensor_tensor(out=ul[:], in0=u2[:], in1=uh[:], op=ALU.subtract), r=["u2", "uh"], w=["ul"])
                for ch in range(8):
                    R.op("pe", lambda e, ch=ch: e.transpose(out=pXh[:, ch * 128:(ch + 1) * 128], in_=uh[:, ch * 128:(ch + 1) * 128], identity=idb[:]),
                         r=["uh", "idb"], w=["PS2"])
                R.op("act", lambda e, t=t: e.copy(out=uT[:, :, t * 128:(t + 1) * 128], in_=pXh.rearrange("p (c q) -> p c q", q=128)), r=["PS2"], w=["uT"])
                for ch in range(8):
                    R.op("pe", lambda e, ch=ch: e.transpose(out=pXl[:, ch * 128:(ch + 1) * 128], in_=ul[:, ch * 128:(ch + 1) * 128], identity=idb[:]),
                         r=["ul", "idb"], w=["PS3"])
                R.op("dve", lambda e: e.tensor_copy(out=ulT[:], in_=pXl), r=["PS3"], w=["ulT"])
                if cut < 3:
                    continue
                for ch in range(8):
                    uhs = uT[:, ch, t * 128:(t + 1) * 128]
                    R.op("pe", lambda e, ch=ch, uhs=uhs: e.matmul(PS[4][:, 0:16], lhsT=uhs, rhs=rwh[:, ch, :], start=(ch == 0), stop=False), r=["uT", "rwh"], w=["PS4"])
                    R.op("pe", lambda e, ch=ch, uhs=uhs: e.matmul(PS[4][:, 0:16], lhsT=uhs, rhs=rwl[:, ch, :], start=False, stop=False), r=["uT", "rwl"], w=["PS4"])
                    R.op("pe", lambda e, ch=ch: e.matmul(PS[4][:, 0:16], lhsT=ulT[:, ch * 128:(ch + 1) * 128], rhs=rwh[:, ch, :], start=False, stop=(ch == 7)),
                         r=["ulT", "rwh"], w=["PS4"])
                v4 = lambda a: a[:].rearrange("p (g k) -> p g k", k=4)
                R.op("act", lambda e: e.activation(out=aff[:], in_=PS[4][:, 0:16], func=AF.Sigmoid), r=["PS4"], w=["aff"])
                if cut < 4:
                    continue
                R.op("dve", lambda e: e.tensor_tensor(out=sel[:], in0=aff[:], in1=rbb[:], op=ALU.add), r=["aff", "rbb"], w=["sel"])
                R.op("dve", lambda e: e.tensor_reduce(out=m1[:], in_=v4(sel), axis=AX.X, op=ALU.max), r=["sel"], w=["m1"])
                R.op("dve", lambda e: e.tensor_tensor(out=v4(eq), in0=v4(sel), in1=m1[:].unsqueeze(2).broadcast_to([128, 4, 4]), op=ALU.is_equal), r=["sel", "m1"], w=["eq"])
                R.op("dve", lambda e: e.scalar_tensor_tensor(out=s2[:], in0=eq[:], scalar=-BIGR, in1=sel[:], op0=ALU.mult, op1=ALU.add), r=["eq", "sel"], w=["s2"])
                R.op("dve", lambda e: e.tensor_reduce(out=m2[:], in_=v4(s2), axis=AX.X, op=ALU.max), r=["s2"], w=["m2"])
                R.op("dve", lambda e: e.tensor_tensor(out=gsum[:], in0=m1[:], in1=m2[:], op=ALU.add), r=["m1", "m2"], w=["gsum"])
                R.op("dve", lambda e: e.tensor_reduce(out=gmax[:], in_=gsum[:], axis=AX.X, op=ALU.max), r=["gsum"], w=["gmax"])
                R.op("dve", lambda e: e.tensor_scalar(out=pen[:], in0=gsum[:], scalar1=gmax[:, 0:1], scalar2=-BIGR, op0=ALU.is_lt, op1=ALU.mult), r=["gsum", "gmax"], w=["pen"])
                R.op("dve", lambda e: e.tensor_tensor(out=v4(selm), in0=v4(sel), in1=pen[:].unsqueeze(2).broadcast_to([128, 4, 4]), op=ALU.add), r=["sel", "pen"], w=["selm"])
                R.op("dve", lambda e: e.tensor_reduce(out=t1[:], in_=selm[:], axis=AX.X, op=ALU.max), r=["selm"], w=["t1"])
                R.op("dve", lambda e: e.tensor_scalar(out=e1[:], in0=selm[:], scalar1=t1[:, 0:1], scalar2=None, op0=ALU.is_equal), r=["selm", "t1"], w=["e1"])
                R.op("dve", lambda e: e.scalar_tensor_tensor(out=s2[:], in0=e1[:], scalar=-BIGR, in1=selm[:], op0=ALU.mult, op1=ALU.add), r=["e1", "selm"], w=["s2"])
                R.op("dve", lambda e: e.tensor_reduce(out=t2[:], in_=s2[:], axis=AX.X, op=ALU.max), r=["s2"], w=["t2"])
                R.op("dve", lambda e: e.tensor_scalar(out=e2[:], in0=s2[:], scalar1=t2[:, 0:1], scalar2=None, op0=ALU.is_equal), r=["s2", "t2"], w=["e2"])
                R.op("dve", lambda e: e.tensor_tensor(out=e1[:], in0=e1[:], in1=e2[:], op=ALU.add), r=["e1", "e2"], w=["e1"])
                R.op("dve", lambda e: e.tensor_tensor(out=e2[:], in0=e1[:], in1=aff[:], op=ALU.mult), r=["e1", "aff"], w=["e2"])
                R.op("dve", lambda e: e.tensor_reduce(out=den[:], in_=e2[:], axis=AX.X, op=ALU.add), r=["e2"], w=["den"])
                R.op("dve", lambda e: e.reciprocal(out=rden[:], in_=den[:]), r=["den"], w=["rden"])
                R.op("dve", lambda e, t=t: e.tensor_scalar(out=comb[:, t, :], in0=e2[:], scalar1=rden[:, 0:1], scalar2=None, op0=ALU.mult), r=["e2", "rden"], w=["comb"])
            R.final_wait = list(R.dma_count.keys())
            if "1" in phases:
                R.emit()
        nc.all_engine_barrier()

        R = Rec(nc)
        with ExitStack() as s1:
            sb = lambda n, s, d=F32: s1.enter_context(nc.sbuf_tensor(pfx + n, s, d))
            w1s = [sb("w1s%d" % i, [128, 8, FF], BF16) for i in range(2)]
            w3s = [sb("w3s%d" % i, [128, 8, FF], BF16) for i in range(2)]
            w2s = [sb("w2s%d" % i, [128, 4, D], BF16) for i in range(2)]
            sl = [sb("sl%d" % i, [128, 512]) for i in range(2)]
            hT = [sb("hT%d" % i, [128, 4, 512], BF16) for i in range(2)]
            kc = [0]

            def m_h(e_, tg, hb):
                wb = e_ % 2
                if tg == 0:
                    R.dma("pool", w1s[wb][:], w1.ap()[e_].rearrange("(ch p) f -> p ch f", p=128), "w1_%d" % wb, w=["w1s%d" % wb])
                    R.dma("pool", w3s[wb][:], w3.ap()[e_].rearrange("(ch p) f -> p ch f", p=128), "w1_%d" % wb, w=["w3s%d" % wb])
                    R.dma("pool", w2s[wb][:], w2.ap()[e_].rearrange("(ch p) n -> p ch n", p=128), "w1_%d" % wb, w=["w2s%d" % wb])
                ntl = min(4, nt - tg * 4)
                ncol = ntl * 128
                tsl = slice(tg * 512, tg * 512 + ncol)
                for fc in range(4):
                    pb = (kc[0] % 2) * 2
                    kc[0] += 1
                    for ch in range(8):
                        R.op("pe", lambda e, ch=ch, fc=fc, pb=pb: e.matmul(PS[pb][:, 0:ncol], lhsT=w1s[wb][:, ch, fc * 128:(fc + 1) * 128], rhs=uT[:, ch, tsl],
                                                                           start=(ch == 0), stop=(ch == 7)), r=["w1s%d" % wb], w=["PS%d" % pb])
                    for ch in range(8):
                        R.op("pe", lambda e, ch=ch, fc=fc, pb=pb: e.matmul(PS[pb + 1][:, 0:ncol], lhsT=w3s[wb][:, ch, fc * 128:(fc + 1) * 128], rhs=uT[:, ch, tsl],
                                                                           start=(ch == 0), stop=(ch == 7)), r=["w3s%d" % wb], w=["PS%d" % (pb + 1)])
                    sb_ = kc[0] % 2
                    R.op("act", lambda e, pb=pb, sb_=sb_: e.activation(out=sl[sb_][:, 0:ncol], in_=PS[pb][:, 0:ncol], func=AF.Silu), r=["PS%d" % pb], w=["sl%d" % sb_])
                    R.op("dve", lambda e, pb=pb, sb_=sb_, fc=fc: e.tensor_tensor(out=hT[hb][:, fc, 0:ncol], in0=PS[pb + 1][:, 0:ncol], in1=sl[sb_][:, 0:ncol], op=ALU.mult),
                         r=["PS%d" % (pb + 1), "sl%d" % sb_], w=["hT%d" % hb])

            def m_w(e_, tg, hb):
                wb = e_ % 2
                ntl = min(4, nt - tg * 4)
                for ti in range(ntl):
                    t = tg * 4 + ti
                    for hf in range(2):
                        ob = 4 + ((t * 2 + hf) % 4)
                        for fc in range(4):
                            R.op("pe", lambda e, fc=fc, ti=ti, hf=hf, ob=ob: e.matmul(PS[ob][:], lhsT=hT[hb][:, fc, ti * 128:(ti + 1) * 128], rhs=w2s[wb][:, fc, hf * 512:(hf + 1) * 512],
                                                                                   start=(fc == 0), stop=(fc == 3)), r=["hT%d" % hb, "w2s%d" % wb], w=["PS%d" % ob])
                        ysl = yacc[:, t, hf * 512:(hf + 1) * 512]
                        if e_ == 0:
                            R.op("dve", lambda e, ob=ob, ysl=ysl, t=t: e.tensor_scalar(out=ysl, in0=PS[ob][:], scalar1=comb[:, t, e_:e_ + 1], scalar2=None, op0=ALU.mult),
                                 r=["PS%d" % ob], w=["y%d_%d" % (t, hf)])
                        else:
                            R.op("dve", lambda e, ob=ob, ysl=ysl, t=t: e.scalar_tensor_tensor(out=ysl, in0=PS[ob][:], scalar=comb[:, t, e_:e_ + 1], in1=ysl, op0=ALU.mult, op1=ALU.add),
                                 r=["PS%d" % ob, "y%d_%d" % (t, hf)], w=["y%d_%d" % (t, hf)])

            mjobs = [(e_, tg) for e_ in range(ne) for tg in range(TG)]
            m_h(*mjobs[0], 0)
            for ji, job in enumerate(mjobs):
                if ji + 1 < len(mjobs):
                    m_h(*mjobs[ji + 1], (ji + 1) % 2)
                m_w(*job, ji % 2)
            R.final_wait = list(R.dma_count.keys())
            if "2" in phases:
                R.emit()
        nc.all_engine_barrier()

        R = Rec(nc)
        with ExitStack() as s2:
            sb = lambda n, s, d=F32: s2.enter_context(nc.sbuf_tensor(pfx + n, s, d))
            xt = [sb("f_xt%d" % i, [128, D]) for i in range(2)]
            xn = [sb("f_xn%d" % i, [128, D]) for i in range(2)]
            tmp = sb("f_tmp", [128, D]); junk = sb("f_junk", [128, D], BF16); ss = sb("f_ss", [128, 1]); rt = sb("f_rt", [128, 1]); rstd = sb("f_rstd", [128, 1])
            for t in range(nt):
                b = t % 2
                R.dma("sp", xt[b][:], x.ap()[t * 128:(t + 1) * 128, :], "x%d" % b, w=["xt%d" % b])
                R.op("dve", lambda e, t=t: e.tensor_tensor(out=tmp[:], in0=yacc[:, t, :], in1=modbc[:, 2 * D:3 * D], op=ALU.mult), r=[], w=["tmp"])
                R.op("dve", lambda e, b=b: e.tensor_tensor(out=xn[b][:], in0=tmp[:], in1=xt[b][:], op=ALU.add), r=["tmp", "xt%d" % b], w=["xn%d" % b])
                if final:
                    R.op("act", lambda e, b=b: e.activation(out=junk[:], in_=xn[b][:], func=AF.Square, accum_out=ss[:]), r=["xn%d" % b], w=["junk", "ss"])
                    R.op("act", lambda e: e.activation(out=rt[:], in_=ss[:], func=AF.Sqrt, scale=1.0 / D, bias=cst[:, 0:1]), r=["ss"], w=["rt"])
                    R.op("dve", lambda e: e.reciprocal(out=rstd[:], in_=rt[:]), r=["rt"], w=["rstd"])
                    R.op("dve", lambda e, b=b: e.scalar_tensor_tensor(out=xn[b][:], in0=xn[b][:], scalar=rstd[:, 0:1], in1=fgb[:], op0=ALU.mult, op1=ALU.mult),
                         r=["xn%d" % b, "rstd"], w=["xn%d" % b])
                R.dma("sp", xo.ap()[t * 128:(t + 1) * 128, :], xn[b][:], "o%d" % b, r=["xn%d" % b])
            R.final_wait = list(R.dma_count.keys())
            if "3" in phases:
                R.emit()


class H:
    def __init__(self, ap):
        self._ap = ap

    def ap(self):
        return self._ap


RBW = 2688
RG = [[0, 1, 2, 3], [4, 5, 6, 7]]


def build_fused(nt=16, stop=None):
    tok = nt * 128
    nc = bass.Bass("TRN2", target_bir_lowering=False)
    di = lambda n, s, d=F32: nc.dram_tensor(n, s, d, kind="ExternalInput")
    dn = lambda n, s, d=BF16: nc.dram_tensor(n, s, d)
    E = {}
    E["x"] = di("x", [tok, D]); E["pos"] = di("pos", [128, nt], I32); E["c"] = di("c", [128, 8])
    E["zmT"] = di("zmT", [128, 512], BF16); E["zq"] = di("zq", [128, 512]); E["cmaskT"] = di("cmaskT", [128, 1024])
    E["rbcore"] = di("rbcore", [2, 8, RBW])
    E["w_mod"] = di("w_mod", [2, D, 6144]); E["b_mod"] = di("b_mod", [2, 1, 6144])
    E["norm1_g"] = di("norm1_g", [2, 1, D]); E["norm2_g"] = di("norm2_g", [2, 1, D]); E["w_in"] = di("w_in", [2, D, INC])
    E["lam4"] = di("lam4", [2, 1, 256]); E["a_norm_g"] = di("a_norm_g", [2, 1, 128]); E["laminit"] = di("laminit", [2, 1, 2])
    E["wa"] = di("wa", [2, 512, D]); E["wb"] = di("wb", [2, 512, D]); E["wc"] = di("wc", [2, 512, D]); E["w_out"] = di("w_out", [2, D, D])
    E["router_w"] = di("router_w", [D, 16]); E["router_b"] = di("router_b", [1, 16])
    E["w1"] = di("w1", [2, NE, D, FF]); E["w3"] = di("w3", [2, NE, D, FF]); E["w2"] = di("w2", [2, NE, FF, D])
    E["final_g"] = di("final_g", [1, D]); E["ident"] = di("ident", [128, 128]); E["aident"] = di("aident", [128, 128])
    E["ropeinv"] = di("ropeinv", [1, 32]); E["p2tab"] = di("p2tab", [1, NIT])
    out = nc.dram_tensor("out", [tok, D], F32, kind="ExternalOutput")
    xin = E["x"]
    for layer in range(2):
        L = "L%d_" % layer
        aqt = dn(L + "aqt", [4, 64, 2 * tok]); bq_iq = dn(L + "bq_iq", [nt, 64, 1536]); iw = dn(L + "iw", [tok, 4], F32)
        cqt = dn(L + "cqt", [64, 8 * tok]); gates = dn(L + "gates", [tok, 3072], F32)
        akt_l = dn(L + "akt_l", [256, 2 * tok]); av_l = dn(L + "av_l", [tok, 516]); bkik_l = dn(L + "bkik_l", [64, 2 * tok])
        bv_l = dn(L + "bv_l", [tok, 65]); ck_l = dn(L + "ck_l", [64, 8 * tok]); cv_l = dn(L + "cv_l", [tok, 520])
        SL = min(4, nt); NCH = nt // SL
        akt_g = [dn(L + "akt_g%d" % i, [4 * 128, 2 * tok]) for i in range(2)]
        av_g = [dn(L + "av_g%d" % i, [4 * SL * 128, 516]) for i in range(NCH)]
        bkik_g = dn(L + "bkik_g", [4 * 64, 2 * tok]); bv_g = dn(L + "bv_g", [4 * tok, 65])
        ck_g = [dn(L + "ck_g%d" % i, [4 * 32, 8 * tok]) for i in range(2)]
        cv_g = [dn(L + "cv_g%d" % i, [4 * SL * 128, 520]) for i in range(NCH)]
        xmid = dn(L + "xmid", [tok, D], F32); xnext = dn(L + "xnext", [tok, D], F32) if layer == 0 else out
        wmod = H(E["w_mod"].ap()[layer]); bmod = H(E["b_mod"].ap()[layer])
        TP = {"x": xin, "pos": E["pos"], "c": E["c"], "w_mod": wmod, "b_mod": bmod, "norm_g": H(E["norm1_g"].ap()[layer]),
              "w_in": H(E["w_in"].ap()[layer]), "ident": E["ident"], "ropeinv": E["ropeinv"],
              "aqt": aqt, "akt": akt_l, "av": av_l, "bqt": bq_iq, "bkt": bkik_l, "bv": bv_l, "iw": iw,
              "cqt": cqt, "ckt": ck_l, "cv": cv_l, "gates": gates}
        emit_P(nc, TP, L + "P_", nt)
        nc.all_engine_barrier()
        if stop == "P":
            return nc
        pairs = [(bkik_l.ap(), bkik_g.ap()), (bv_l.ap(), bv_g.ap())]
        for i in range(2):
            pairs.append((akt_l.ap()[i * 128:(i + 1) * 128, :], akt_g[i].ap()))
            pairs.append((ck_l.ap()[i * 32:(i + 1) * 32, :], ck_g[i].ap()))
        for i in range(NCH):
            pairs.append((av_l.ap()[i * SL * 128:(i + 1) * SL * 128, :], av_g[i].ap()))
            pairs.append((cv_l.ap()[i * SL * 128:(i + 1) * SL * 128, :], cv_g[i].ap()))
        ccs = nc.alloc_semaphore(name=L + "ccs")
        with nc.Block() as blk:
            @blk.gpsimd
            def _(g):
                for (lo_, ga_) in pairs:
                    g.collective_compute("AllGather", mybir.AluOpType.bypass, replica_groups=RG,
                                         ins=[lo_], outs=[ga_]).then_inc(ccs, 1)
                g.wait_ge(ccs, len(pairs))
        nc.all_engine_barrier()
        nc.clear_and_free_semaphores([ccs])
        nc.all_engine_barrier()
        if stop == "AG":
            return nc
        TT = {"aqt": aqt, "bq_iq": bq_iq, "iw": iw, "cqt": cqt, "gates": gates, "x": xin,
              "akt": akt_g, "av": av_g, "bkik": bkik_g, "bv": bv_g, "ckb": ck_g, "cvb": cv_g,
              "zmT": E["zmT"], "zq": E["zq"], "cmaskT": E["cmaskT"],
              "wa": H(E["wa"].ap()[layer]), "wb": H(E["wb"].ap()[layer]), "wc": H(E["wc"].ap()[layer]), "w_out": H(E["w_out"].ap()[layer]),
              "w_mod": wmod, "b_mod": bmod, "c": E["c"], "lam4": H(E["lam4"].ap()[layer]), "a_norm_g": H(E["a_norm_g"].ap()[layer]),
              "rbext": E["rbcore"], "rb_off": layer * 8 * RBW, "rb_w": RBW, "ident": E["ident"], "aident": E["aident"],
              "laminit": H(E["laminit"].ap()[layer]), "p2tab": E["p2tab"], "xo": xmid}
        emit_T(nc, TT, L + "T_", nt, phases=(stop[1:] if (stop or "").startswith("T") else "0ABCM"))
        nc.all_engine_barrier()
        if (stop or "").startswith("T"):
            return nc
        TM = {"x": xmid, "c": E["c"], "w_mod": wmod, "b_mod": bmod, "norm_g": H(E["norm2_g"].ap()[layer]),
              "router_w": E["router_w"], "router_b": E["router_b"], "w1": H(E["w1"].ap()[layer]), "w3": H(E["w3"].ap()[layer]),
              "w2": H(E["w2"].ap()[layer]), "ident": E["ident"], "final_g": E["final_g"], "xo": xnext}
        emit_M(nc, TM, L + "M_", nt, final=(layer == 1))
        nc.all_engine_barrier()
        xin = xnext
    return nc


BF = ml_dtypes.bfloat16
_CACHE = {}


def _core_rows(r, nt=16):
    return np.concatenate([np.arange((4 * t + r) * 128, (4 * t + r + 1) * 128) for t in range(nt)])


def _masks(r):
    k = np.arange(128)[:, None]
    q = np.arange(128)[None, :]
    zmT = np.zeros((128, 4, 128), np.float32)
    zq = np.zeros((128, 4, 128), np.float32)
    for j in range(4):
        if j == r:
            m = (k >= 64) & (q < 64)
        elif j > r:
            m = np.ones((128, 128), bool)
        else:
            m = np.zeros((128, 128), bool)
        zmT[:, j, :] = np.where(m, -BIG, 0.0)
        zq[:, j, :] = np.where(m.T, -1e30, 0.0)
    cm = np.full((128, 8, 128), -BIG, np.float32)
    for i in range(8):
        j = i - r
        if 1 <= j <= 3:
            cm[:, i, :] = 0.0
        elif j == 0:
            cm[:, i, :] = np.where((k < 64) & (q >= 64), -BIG, 0.0)
        elif j == 4:
            cm[:, i, :] = np.where((k >= 64) & (q < 64), -BIG, 0.0)
    return zmT.reshape(128, 512).astype(BF), zq.reshape(128, 512), np.ascontiguousarray(cm[::-1]).reshape(128, 1024)


def kernel(x, c, positions, norm1_g, norm2_g, w_mod, b_mod, w_in, lambda_q1, lambda_k1, lambda_q2, lambda_k2,
           a_norm_g, c_rel_bias, w_branch_a, w_branch_b, w_branch_c, w_out, router_w, router_b,
           exp_w1, exp_w3, exp_w2, final_g, _nt=16, _runner=None, _stop=None):
    f32 = np.float32
    nt = _nt
    A = lambda a: np.ascontiguousarray(np.asarray(a, f32))
    x = A(x); c = A(c); positions = np.asarray(positions, np.int32)
    cores = [(b, r) for b in range(2) for r in range(4)]
    rows = [_core_rows(r, nt) for r in range(4)]
    ident = np.eye(128, dtype=f32)
    lam_init = [0.8 - 0.6 * math.exp(-0.3 * l) for l in range(2)]
    rb = A(c_rel_bias)
    rbext = np.concatenate([rb, np.repeat(rb[:, :, 512:513], 511, axis=2)], axis=2)
    rbbig = np.zeros((2, 8, 3072), f32); rbbig[:, :, 1024:2048] = rbext
    shared = {
        "w_mod": A(w_mod), "b_mod": A(b_mod)[:, None, :], "norm1_g": A(norm1_g)[:, None, :], "norm2_g": A(norm2_g)[:, None, :],
        "w_in": A(w_in), "lam4": np.concatenate([A(lambda_q1), A(lambda_k1), A(lambda_q2), A(lambda_k2)], axis=1)[:, None, :],
        "a_norm_g": A(a_norm_g)[:, None, :], "laminit": np.array([[[l, 1.0 - l]] for l in lam_init], f32),
        "wa": A(w_branch_a), "wb": A(w_branch_b), "wc": A(w_branch_c), "w_out": A(w_out),
        "router_w": A(router_w), "router_b": A(router_b)[None, :], "w1": A(exp_w1), "w3": A(exp_w3), "w2": A(exp_w2),
        "final_g": A(final_g)[None, :], "ident": ident, "aident": np.ascontiguousarray(ident[::-1]),
        "ropeinv": (np.float32(10000.0) ** (-np.arange(32, dtype=f32) / np.float32(32))).astype(f32)[None, :],
        "p2tab": (2.0 ** -(np.arange(NIT) + 1.0))[None, :].astype(f32),
    }
    shared = {k_: np.ascontiguousarray(v) for k_, v in shared.items()}
    ims = []
    for (b, r) in cores:
        zmT, zq, cm = _masks(r)
        im = dict(shared)
        im.update({"x": np.ascontiguousarray(x[b][rows[r]]), "pos": np.ascontiguousarray(positions[b][rows[r]].reshape(nt, 128).T),
                   "c": np.ascontiguousarray(c[b].reshape(8, 128).T), "zmT": zmT, "zq": np.ascontiguousarray(zq), "cmaskT": cm,
                   "rbcore": np.ascontiguousarray(rbbig[:, :, 128 * r:128 * r + RBW])})
        ims.append(im)
    if ("F", nt) not in _CACHE:
        _CACHE[("F", nt)] = build_fused(nt, stop=_stop)
    if _runner is None:
        results = run_bass_kernel_spmd(_CACHE[("F", nt)], ims, core_ids=list(range(8))).results
    else:
        results = _runner(_CACHE[("F", nt)], ims)
    out = np.zeros((2, 512 * nt, 1024), f32)
    for ci, (b, r) in enumerate(cores):
        out[b][rows[r]] = np.asarray(results[ci]["out"], f32)
    return out
```

```python
import math
from contextlib import ExitStack
import numpy as np
import ml_dtypes
import concourse.bass as bass
import concourse.mybir as mybir
from concourse.bass_utils import run_bass_kernel_spmd

F32 = mybir.dt.float32
BF16 = mybir.dt.bfloat16
I32 = mybir.dt.int32
ALU = mybir.AluOpType
AF = mybir.ActivationFunctionType
AX = mybir.AxisListType

ENGS = ("pe", "act", "dve", "pool", "sp")


class Op:
    __slots__ = ("eng", "fn", "deps", "is_dma", "sem", "ticket", "needs_inc", "idx")

    def __init__(self, eng, fn, is_dma, sem):
        self.eng = eng
        self.fn = fn
        self.deps = []
        self.is_dma = is_dma
        self.sem = sem
        self.ticket = None
        self.needs_inc = False


class Rec:
    def __init__(self, nc):
        self.nc = nc
        self.streams = {e: [] for e in ENGS}
        self.last_w = {}
        self.readers = {}
        self.dma_count = {}
        self.all_ops = []

    def _add(self, op, r, w):
        deps = []
        for k in r:
            lw = self.last_w.get(k)
            if lw is not None:
                deps.append(lw)
        for k in w:
            lw = self.last_w.get(k)
            if lw is not None:
                deps.append(lw)
            deps.extend(self.readers.get(k, ()))
        seen = set()
        for d in deps:
            if d is op or id(d) in seen:
                continue
            seen.add(id(d))
            if d.eng == "pe" and op.eng == "pe" and not d.is_dma and not op.is_dma:
                continue
            op.deps.append(d)
            d.needs_inc = True
        for k in w:
            self.last_w[k] = op
            self.readers[k] = []
        for k in r:
            self.readers.setdefault(k, []).append(op)
        self.streams[op.eng].append(op)
        self.all_ops.append(op)
        return op

    def op(self, eng, fn, r=(), w=()):
        return self._add(Op(eng, fn, False, None), r, w)

    def dma(self, eng, out, in_, sem, r=(), w=()):
        o = Op(eng, lambda e: e.dma_start(out=out, in_=in_), True, sem)
        self.dma_count[sem] = self.dma_count.get(sem, 0) + 1
        o.ticket = self.dma_count[sem]
        return self._add(o, r, w)

    def emit(self):
        nc = self.nc
        cnt = {e: 0 for e in ENGS}
        for e in ENGS:
            for o in self.streams[e]:
                if o.is_dma:
                    continue
                if o.needs_inc:
                    cnt[e] += 1
                    o.ticket = cnt[e]
        order = {id(o): i for i, o in enumerate(self.all_ops)}
        dma_hist = {}
        for i, o in enumerate(self.all_ops):
            if o.is_dma:
                dma_hist.setdefault(o.sem, []).append((i, o.ticket))
        import bisect
        dma_keys = sorted(self.dma_count.keys(), key=str)
        from contextlib import ExitStack
        esem = {e: nc.alloc_semaphore(name=nc.make_name("s_" + e, add_next_id=True)) for e in ENGS}
        dsem = {k: nc.alloc_semaphore(name=nc.make_name("d_%d" % i, add_next_id=True)) for i, k in enumerate(dma_keys)}
        with ExitStack() as st:
            block = st.enter_context(nc.Block())

            def run_stream(ename):
                def body(e):
                    waited = {}
                    for o in self.streams[ename]:
                        me = order[id(o)]
                        for d in o.deps:
                            if d.is_dma:
                                hist = dma_hist[d.sem]
                                j = bisect.bisect_left(hist, (me, 0)) - 1
                                val = 16 * hist[j][1]
                                sem = dsem[d.sem]
                                key = ("d", d.sem)
                            else:
                                val = d.ticket
                                sem = esem[d.eng]
                                key = ("e", d.eng)
                            if waited.get(key, 0) >= val:
                                continue
                            waited[key] = val
                            e.wait_ge(sem, val)
                        ins = o.fn(e)
                        if o.is_dma:
                            ins.then_inc(dsem[o.sem], 16)
                        elif o.needs_inc:
                            ins.then_inc(esem[ename], 1)
                    if ename == "sp":
                        for k in getattr(self, "final_wait", ()):
                            e.wait_ge(dsem[k], 16 * self.dma_count[k])
                return body

            block.tensor(run_stream("pe"))
            block.scalar(run_stream("act"))
            block.vector(run_stream("dve"))
            block.gpsimd(run_stream("pool"))
            block.sync(run_stream("sp"))
        nc.all_engine_barrier()
        nc.clear_and_free_semaphores(list(esem.values()) + list(dsem.values()))
        nc.all_engine_barrier()


def dram_ap(t, offset, pattern):
    return bass.AP(t, offset, pattern)


D = 1024
NT = 16
TOK = NT * 128
INC = 7108
C_AQ, C_AK, C_AV, C_BQ, C_BK, C_BV, C_IQ, C_IK, C_IW, C_CQ, C_CK, C_CV, C_G = (
    0, 512, 1024, 1536, 2048, 2112, 2176, 2432, 2496, 2500, 3012, 3524, 4036)
EPS = 1e-6
TWO_PI = 2.0 * math.pi


def emit_P(nc, T, pfx, nt=NT):
    tok = nt * 128
    di = lambda n, s, d=F32: T[n]
    do = lambda n, s, d=BF16: T[n]
    x = di("x", [tok, D]); pos = di("pos", [128, nt], I32); cvec = di("c", [128, 8])
    w_mod = di("w_mod", [D, 6144]); b_mod = di("b_mod", [1, 6144]); norm_g = di("norm_g", [1, D])
    w_in = di("w_in", [D, INC]); ident = di("ident", [128, 128]); ropeinv = di("ropeinv", [1, 32])
    aqt = do("aqt", [4, 128, tok]); akt = do("akt", [4, 128, tok]); av = do("av", [tok, 516])
    bqt = do("bqt", [nt, 64, 1024]); bkt = do("bkt", [64, tok]); bv = do("bv", [tok, 65])
    iw = do("iw", [tok, 4], F32)
    cqt = do("cqt", [4, 128, tok]); ckt = do("ckt", [4, 128, tok]); cv = do("cv", [tok, 520])
    gates = do("gates", [tok, 3072], F32)

    R = Rec(nc)
    with ExitStack() as st, nc.allow_low_precision("bf16 matmul operands, fp32 accumulation"):
        sb = lambda n, s, d=F32: st.enter_context(nc.sbuf_tensor(pfx + n, s, d))
        ps = lambda n, s, d=F32: st.enter_context(nc.psum_tensor(pfx + n, s, d))
        cs = sb("cs", [128, 8]); ca = sb("ca", [128, 8]); CA = sb("CA", [128, 8, 128])
        modbc = sb("modbc", [128, 2048]); gs = sb("gs", [128, D])
        wsb = sb("wsb", [128, 8, INC], BF16)
        idf = sb("idf", [128, 128]); idb = sb("idb", [128, 128], BF16)
        inv = sb("inv", [128, 32]); posi = sb("posi", [128, nt], I32); posf = sb("posf", [128, nt])
        xt = [sb("xt%d" % i, [128, D]) for i in range(2)]
        junk = sb("junk", [128, D], BF16); ss = sb("ss", [128, 1]); rstd = sb("rstd", [128, 1]); rt = sb("rt", [128, 1])
        tmp = sb("tmp", [128, D]); ub = sb("ub", [128, D], BF16); uT = sb("uT", [128, D], BF16)
        pj = sb("pj", [128, INC])
        ang = sb("ang", [128, nt, 32]); ang2 = sb("ang2", [128, nt, 32]); kf = sb("kf", [128, nt, 32]); ki = sb("ki", [128, nt, 32], I32)
        SN = sb("SN", [128, nt, 32]); CN = sb("CN", [128, nt, 32])
        t1 = sb("t1", [128, 512]); t2 = sb("t2", [128, 512])
        rb = sb("rb", [128, C_CV], BF16)
        avb = sb("avb", [128, 4, 129], BF16); bvb = sb("bvb", [128, 65], BF16); cvb = sb("cvb", [128, 8, 65], BF16)
        iwb = sb("iwb", [128, 4]); cst = sb("cst", [128, 2])
        wm = [pj[:, b * 2048:(b + 1) * 2048].rearrange("p (ch n) -> p ch n", n=256) for b in range(2)]
        WMK = [["pj%d" % i for i in range(4)], ["pj%d" % i for i in range(4, 8)]]
        bmb = pj[:, 4096:6144]; BMK = ["pj%d" % i for i in range(8, 12)]
        gbc = tmp
        tA = [sb("tA%d" % i, [128, 1024], BF16) for i in range(2)]
        pT = ps("pT", [128, 1024], BF16)
        pp = [ps("pp%d" % i, [128, 512]) for i in range(2)]
        pX = [ps("pX%d" % i, [128, 1024], BF16) for i in range(2)]

        R.dma("sp", cs[:], cvec.ap(), "c", w=["cs"])
        R.op("act", lambda e: e.activation(out=ca[:], in_=cs[:], func=AF.Silu), r=["cs"], w=["ca"])
        R.op("dve", lambda e: e.tensor_copy(out=CA[:], in_=ca[:].unsqueeze(2).broadcast_to([128, 8, 128])), r=["ca"], w=["CA"])
        R.dma("sp", bmb, b_mod.ap()[0:1, 0:2048].partition_broadcast(128), "c", w=BMK)
        R.dma("sp", gbc[:], norm_g.ap()[0:1, :].partition_broadcast(128), "c", w=["tmp"])
        R.dma("sp", idf[:], ident.ap(), "c", w=["idf"])
        R.dma("sp", inv[:], ropeinv.ap()[0:1, :].partition_broadcast(128), "c", w=["inv"])
        R.dma("sp", posi[:], pos.ap(), "c", w=["posi"])
        R.op("dve", lambda e: e.tensor_copy(out=idb[:], in_=idf[:]), r=["idf"], w=["idb"])
        R.op("dve", lambda e: e.tensor_copy(out=posf[:], in_=posi[:]), r=["posi"], w=["posf"])
        R.op("pool", lambda e: e.memset(cst[:, 0:1], EPS), w=["cst"])
        R.op("pool", lambda e: e.memset(cst[:, 1:2], math.pi), w=["cst"])
        R.op("pool", lambda e: e.memset(avb[:], 1.0), w=["avb"])
        R.op("pool", lambda e: e.memset(bvb[:], 1.0), w=["bvb"])
        R.op("pool", lambda e: e.memset(cvb[:], 1.0), w=["cvb"])
        R.op("dve", lambda e: e.tensor_tensor(out=ang[:], in0=inv[:].unsqueeze(1).broadcast_to([128, nt, 32]),
                                              in1=posf[:].unsqueeze(2).broadcast_to([128, nt, 32]), op=ALU.mult), r=["inv", "posf"], w=["ang"])
        R.op("dve", lambda e: e.tensor_scalar(out=ang2[:], in0=ang[:], scalar1=math.pi / 2, scalar2=None, op0=ALU.add), r=["ang"], w=["ang2"])
        for (src, dst, nm) in ((ang, SN, "SN"), (ang2, CN, "CN")):
            sk = "ang" if src is ang else "ang2"
            R.op("dve", lambda e, src=src: e.tensor_scalar(out=ki[:], in0=src[:], scalar1=1.0 / TWO_PI, scalar2=None, op0=ALU.mult), r=[sk], w=["ki"])
            R.op("dve", lambda e: e.tensor_copy(out=kf[:], in_=ki[:]), r=["ki"], w=["kf"])
            R.op("dve", lambda e, src=src: e.scalar_tensor_tensor(out=kf[:], in0=kf[:], scalar=-TWO_PI, in1=src[:], op0=ALU.mult, op1=ALU.add),
                 r=["kf", sk], w=["kf"])
            R.op("dve", lambda e: e.tensor_scalar(out=kf[:], in0=kf[:], scalar1=3.14159, scalar2=-3.14159, op0=ALU.min, op1=ALU.max), r=["kf"], w=["kf"])
            R.op("act", lambda e, dst=dst: e.activation(out=dst[:], in_=kf[:], func=AF.Sin), r=["kf"], w=[nm])
        wmv = w_mod.ap().rearrange("(ch p) n -> p ch n", p=128)
        for j in range(8):
            b = j % 2
            R.dma("sp", wm[b], wmv[:, :, j * 256:(j + 1) * 256], "wm%d" % b, w=WMK[b])
            for ch in range(8):
                R.op("pe", lambda e, ch=ch, b=b: e.matmul(pp[b][:, 0:256], lhsT=CA[:, ch, :], rhs=wm[b][:, ch, :],
                                                          start=(ch == 0), stop=(ch == 7)),
                     r=["CA"] + WMK[b], w=["pp%d" % b])
            R.op("dve", lambda e, j=j, b=b: e.tensor_tensor(out=modbc[:, j * 256:(j + 1) * 256], in0=pp[b][:, 0:256],
                                                            in1=bmb[:, j * 256:(j + 1) * 256], op=ALU.add),
                 r=["pp%d" % b] + BMK, w=["modbc"])
        R.op("dve", lambda e: e.scalar_tensor_tensor(out=gs[:], in0=modbc[:, 1024:2048], scalar=1.0, in1=gbc[:],
                                                     op0=ALU.add, op1=ALU.mult), r=["modbc", "tmp"], w=["gs"])
        wiv = w_in.ap().rearrange("(ch p) n -> p ch n", p=128)
        for ch in range(8):
            R.dma("pool", wsb[:, ch, :], wiv[:, ch, :], "wsb", w=["wsb%d" % ch])
        WS = ["wsb%d" % ch for ch in range(8)]

        for t in range(nt):
            xb = t % 2
            xk = "xt%d" % xb
            R.dma("sp", xt[xb][:], x.ap()[t * 128:(t + 1) * 128, :], xk, w=[xk])
            R.op("act", lambda e, xb=xb: e.activation(out=junk[:], in_=xt[xb][:], func=AF.Square, accum_out=ss[:]),
                 r=[xk], w=["junk", "ss"])
            R.op("act", lambda e: e.activation(out=rt[:], in_=ss[:], func=AF.Sqrt, scale=1.0 / D, bias=cst[:, 0:1]),
                 r=["ss", "cst"], w=["rt"])
            R.op("dve", lambda e: e.reciprocal(out=rstd[:], in_=rt[:]), r=["rt"], w=["rstd"])
            R.op("dve", lambda e, xb=xb: e.scalar_tensor_tensor(out=tmp[:], in0=xt[xb][:], scalar=rstd[:, 0:1], in1=gs[:],
                                                                op0=ALU.mult, op1=ALU.mult), r=[xk, "rstd", "gs"], w=["tmp"])
            R.op("dve", lambda e: e.tensor_tensor(out=ub[:], in0=tmp[:], in1=modbc[:, 0:1024], op=ALU.add),
                 r=["tmp", "modbc"], w=["ub"])
            for ch in range(8):
                R.op("pe", lambda e, ch=ch: e.transpose(out=pT[:, ch * 128:(ch + 1) * 128], in_=ub[:, ch * 128:(ch + 1) * 128],
                                                        identity=idb[:]), r=["ub", "idb"], w=["pT"])
            R.op("act", lambda e: e.copy(out=uT[:], in_=pT[:]), r=["pT"], w=["uT"])
            nchunks = (INC + 511) // 512
            for n in range(nchunks):
                n0, n1 = n * 512, min(INC, (n + 1) * 512)
                b = n % 2
                for ch in range(8):
                    R.op("pe", lambda e, ch=ch, b=b, n0=n0, n1=n1: e.matmul(pp[b][:, 0:n1 - n0], lhsT=uT[:, ch * 128:(ch + 1) * 128],
                                                                              rhs=wsb[:, ch, n0:n1], start=(ch == 0), stop=(ch == 7)),
                         r=["uT", WS[ch]], w=["pp%d" % b])
                eng = "act" if n % 2 == 0 else "dve"
                if eng == "act":
                    R.op("act", lambda e, b=b, n0=n0, n1=n1: e.copy(out=pj[:, n0:n1], in_=pp[b][:, 0:n1 - n0]),
                         r=["pp%d" % b], w=["pj%d" % n])
                else:
                    R.op("dve", lambda e, b=b, n0=n0, n1=n1: e.tensor_copy(out=pj[:, n0:n1], in_=pp[b][:, 0:n1 - n0]),
                         r=["pp%d" % b], w=["pj%d" % n])
            PJ = lambda c0, c1: ["pj%d" % n for n in range(c0 // 512, (c1 - 1) // 512 + 1)]
            for (c0, H) in ((C_AQ, 16), (C_BQ, 9), (C_IQ, 5)):
                c1 = c0 + 64 * H
                xv = pj[:, c0:c1].rearrange("p (h two d) -> p h two d", two=2, d=32)
                ov = rb[:, c0:c1].rearrange("p (h two d) -> p h two d", two=2, d=32)
                x1, x2 = xv[:, :, 0, :], xv[:, :, 1, :]
                o1, o2 = ov[:, :, 0, :], ov[:, :, 1, :]
                cb = CN[:, t:t + 1, :].broadcast_to([128, H, 32])
                sbv = SN[:, t:t + 1, :].broadcast_to([128, H, 32])
                a1 = t1[:, 0:32 * H].rearrange("p (h d) -> p h d", d=32)
                a2 = t2[:, 0:32 * H].rearrange("p (h d) -> p h d", d=32)
                rk = PJ(c0, c1)
                R.op("dve", lambda e, a1=a1, x1=x1, cb=cb: e.tensor_tensor(out=a1, in0=x1, in1=cb, op=ALU.mult), r=rk + ["CN"], w=["t1"])
                R.op("dve", lambda e, a2=a2, x2=x2, sbv=sbv: e.tensor_tensor(out=a2, in0=x2, in1=sbv, op=ALU.mult), r=rk + ["SN"], w=["t2"])
                R.op("dve", lambda e, o1=o1, a1=a1, a2=a2: e.tensor_tensor(out=o1, in0=a1, in1=a2, op=ALU.subtract), r=["t1", "t2"], w=["rb"])
                R.op("dve", lambda e, a1=a1, x2=x2, cb=cb: e.tensor_tensor(out=a1, in0=x2, in1=cb, op=ALU.mult), r=rk + ["CN"], w=["t1"])
                R.op("dve", lambda e, a2=a2, x1=x1, sbv=sbv: e.tensor_tensor(out=a2, in0=x1, in1=sbv, op=ALU.mult), r=rk + ["SN"], w=["t2"])
                R.op("dve", lambda e, o2=o2, a1=a1, a2=a2: e.tensor_tensor(out=o2, in0=a1, in1=a2, op=ALU.add), r=["t1", "t2"], w=["rb"])
            R.op("pool", lambda e: e.tensor_copy(out=rb[:, C_CQ:C_CV], in_=pj[:, C_CQ:C_CV]), r=PJ(C_CQ, C_CV), w=["rb"])
            R.op("pool", lambda e: e.tensor_copy(out=avb[:, :, 0:128], in_=pj[:, C_AV:C_BQ].rearrange("p (h d) -> p h d", d=128)),
                 r=PJ(C_AV, C_BQ), w=["avb"])
            R.op("pool", lambda e: e.tensor_copy(out=bvb[:, 0:64], in_=pj[:, C_BV:C_IQ]), r=PJ(C_BV, C_IQ), w=["bvb"])
            R.op("pool", lambda e: e.tensor_copy(out=cvb[:, :, 0:64], in_=pj[:, C_CV:C_G].rearrange("p (h d) -> p h d", d=64)),
                 r=PJ(C_CV, C_G), w=["cvb"])
            R.op("pool", lambda e: e.tensor_scalar(out=iwb[:], in0=pj[:, C_IW:C_CQ], scalar1=1.0 / 16.0, scalar2=None, op0=ALU.mult),
                 r=PJ(C_IW, C_CQ), w=["iwb"])
            R.op("act", lambda e: e.activation(out=pj[:, C_G:INC], in_=pj[:, C_G:INC], func=AF.Sigmoid), r=PJ(C_G, INC), w=PJ(C_G, INC))
            ts = slice(t * 128, (t + 1) * 128)
            R.dma("sp", av.ap()[ts, :], avb[:].rearrange("p h d -> p (h d)"), "out", r=["avb"])
            R.dma("sp", bv.ap()[ts, :], bvb[:], "out", r=["bvb"])
            R.dma("sp", cv.ap()[ts, :], cvb[:].rearrange("p h d -> p (h d)"), "out", r=["cvb"])
            R.dma("sp", iw.ap()[ts, :], iwb[:], "out", r=["iwb"])
            R.dma("sp", gates.ap()[ts, :], pj[:, C_G:INC], "out", r=PJ(C_G, INC))
            g = 0
            for blk in range(8):
                c0 = blk * 128
                R.op("pe", lambda e, blk=blk, c0=c0: e.transpose(out=pX[0][:, blk * 128:(blk + 1) * 128], in_=rb[:, c0:c0 + 128], identity=idb[:]),
                     r=["rb", "idb"], w=["pX0"])
            R.op("act", lambda e: e.copy(out=tA[0][:], in_=pX[0][:]), r=["pX0"], w=["tA0"])
            for m in range(2):
                R.dma("sp", bass.AP(aqt, m * tok + t * 128, [[2 * tok, 64], [64 * 2 * tok, 4], [1, 128]]),
                      tA[0][m * 64:(m + 1) * 64, 0:512].rearrange("p (h q) -> p h q", q=128), "out", r=["tA0"])
                R.dma("sp", bass.AP(akt, m * tok + t * 128, [[2 * tok, 64], [64 * 2 * tok, 4], [1, 128]]),
                      tA[0][m * 64:(m + 1) * 64, 512:1024].rearrange("p (h q) -> p h q", q=128), "out", r=["tA0"])
            for blk in range(8):
                c0 = C_CQ + blk * 128
                R.op("pe", lambda e, blk=blk, c0=c0: e.transpose(out=pX[1][:, blk * 128:(blk + 1) * 128], in_=rb[:, c0:c0 + 128], identity=idb[:]),
                     r=["rb", "idb"], w=["pX1"])
            R.op("dve", lambda e: e.tensor_copy(out=tA[1][:], in_=pX[1][:]), r=["pX1"], w=["tA1"])
            for hf in range(2):
                R.dma("sp", bass.AP(cqt, hf * tok + t * 128, [[8 * tok, 64], [2 * tok, 4], [1, 128]]),
                      tA[1][hf * 64:(hf + 1) * 64, 0:512].rearrange("p (h q) -> p h q", q=128), "out", r=["tA1"])
                R.dma("sp", bass.AP(ckt, t * 1024 + hf * 128, [[8 * tok, 64], [256, 4], [1, 128]]),
                      tA[1][hf * 64:(hf + 1) * 64, 512:1024].rearrange("p (h q) -> p h q", q=128), "out", r=["tA1"])
            for h in range(8):
                c0 = C_BQ + h * 64
                R.op("pe", lambda e, h=h, c0=c0: e.transpose(out=pX[0][0:64, h * 128:(h + 1) * 128], in_=rb[:, c0:c0 + 64], identity=idb[:]),
                     r=["rb", "idb"], w=["pX0"])
            R.op("act", lambda e: e.copy(out=tA[0][0:64, :], in_=pX[0][0:64, :]), r=["pX0"], w=["tA0"])
            R.dma("sp", bqt.ap()[t][:, 0:1024], tA[0][0:64, :], "out", r=["tA0"])
            srcs = [C_IQ + h * 64 for h in range(4)] + [C_BK, C_IK]
            for i, c0 in enumerate(srcs):
                R.op("pe", lambda e, i=i, c0=c0: e.transpose(out=pX[1][0:64, i * 128:(i + 1) * 128], in_=rb[:, c0:c0 + 64], identity=idb[:]),
                     r=["rb", "idb"], w=["pX1"])
            R.op("dve", lambda e: e.tensor_copy(out=tA[1][0:64, 0:768], in_=pX[1][0:64, 0:768]), r=["pX1"], w=["tA1"])
            R.dma("sp", bqt.ap()[t][:, 1024:1536], tA[1][0:64, 0:512], "out", r=["tA1"])
            R.dma("sp", bkt.ap()[:, t * 128:(t + 1) * 128], tA[1][0:64, 512:640], "out", r=["tA1"])
            R.dma("sp", bkt.ap()[:, tok + t * 128:tok + (t + 1) * 128], tA[1][0:64, 640:768], "out", r=["tA1"])
        R.final_wait = ["out"]
        R.emit()


D = 1024
BIG = 30000.0
NIT = 22
EPS = 1e-6


def emit_T(nc, T, pfx, nt=16, phases="0ABCM"):
    tok = nt * 128
    NKT = 4 * nt
    S = NKT * 128
    di = lambda n, s, d=F32: T[n]
    aqt = di("aqt", [4, 64, 2 * tok], BF16); bq_iq = di("bq_iq", [nt, 64, 1536], BF16); iw = di("iw", [tok, 4])
    cqt = di("cqt", [64, 8 * tok], BF16); gates = di("gates", [tok, 3072]); x = di("x", [tok, D])
    akt = di("akt", [4, 64, 2 * S], BF16); av = di("av", [4, 128, NKT * 129], BF16); bkik = di("bkik", [64, 2 * S], BF16)
    bv = di("bv", [128, NKT * 65], BF16); ckb = di("ckb", [nt, 64, 8 * 640], BF16); cvb = di("cvb", [nt, 128, 5 * 520], BF16)
    zmT = di("zmT", [128, 512], BF16); zq = di("zq", [128, 512]); cmaskT = di("cmaskT", [128, 8 * 128])
    wa = di("wa", [512, D]); wb = di("wb", [512, D]); wc = di("wc", [512, D]); w_out = di("w_out", [D, D])
    w_mod = di("w_mod", [D, 6144]); b_mod = di("b_mod", [1, 6144]); cvec = di("c", [128, 8])
    lam4 = di("lam4", [1, 256]); ang_in = di("a_norm_g", [1, 128]); rbext = di("rbext", [8, 1024])
    ident = di("ident", [128, 128]); aident = di("aident", [128, 128]); laminit = di("laminit", [1, 2]); p2tab = di("p2tab", [1, NIT])
    xo = T["xo"]

    with ExitStack() as st, nc.allow_low_precision("bf16 matmul operands, fp32 accumulation"):
        sbo = lambda n, s, d=F32: st.enter_context(nc.sbuf_tensor(pfx + n, s, d))
        pso = lambda n, s, d=F32: st.enter_context(nc.psum_tensor(pfx + n, s, d))
        ya = sbo("ya", [128, nt, 512], BF16); yb = sbo("yb", [128, nt, 512], BF16); yc = sbo("yc", [128, nt, 512], BF16)
        g1bc = sbo("g1bc", [128, D]); idf = sbo("idf", [128, 128]); idb = sbo("idb", [128, 128], BF16); jdb = sbo("jdb", [128, 128], BF16)
        bigi4 = sbo("bigi4", [128, 512], BF16); nlam = sbo("nlam", [128, 1]); gn = sbo("gn", [128, 128])
        cst = sbo("cst", [128, 2])
        psS = [pso("psS%d" % i, [128, 1024]) for i in range(2)]
        psO = [pso("psO%d" % i, [128, 1024]) for i in range(2)]

        R = Rec(nc)
        with ExitStack() as s0:
            sb = lambda n, s, d=F32: s0.enter_context(nc.sbuf_tensor(pfx + n, s, d))
            cs = sb("cs", [128, 8]); ca = sb("ca", [128, 8]); CA = sb("CA", [128, 8, 128])
            wm = [sb("wm%d" % i, [128, 8, 256]) for i in range(2)]
            bmb = sb("bmb", [128, D]); l4 = sb("l4", [128, 4, 64]); lt = sb("lt", [128, 2, 64]); ls = sb("ls", [128, 2]); le = sb("le", [128, 2])
            li = sb("li", [128, 2]); agb = sb("agb", [128, 128]); stg = sb("stg", [128, 8, 128])
            R.dma("sp", cs[:], cvec.ap(), "c", w=["cs"])
            R.dma("sp", idf[:], ident.ap(), "c", w=["idf"])
            R.dma("sp", bmb[:], b_mod.ap()[0:1, 2048:3072].partition_broadcast(128), "c", w=["bmb"])
            R.dma("sp", l4[:].rearrange("p a b -> p (a b)"), lam4.ap()[0:1, :].partition_broadcast(128), "c", w=["l4"])
            R.dma("sp", li[:], laminit.ap()[0:1, :].partition_broadcast(128), "c", w=["li"])
            R.dma("sp", agb[:], ang_in.ap()[0:1, :].partition_broadcast(128), "c", w=["agb"])
            R.op("pool", lambda e: e.memset(cst[:, 0:1], EPS), w=["cst"])
            R.op("act", lambda e: e.activation(out=ca[:], in_=cs[:], func=AF.Silu), r=["cs"], w=["ca"])
            R.op("dve", lambda e: e.tensor_copy(out=CA[:], in_=ca[:].unsqueeze(2).broadcast_to([128, 8, 128])), r=["ca"], w=["CA"])
            R.op("dve", lambda e: e.tensor_copy(out=idb[:], in_=idf[:]), r=["idf"], w=["idb"])
            R.dma("sp", stg[:, 0, :], aident.ap(), "c", w=["stg"])
            R.op("dve", lambda e: e.tensor_copy(out=jdb[:], in_=stg[:, 0, :]), r=["stg"], w=["jdb"])
            for k in range(4):
                R.op("dve", lambda e, k=k: e.tensor_scalar(out=bigi4[:, k * 128:(k + 1) * 128], in0=idf[:], scalar1=BIG, scalar2=None, op0=ALU.mult),
                     r=["idf"], w=["bigi4"])
            wmv = w_mod.ap().rearrange("(ch p) n -> p ch n", p=128)
            for j in range(4):
                b = j % 2
                R.dma("sp", wm[b][:], wmv[:, :, 2048 + j * 256:2048 + (j + 1) * 256], "wm%d" % b, w=["wm%d" % b])
                for ch in range(8):
                    R.op("pe", lambda e, ch=ch, b=b: e.matmul(psS[b][:, 0:256], lhsT=CA[:, ch, :], rhs=wm[b][:, ch, :], start=(ch == 0), stop=(ch == 7)),
                         r=["CA", "wm%d" % b], w=["psS%d" % b])
                R.op("dve", lambda e, j=j, b=b: e.tensor_tensor(out=g1bc[:, j * 256:(j + 1) * 256], in0=psS[b][:, 0:256], in1=bmb[:, j * 256:(j + 1) * 256], op=ALU.add),
                     r=["psS%d" % b, "bmb"], w=["g1bc"])
            R.op("dve", lambda e: e.tensor_tensor(out=lt[:, 0, :], in0=l4[:, 0, :], in1=l4[:, 1, :], op=ALU.mult), r=["l4"], w=["lt"])
            R.op("dve", lambda e: e.tensor_tensor(out=lt[:, 1, :], in0=l4[:, 2, :], in1=l4[:, 3, :], op=ALU.mult), r=["l4"], w=["lt"])
            R.op("dve", lambda e: e.tensor_reduce(out=ls[:], in_=lt[:], axis=AX.X, op=ALU.add), r=["lt"], w=["ls"])
            R.op("act", lambda e: e.activation(out=le[:], in_=ls[:], func=AF.Exp), r=["ls"], w=["le"])
            R.op("dve", lambda e: e.tensor_tensor(out=nlam[:], in0=le[:, 1:2], in1=le[:, 0:1], op=ALU.subtract), r=["le"], w=["nlam"])
            R.op("dve", lambda e: e.tensor_tensor(out=nlam[:], in0=nlam[:], in1=li[:, 0:1], op=ALU.subtract), r=["nlam", "li"], w=["nlam"])
            R.op("dve", lambda e: e.tensor_scalar(out=gn[:], in0=agb[:], scalar1=li[:, 1:2], scalar2=None, op0=ALU.mult), r=["agb", "li"], w=["gn"])
            R.final_wait = list(R.dma_count.keys())
            if "0" in phases:
                R.emit()
        nc.all_engine_barrier()

        def attn_epilogue_BC(R, po, pk, ydst, yk, rinv):
            pv = po[:].rearrange("p (a c) -> p a c", a=2)[:, :, 0:260].rearrange("p a (h e) -> p a h e", e=65)
            R.op("dve", lambda e: e.reciprocal(out=rinv[:].rearrange("p (a h) -> p a h", a=2), in_=pv[:, :, :, 64]), r=[pk], w=["rinv"])
            R.op("dve", lambda e: e.tensor_tensor(out=ydst.rearrange("p (a h e) -> p a h e", a=2, e=64), in0=pv[:, :, :, 0:64],
                                                  in1=rinv[:].rearrange("p (a h) -> p a h", a=2).unsqueeze(3).broadcast_to([128, 2, 4, 64]), op=ALU.mult),
                 r=[pk, "rinv"], w=[yk])

        def hoff(h):
            return (h // 4) * 512 + (h % 4) * 65

        R = Rec(nc)
        with ExitStack() as s1:
            sb = lambda n, s, d=F32: s1.enter_context(nc.sbuf_tensor(pfx + n, s, d))
            ktb = [sb("ktb%d" % i, [64, 2, S], BF16) for i in range(2)]
            avh = [sb("avh%d" % i, [128, NKT, 129], BF16) for i in range(2)]
            qh = [sb("qh%d" % i, [64, 2, tok], BF16) for i in range(2)]
            zm = sb("zm", [128, 4, 128], BF16)
            pt = [sb("pt%d" % i, [128, 4, 2, 128], BF16) for i in range(2)]
            r12 = sb("r12", [128, 2]); nr2 = sb("nr2", [128, 1]); d1 = sb("d1", [128, 128]); dd = sb("dd", [128, 128])
            jk = sb("jk", [128, 128]); ssq = sb("ssq", [128, 1]); lnv = sb("lnv", [128, 1]); rstd = sb("rstd", [128, 1])
            R.dma("sp", zm[:].rearrange("p a b -> p (a b)"), zmT.ap(), "c", w=["zm"])
            gi = 0
            for h in range(4):
                hb = h % 2
                for m_ in range(2):
                    for r_ in range(4):
                        R.dma("sp", ktb[hb][:, m_, :].rearrange("d (t r p) -> d t r p", r=4, p=128)[:, :, r_, :],
                              bass.AP(akt[h // 2], ((r_ * 2 + h % 2) * 64) * 2 * tok + m_ * tok, [[2 * tok, 64], [128, nt], [1, 128]]), "kt%d" % hb, w=["ktb%d" % hb])
                SL = min(4, nt)
                for c_ in range(nt // SL):
                    for r_ in range(4):
                        R.dma("sp", avh[hb][:].rearrange("p (t r) e -> p t r e", r=4)[:, c_ * SL:(c_ + 1) * SL, r_, :],
                              bass.AP(av[c_], r_ * SL * 128 * 516 + h * 129, [[516, 128], [128 * 516, SL], [1, 129]]), "av%d" % hb, w=["avh%d" % hb])
                R.dma("sp", qh[hb][:].rearrange("p a b -> p (a b)"), aqt.ap()[h], "qh%d" % hb, w=["qh%d" % hb])
                jobs = [(t, g) for t in range(nt) for g in range(t + 1)]

                def a_qk(t, g, b, hb=hb):
                    qs = slice(t * 128, (t + 1) * 128)
                    zone = (g == t)
                    for i in range(4):
                        kt = 4 * g + i
                        ks = slice(kt * 128, (kt + 1) * 128)
                        for m in range(2):
                            ps_out = psS[b][:, (i * 2 + m) * 128:(i * 2 + m + 1) * 128]
                            R.op("pe", lambda e, ps_out=ps_out, m=m, ks=ks, qs=qs, zone=zone: e.matmul(
                                ps_out, lhsT=ktb[hb][:, m, ks], rhs=qh[hb][:, m, qs], start=True, stop=not zone),
                                r=["ktb%d" % hb, "qh%d" % hb], w=["psS%d" % b])
                            if zone:
                                R.op("pe", lambda e, ps_out=ps_out, i=i: e.matmul(ps_out, lhsT=idb[:], rhs=zm[:, i, :], start=False, stop=True),
                                     r=["zm"], w=["psS%d" % b])

                def a_pv(t, g, b, hb=hb):
                    ob = t % 2
                    ok = "psO%d" % ob
                    R.op("act", lambda e: e.activation(out=pt[b][:].rearrange("p a m q -> p (a m q)"), in_=psS[b][:], func=AF.Exp, scale=0.125),
                         r=["psS%d" % b], w=["pt%d" % b])
                    for i in range(4):
                        kt = 4 * g + i
                        for m in range(2):
                            R.op("pe", lambda e, i=i, m=m, kt=kt: e.matmul(
                                psO[ob][:, m * 512:m * 512 + 129], lhsT=pt[b][:, i, m, :], rhs=avh[hb][:, kt, :],
                                start=(g == 0 and i == 0), stop=(g == t and i == 3)),
                                r=["pt%d" % b, "avh%d" % hb], w=[ok])
                    if g != t:
                        return
                    po = psO[ob]
                    R.op("dve", lambda e: e.reciprocal(out=r12[:, 0:1], in_=po[:, 128:129]), r=[ok], w=["r12"])
                    R.op("dve", lambda e: e.reciprocal(out=r12[:, 1:2], in_=po[:, 640:641]), r=[ok], w=["r12"])
                    R.op("dve", lambda e: e.tensor_tensor(out=nr2[:], in0=r12[:, 1:2], in1=nlam[:], op=ALU.mult), r=["r12"], w=["nr2"])
                    R.op("dve", lambda e: e.tensor_scalar(out=d1[:], in0=po[:, 0:128], scalar1=r12[:, 0:1], scalar2=None, op0=ALU.mult),
                         r=[ok, "r12"], w=["d1"])
                    R.op("dve", lambda e: e.scalar_tensor_tensor(out=dd[:], in0=po[:, 512:640], scalar=nr2[:, 0:1], in1=d1[:], op0=ALU.mult, op1=ALU.add),
                         r=[ok, "nr2", "d1"], w=["dd"])

                def a_norm(t, h=h):
                    R.op("act", lambda e: e.activation(out=jk[:], in_=dd[:], func=AF.Square, accum_out=ssq[:]), r=["dd"], w=["jk", "ssq"])
                    R.op("act", lambda e: e.activation(out=lnv[:], in_=ssq[:], func=AF.Ln, scale=1.0 / 128.0, bias=cst[:, 0:1]), r=["ssq"], w=["lnv"])
                    R.op("act", lambda e: e.activation(out=rstd[:], in_=lnv[:], func=AF.Exp, scale=-0.5), r=["lnv"], w=["rstd"])
                    R.op("dve", lambda e: e.scalar_tensor_tensor(out=ya[:, t, h * 128:(h + 1) * 128], in0=dd[:], scalar=rstd[:, 0:1], in1=gn[:],
                                                                 op0=ALU.mult, op1=ALU.mult), r=["dd", "rstd"], w=["ya"])

                pend = None
                a_qk(jobs[0][0], jobs[0][1], gi % 2)
                for ji, (t, g) in enumerate(jobs):
                    b = gi % 2
                    gi += 1
                    if ji + 1 < len(jobs):
                        a_qk(jobs[ji + 1][0], jobs[ji + 1][1], gi % 2)
                    a_pv(t, g, b)
                    if pend is not None:
                        a_norm(pend)
                        pend = None
                    if g == t:
                        pend = t
                if pend is not None:
                    a_norm(pend)
            R.final_wait = list(R.dma_count.keys())
            if "A" in phases:
                R.emit()
        nc.all_engine_barrier()

        R = Rec(nc)
        with ExitStack() as s2:
            sb = lambda n, s, d=F32: s2.enter_context(nc.sbuf_tensor(pfx + n, s, d))
            kk = sb("kk", [64, 2, S], BF16); bvs = sb("bvs", [128, NKT, 65], BF16)
            sc = sb("sc", [128, S]); cA = sb("cA", [128, S], BF16); cB = sb("cB", [128, S], BF16)
            mk = [sb("mk%d" % i, [128, S], BF16) for i in range(2)]
            bqi = [sb("bqi%d" % i, [64, 1536], BF16) for i in range(2)]
            iwt = [sb("iwt%d" % i, [128, 4]) for i in range(2)]
            pt = [sb("ptb%d" % i, [128, 1024], BF16) for i in range(2)]
            rl = [sb("rl%d" % i, [128, 512]) for i in range(2)] * 2
            zqs = sb("zqs", [128, 512]); p2 = sb("p2", [128, NIT]); WN = sb("WN", [128, NIT])
            rmin = sb("rmin", [128, 1]); rmax = sb("rmax", [128, 1]); w0 = sb("w0", [128, 1]); lo = sb("lo", [128, 1]); mid = sb("mid", [128, 1])
            cnt = sb("cnt", [128, 1]); dl = sb("dl", [128, 1]); hi = sb("hi", [128, 1]); chi = sb("chi", [128, 1]); mrem = sb("mrem", [128, 1])
            rinv = sb("rinv", [128, 8])
            for m_ in range(2):
                for r_ in range(4):
                    R.dma("sp", kk[:, m_, :].rearrange("d (t r p) -> d t r p", r=4, p=128)[:, :, r_, :],
                          bass.AP(bkik, r_ * 64 * 2 * tok + m_ * tok, [[2 * tok, 64], [128, nt], [1, 128]]), "c", w=["kk"])
            for r_ in range(4):
                R.dma("sp", bvs[:].rearrange("p (t r) e -> p t r e", r=4)[:, :, r_, :],
                      bass.AP(bv, r_ * tok * 65, [[65, 128], [128 * 65, nt], [1, 65]]), "c", w=["bvs"])
            R.dma("sp", zqs[:], zq.ap(), "c", w=["zqs"])
            R.dma("sp", p2[:], p2tab.ap()[0:1, :].partition_broadcast(128), "c", w=["p2"])
            psI = [psS[hh // 2][:, (hh % 2) * 512:(hh % 2) * 512 + 512] for hh in range(4)]
            def b_select(t):
                b = t % 2
                n = (4 * t + 4) * 128
                R.dma("sp", bqi[b][:], bq_iq.ap()[t], "bqi%d" % b, w=["bqi%d" % b])
                R.dma("sp", iwt[b][:], iw.ap()[t * 128:(t + 1) * 128, :], "bqi%d" % b, w=["iwt%d" % b])
                for g in range(t + 1):
                    gs_ = slice(g * 512, (g + 1) * 512)
                    for hh in range(4):
                        R.op("pe", lambda e, hh=hh, b=b, gs_=gs_: e.matmul(psI[hh], lhsT=bqi[b][:, 1024 + hh * 128:1024 + (hh + 1) * 128], rhs=kk[:, 1, gs_],
                                                                           start=True, stop=True), r=["bqi%d" % b, "kk"], w=["psS%d" % (hh // 2)])
                        R.op("act", lambda e, hh=hh: e.activation(out=rl[hh][:], in_=psI[hh], func=AF.Relu), r=["psS%d" % (hh // 2)], w=["rl%d" % (hh % 2)])
                        if hh == 0:
                            R.op("dve", lambda e, b=b, gs_=gs_: e.tensor_scalar(out=sc[:, gs_], in0=rl[0][:], scalar1=iwt[b][:, 0:1], scalar2=None, op0=ALU.mult),
                                 r=["rl0", "iwt%d" % b], w=["sc"])
                        else:
                            R.op("dve", lambda e, hh=hh, b=b, gs_=gs_: e.scalar_tensor_tensor(out=sc[:, gs_], in0=rl[hh][:], scalar=iwt[b][:, hh:hh + 1], in1=sc[:, gs_],
                                                                                               op0=ALU.mult, op1=ALU.add), r=["rl%d" % (hh % 2), "iwt%d" % b, "sc"], w=["sc"])
                R.op("dve", lambda e, n=n: e.tensor_reduce(out=rmin[:], in_=sc[:, 0:n], axis=AX.X, op=ALU.min), r=["sc"], w=["rmin"])
                R.op("dve", lambda e, t=t: e.tensor_tensor(out=sc[:, t * 512:(t + 1) * 512], in0=sc[:, t * 512:(t + 1) * 512], in1=zqs[:], op=ALU.add),
                     r=["sc", "zqs"], w=["sc"])
                R.op("dve", lambda e, n=n: e.tensor_reduce(out=rmax[:], in_=sc[:, 0:n], axis=AX.X, op=ALU.max), r=["sc"], w=["rmax"])
                R.op("dve", lambda e: e.tensor_tensor(out=w0[:], in0=rmax[:], in1=rmin[:], op=ALU.subtract), r=["rmax", "rmin"], w=["w0"])
                R.op("dve", lambda e: e.tensor_scalar(out=w0[:], in0=w0[:], scalar1=1.001, scalar2=1e-6, op0=ALU.mult, op1=ALU.add), r=["w0"], w=["w0"])
                R.op("dve", lambda e: e.tensor_scalar(out=WN[:], in0=p2[:], scalar1=w0[:, 0:1], scalar2=None, op0=ALU.mult), r=["p2", "w0"], w=["WN"])
                R.op("dve", lambda e: e.tensor_copy(out=lo[:], in_=rmin[:]), r=["rmin"], w=["lo"])
                for it in range(NIT):
                    R.op("dve", lambda e, it=it: e.tensor_tensor(out=mid[:], in0=lo[:], in1=WN[:, it:it + 1], op=ALU.add), r=["lo", "WN"], w=["mid"])
                    R.op("dve", lambda e, n=n: e.tensor_scalar(out=cA[:, 0:n], in0=sc[:, 0:n], scalar1=mid[:, 0:1], scalar2=None, op0=ALU.is_ge, op1=ALU.add,
                                                               accum_out=cnt[:]), r=["sc", "mid"], w=["cA", "cnt"])
                    R.op("dve", lambda e, it=it: e.tensor_scalar(out=dl[:], in0=cnt[:], scalar1=256.0, scalar2=WN[:, it:it + 1], op0=ALU.is_ge, op1=ALU.mult),
                         r=["cnt", "WN"], w=["dl"])
                    R.op("dve", lambda e: e.tensor_tensor(out=lo[:], in0=lo[:], in1=dl[:], op=ALU.add), r=["lo", "dl"], w=["lo"])
                R.op("dve", lambda e: e.tensor_tensor(out=hi[:], in0=lo[:], in1=WN[:, NIT - 1:NIT], op=ALU.add), r=["lo", "WN"], w=["hi"])
                R.op("dve", lambda e, n=n: e.tensor_scalar(out=cB[:, 0:n], in0=sc[:, 0:n], scalar1=hi[:, 0:1], scalar2=None, op0=ALU.is_ge, op1=ALU.add,
                                                           accum_out=chi[:]), r=["sc", "hi"], w=["cB", "chi"])
                R.op("dve", lambda e, n=n: e.tensor_scalar(out=cA[:, 0:n], in0=sc[:, 0:n], scalar1=lo[:, 0:1], scalar2=None, op0=ALU.is_ge), r=["sc", "lo"], w=["cA"])
                R.op("dve", lambda e, n=n: e.tensor_tensor(out=cA[:, 0:n], in0=cA[:, 0:n], in1=cB[:, 0:n], op=ALU.subtract), r=["cA", "cB"], w=["cA"])
                R.op("dve", lambda e: e.tensor_scalar(out=mrem[:], in0=chi[:], scalar1=-1.0, scalar2=256.0, op0=ALU.mult, op1=ALU.add), r=["chi"], w=["mrem"])
                R.op("dve", lambda e, n=n: e.tensor_tensor_scan(out=sc[:, 0:n], data0=cA[:, 0:n], data1=cA[:, 0:n], initial=0.0, op0=ALU.add, op1=ALU.max),
                     r=["cA"], w=["sc"])
                R.op("dve", lambda e, n=n: e.scalar_tensor_tensor(out=cA[:, 0:n], in0=sc[:, 0:n], scalar=mrem[:, 0:1], in1=cA[:, 0:n], op0=ALU.is_le, op1=ALU.mult),
                     r=["sc", "mrem", "cA"], w=["cA"])
                R.op("dve", lambda e, n=n, b=b: e.scalar_tensor_tensor(out=mk[b][:, 0:n], in0=cA[:, 0:n], scalar=-1.0, in1=cB[:, 0:n], op0=ALU.add, op1=ALU.add),
                     r=["cA", "cB"], w=["mk%d" % b])

            def b_attend(t):
                b = t % 2
                ob = t % 2
                ok = "psO%d" % ob
                for kt in range(4 * t + 4):
                    b2 = kt % 2
                    ks = slice(kt * 128, (kt + 1) * 128)
                    for half in range(2):
                        hs = slice(half * 512, (half + 1) * 512)
                        R.op("pe", lambda e, b2=b2, hs=hs, ks=ks, b=b: e.matmul(psS[b2][:, hs], lhsT=kk[:, 0, ks], rhs=bqi[b][:, hs], start=True, stop=False),
                             r=["kk", "bqi%d" % b], w=["psS%d" % b2])
                        R.op("pe", lambda e, b2=b2, hs=hs, ks=ks, b=b: e.matmul(psS[b2][:, hs], lhsT=mk[b][:, ks], rhs=bigi4[:], start=False, stop=True),
                             r=["mk%d" % b], w=["psS%d" % b2])
                    R.op("act", lambda e, b2=b2: e.activation(out=pt[b2][:], in_=psS[b2][:], func=AF.Exp, scale=0.125), r=["psS%d" % b2], w=["ptb%d" % b2])
                    for h in range(8):
                        R.op("pe", lambda e, h=h, b2=b2, kt=kt, ob=ob, t=t: e.matmul(psO[ob][:, hoff(h):hoff(h) + 65], lhsT=pt[b2][:, h * 128:(h + 1) * 128],
                                                                                    rhs=bvs[:, kt, :], start=(kt == 0 and h % 4 == 0), stop=(kt == 4 * t + 3 and h % 4 == 3)),
                             r=["ptb%d" % b2, "bvs"], w=[ok])
                attn_epilogue_BC(R, psO[ob], ok, yb[:, t, :], "yb", rinv)

            b_select(0)
            for t in range(1, nt):
                b_select(t)
                b_attend(t - 1)
            b_attend(nt - 1)
            R.final_wait = list(R.dma_count.keys())
            if "B" in phases:
                R.emit()
        nc.all_engine_barrier()

        R = Rec(nc)
        with ExitStack() as s3:
            sb = lambda n, s, d=F32: s3.enter_context(nc.sbuf_tensor(pfx + n, s, d))
            cqs = sb("cqs", [64, 8, tok], BF16)
            EBT = sb("EBT", [128, 8, 8, 128], BF16); stg = sb("stgc", [128, 8, 128]); cmk = sb("cmk", [128, 8, 128])
            R.dma("sp", cmk[:].rearrange("p a b -> p (a b)"), cmaskT.ap(), "c", w=["cmk"])
            for j in range(8):
                src = bass.AP(rbext, T["rb_off"] + 1665 - 128 * j, [[1, 128], [T["rb_w"], 8], [1, 128]])
                R.dma("sp", stg[:], src, "stg", w=["stg"])
                R.op("dve", lambda e, j=j: e.scalar_tensor_tensor(out=EBT[:, j, :, :], in0=stg[:], scalar=8.0,
                                                                   in1=cmk[:, j, :].unsqueeze(1).broadcast_to([128, 8, 128]),
                                                                   op0=ALU.mult, op1=ALU.add), r=["stg", "cmk"], w=["EBT"])
            ck = [sb("ck%d" % i, [64, 8, 8, 128], BF16) for i in range(2)]
            cvs = [sb("cvs%d" % i, [128, 8, 520], BF16) for i in range(2)]
            pt = [sb("ptc%d" % i, [128, 1024], BF16) for i in range(2)]
            rinv = sb("rinvc", [128, 8])
            R.dma("sp", cqs[:].rearrange("p a b -> p (a b)"), cqt.ap(), "c", w=["cqs"])
            gi = 0
            for t in range(nt):
                b = t % 2
                for si, s_ in enumerate((t - 1, t)):
                    if s_ < 0:
                        R.op("pool", lambda e, b=b: e.memset(ck[b][:, 0:4, :, :], 0.0), w=["ck%d" % b])
                        R.op("pool", lambda e, b=b: e.memset(cvs[b][:, 0:4, :], 0.0), w=["cvs%d" % b])
                        continue
                    for r_ in range(4):
                        for c_ in range(2):
                            R.dma("sp", ck[b][c_ * 32:(c_ + 1) * 32, si * 4 + r_, :, :].rearrange("p h k -> p (h k)"),
                                  bass.AP(ckb[c_], r_ * 32 * 8 * tok + s_ * 1024, [[8 * tok, 32], [1, 1024]]), "ck%d" % b, w=["ck%d" % b])
                    SLc = min(4, nt)
                    R.dma("sp", cvs[b][:, si * 4:(si + 1) * 4, :], bass.AP(cvb[s_ // SLc], (s_ % SLc) * 128 * 520, [[520, 128], [SLc * 128 * 520, 4], [1, 520]]),
                          "ck%d" % b, w=["cvs%d" % b])
                ob = t % 2
                ok = "psO%d" % ob
                for j in range(8):
                    b2 = gi % 2
                    gi += 1
                    for h in range(8):
                        R.op("pe", lambda e, h=h, b=b, b2=b2, j=j, t=t: e.matmul(
                            psS[b2][:, h * 128:(h + 1) * 128], lhsT=ck[b][:, j, h, :],
                            rhs=cqs[:, h, t * 128:(t + 1) * 128], start=(h % 4 == 0), stop=False), r=["ck%d" % b, "cqs"], w=["psS%d" % b2])
                    for half in range(2):
                        R.op("pe", lambda e, half=half, b2=b2, j=j: e.matmul(psS[b2][:, half * 512:(half + 1) * 512], lhsT=jdb[:],
                                                                             rhs=EBT[:, j, half * 4:(half + 1) * 4, :].rearrange("p h q -> p (h q)"),
                                                                             start=False, stop=True), r=["EBT"], w=["psS%d" % b2])
                    R.op("act", lambda e, b2=b2: e.activation(out=pt[b2][:], in_=psS[b2][:], func=AF.Exp, scale=0.125), r=["psS%d" % b2], w=["ptc%d" % b2])
                    for h in range(8):
                        R.op("pe", lambda e, h=h, b2=b2, j=j, b=b, ob=ob: e.matmul(psO[ob][:, hoff(h):hoff(h) + 65], lhsT=pt[b2][:, h * 128:(h + 1) * 128],
                                                                                  rhs=cvs[b][:, j, h * 65:(h + 1) * 65], start=(j == 0 and h % 4 == 0), stop=(j == 7 and h % 4 == 3)),
                             r=["ptc%d" % b2, "cvs%d" % b], w=[ok])
                attn_epilogue_BC(R, psO[ob], ok, yc[:, t, :], "yc", rinv)
            R.final_wait = list(R.dma_count.keys())
            if "C" in phases:
                R.emit()
        nc.all_engine_barrier()

        R = Rec(nc)
        with ExitStack() as s4:
            sb = lambda n, s, d=F32: s4.enter_context(nc.sbuf_tensor(pfx + n, s, d))
            wbr = [sb("wbr%d" % i, [128, 4, D], BF16) for i in range(3)]
            wo = sb("wo", [128, 8, D], BF16)
            gt = [sb("gt%d" % i, [128, 3072]) for i in range(2)]
            xt = [sb("xt%d" % i, [128, D]) for i in range(2)]
            yT = sb("yT", [128, 512], BF16); mg = sb("mg", [128, D]); tt = sb("tt", [128, 512]); mgb = sb("mgb", [128, D], BF16)
            mT = sb("mT", [128, D], BF16); xot = [sb("xot%d" % i, [128, D]) for i in range(2)]
            for i, wsrc in enumerate((wa, wb, wc)):
                R.dma("pool", wbr[i][:], wsrc.ap().rearrange("(ch p) n -> p ch n", p=128), "w", w=["wbr%d" % i])
            R.dma("pool", wo[:], w_out.ap().rearrange("(ch p) n -> p ch n", p=128), "w", w=["wo"])
            pXt = psO[0][:, 0:512].bitcast(BF16)
            assert tuple(pXt.shape) == (128, 1024), pXt.shape
            for t in range(nt):
                b = t % 2
                R.dma("sp", gt[b][:], gates.ap()[t * 128:(t + 1) * 128, :], "gx%d" % b, w=["gt%d" % b])
                R.dma("sp", xt[b][:], x.ap()[t * 128:(t + 1) * 128, :], "gx%d" % b, w=["xt%d" % b])
                for bi, (ysrc, yk) in enumerate(((ya, "ya"), (yb, "yb"), (yc, "yc"))):
                    for ch in range(4):
                        R.op("pe", lambda e, ysrc=ysrc, ch=ch, t=t: e.transpose(out=pXt[:, ch * 128:(ch + 1) * 128], in_=ysrc[:, t, ch * 128:(ch + 1) * 128], identity=idb[:]),
                             r=[], w=["pXt"])
                    R.op("act", lambda e: e.copy(out=yT[:], in_=pXt[:, 0:512]), r=["pXt"], w=["yT"])
                    for half in range(2):
                        hs = slice(half * 512, (half + 1) * 512)
                        for ch in range(4):
                            R.op("pe", lambda e, ch=ch, bi=bi, half=half, hs=hs: e.matmul(psS[half][:, 0:512], lhsT=yT[:, ch * 128:(ch + 1) * 128], rhs=wbr[bi][:, ch, hs],
                                                                                          start=(ch == 0), stop=(ch == 3)), r=["yT", "wbr%d" % bi], w=["psS%d" % half])
                        gsl = gt[b][:, bi * 1024 + half * 512: bi * 1024 + (half + 1) * 512]
                        if bi == 0:
                            R.op("dve", lambda e, half=half, hs=hs, gsl=gsl: e.tensor_tensor(out=mg[:, hs], in0=psS[half][:, 0:512], in1=gsl, op=ALU.mult),
                                 r=["psS%d" % half, "gt%d" % b], w=["mg%d" % half])
                        else:
                            R.op("dve", lambda e, half=half, gsl=gsl: e.tensor_tensor(out=tt[:], in0=psS[half][:, 0:512], in1=gsl, op=ALU.mult),
                                 r=["psS%d" % half, "gt%d" % b], w=["tt"])
                            dst = mg if bi == 1 else mgb
                            R.op("dve", lambda e, hs=hs, dst=dst: e.tensor_tensor(out=dst[:, hs], in0=mg[:, hs], in1=tt[:], op=ALU.add),
                                 r=["tt", "mg%d" % half], w=["mg%d" % half if bi == 1 else "mgb%d" % half])
                for ch in range(8):
                    R.op("pe", lambda e, ch=ch: e.transpose(out=pXt[:, ch * 128:(ch + 1) * 128], in_=mgb[:, ch * 128:(ch + 1) * 128], identity=idb[:]),
                         r=["mgb0", "mgb1"], w=["pXt"])
                R.op("act", lambda e: e.copy(out=mT[:], in_=pXt[:]), r=["pXt"], w=["mT"])
                for half in range(2):
                    hs = slice(half * 512, (half + 1) * 512)
                    for ch in range(8):
                        R.op("pe", lambda e, ch=ch, half=half, hs=hs: e.matmul(psS[half][:, 0:512], lhsT=mT[:, ch * 128:(ch + 1) * 128], rhs=wo[:, ch, hs],
                                                                               start=(ch == 0), stop=(ch == 7)), r=["mT", "wo"], w=["psS%d" % half])
                    R.op("dve", lambda e, half=half, hs=hs: e.tensor_tensor(out=tt[:], in0=psS[half][:, 0:512], in1=g1bc[:, hs], op=ALU.mult),
                         r=["psS%d" % half], w=["tt"])
                    R.op("dve", lambda e, hs=hs, b=b: e.tensor_tensor(out=xot[b][:, hs], in0=tt[:], in1=xt[b][:, hs], op=ALU.add),
                         r=["tt", "xt%d" % b], w=["xot%d_%d" % (b, half)])
                R.dma("sp", xo.ap()[t * 128:(t + 1) * 128, :], xot[b][:], "out%d" % b, r=["xot%d_0" % b, "xot%d_1" % b])
            R.final_wait = list(R.dma_count.keys())
            if "M" in phases:
                R.emit()


D = 1024
EPS = 1e-6
NE = 16
FF = 512
BIGR = 1.0e4


def emit_M(nc, T, pfx, nt=16, final=False, ne=NE, phases="123", cut=9):
    tok = nt * 128
    di = lambda n, s, d=F32: T[n]
    x = di("x", [tok, D]); cvec = di("c", [128, 8]); w_mod = di("w_mod", [D, 6144]); b_mod = di("b_mod", [1, 6144])
    norm_g = di("norm_g", [1, D]); router_w = di("router_w", [D, 16]); router_b = di("router_b", [1, 16])
    w1 = di("w1", [NE, D, FF]); w3 = di("w3", [NE, D, FF]); w2 = di("w2", [NE, FF, D]); ident = di("ident", [128, 128])
    final_g = di("final_g", [1, D])
    xo = T["xo"]
    TG = (nt + 3) // 4

    with ExitStack() as st, nc.allow_low_precision("bf16 matmul operands, fp32 accumulation"):
        sbo = lambda n, s, d=F32: st.enter_context(nc.sbuf_tensor(pfx + n, s, d))
        pso = lambda n, s, d=F32: st.enter_context(nc.psum_tensor(pfx + n, s, d))
        modbc = sbo("modbc", [128, 3 * D]); gs = sbo("gs", [128, D]); idf = sbo("idf", [128, 128])
        uT = sbo("uT", [128, 8, tok], BF16); comb = sbo("comb", [128, nt, 16]); yacc = sbo("yacc", [128, nt, D])
        cst = sbo("cst", [128, 2]); fgb = sbo("fgb", [128, D])
        PS = [pso("PS%d" % i, [128, 512]) for i in range(8)]

        R = Rec(nc)
        with ExitStack() as s0:
            sb = lambda n, s, d=F32: s0.enter_context(nc.sbuf_tensor(pfx + n, s, d))
            cs = sb("cs", [128, 8]); ca = sb("ca", [128, 8]); CA = sb("CA", [128, 8, 128])
            wm = [sb("wm%d" % i, [128, 8, 256]) for i in range(2)]
            bmb = sb("bmb", [128, 3 * D]); gbc = sb("gbc", [128, D]); rw = sb("rw", [128, 8, 16]); rbb = sb("rbb", [128, 16])
            xt = [sb("xt%d" % i, [128, D]) for i in range(2)]
            junk = sb("junk", [128, D], BF16); ss = sb("ss", [128, 1]); rt = sb("rt", [128, 1]); rstd = sb("rstd", [128, 1])
            tmp = sb("tmp", [128, D]); u2 = sb("u2", [128, D]); uh = sb("uh", [128, D], BF16); ul = sb("ul", [128, D], BF16)
            ulT = sb("ulT", [128, D], BF16); idb = sb("idb", [128, 128], BF16); rwh = sb("rwh", [128, 8, 16], BF16); rwl = sb("rwl", [128, 8, 16], BF16)
            pXh = PS[2][:].bitcast(BF16); pXl = PS[3][:].bitcast(BF16)
            aff = sb("aff", [128, 16]); sel = sb("sel", [128, 16]); m1 = sb("m1", [128, 4]); eq = sb("eq", [128, 16]); s2 = sb("s2", [128, 16])
            m2 = sb("m2", [128, 4]); gsum = sb("gsum", [128, 4]); gmax = sb("gmax", [128, 1]); ing = sb("ing", [128, 4]); pen = sb("pen", [128, 4])
            selm = sb("selm", [128, 16]); t1 = sb("t1", [128, 1]); e1 = sb("e1", [128, 16]); t2 = sb("t2", [128, 1]); e2 = sb("e2", [128, 16])
            den = sb("den", [128, 1]); rden = sb("rden", [128, 1])
            R.dma("sp", cs[:], cvec.ap(), "c", w=["cs"])
            R.dma("sp", idf[:], ident.ap(), "c", w=["idf"])
            R.dma("sp", bmb[:], b_mod.ap()[0:1, 3072:6144].partition_broadcast(128), "c", w=["bmb"])
            R.dma("sp", gbc[:], norm_g.ap()[0:1, :].partition_broadcast(128), "c", w=["gbc"])
            R.dma("sp", fgb[:], final_g.ap()[0:1, :].partition_broadcast(128), "c", w=["fgb"])
            R.dma("sp", rbb[:], router_b.ap()[0:1, :].partition_broadcast(128), "c", w=["rbb"])
            R.dma("sp", rw[:], router_w.ap().rearrange("(ch p) n -> p ch n", p=128), "c", w=["rw"])
            R.op("pool", lambda e: e.memset(cst[:, 0:1], EPS), w=["cst"])
            R.op("dve", lambda e: e.tensor_copy(out=idb[:], in_=idf[:]), r=["idf"], w=["idb"])
            R.op("dve", lambda e: e.tensor_copy(out=rwh[:], in_=rw[:]), r=["rw"], w=["rwh"])
            R.op("dve", lambda e: e.tensor_tensor(out=rwl[:], in0=rw[:], in1=rwh[:], op=ALU.subtract), r=["rw", "rwh"], w=["rwl"])
            R.op("act", lambda e: e.activation(out=ca[:], in_=cs[:], func=AF.Silu), r=["cs"], w=["ca"])
            R.op("dve", lambda e: e.tensor_copy(out=CA[:], in_=ca[:].unsqueeze(2).broadcast_to([128, 8, 128])), r=["ca"], w=["CA"])
            wmv = w_mod.ap().rearrange("(ch p) n -> p ch n", p=128)
            for j in range(12):
                b = j % 2
                R.dma("sp", wm[b][:], wmv[:, :, 3072 + j * 256:3072 + (j + 1) * 256], "wm%d" % b, w=["wm%d" % b])
                for ch in range(8):
                    R.op("pe", lambda e, ch=ch, b=b: e.matmul(PS[b][:, 0:256], lhsT=CA[:, ch, :], rhs=wm[b][:, ch, :], start=(ch == 0), stop=(ch == 7)),
                         r=["CA", "wm%d" % b], w=["PS%d" % b])
                R.op("dve", lambda e, j=j, b=b: e.tensor_tensor(out=modbc[:, j * 256:(j + 1) * 256], in0=PS[b][:, 0:256], in1=bmb[:, j * 256:(j + 1) * 256], op=ALU.add),
                     r=["PS%d" % b, "bmb"], w=["modbc"])
            R.op("dve", lambda e: e.scalar_tensor_tensor(out=gs[:], in0=modbc[:, D:2 * D], scalar=1.0, in1=gbc[:], op0=ALU.add, op1=ALU.mult),
                 r=["modbc", "gbc"], w=["gs"])
            for t in range(nt):
                b = t % 2
                xk = "xt%d" % b
                R.dma("sp", xt[b][:], x.ap()[t * 128:(t + 1) * 128, :], xk, w=[xk])
                R.op("act", lambda e, b=b: e.activation(out=junk[:], in_=xt[b][:], func=AF.Square, accum_out=ss[:]), r=[xk], w=["junk", "ss"])
                R.op("act", lambda e: e.activation(out=rt[:], in_=ss[:], func=AF.Sqrt, scale=1.0 / D, bias=cst[:, 0:1]), r=["ss", "cst"], w=["rt"])
                R.op("dve", lambda e: e.reciprocal(out=rstd[:], in_=rt[:]), r=["rt"], w=["rstd"])
                R.op("dve", lambda e, b=b: e.scalar_tensor_tensor(out=tmp[:], in0=xt[b][:], scalar=rstd[:, 0:1], in1=gs[:], op0=ALU.mult, op1=ALU.mult),
                     r=[xk, "rstd", "gs"], w=["tmp"])
                R.op("dve", lambda e: e.tensor_tensor(out=u2[:], in0=tmp[:], in1=modbc[:, 0:D], op=ALU.add), r=["tmp", "modbc"], w=["u2"])
                if cut < 2:
                    continue
                R.op("dve", lambda e: e.tensor_copy(out=uh[:], in_=u2[:]), r=["u2"], w=["uh"])
                R.op("dve", lambda e: e.tensor_tensor(out=ul[:], in0=u2[:], in1=uh[:], op=ALU.subtract), r=["u2", "uh"], w=["ul"])
                for ch in range(8):
                    R.op("pe", lambda e, ch=ch: e.transpose(out=pXh[:, ch * 128:(ch + 1) * 128], in_=uh[:, ch * 128:(ch + 1) * 128], identity=idb[:]),
                         r=["uh", "idb"], w=["PS2"])
                R.op("act", lambda e, t=t: e.copy(out=uT[:, :, t * 128:(t + 1) * 128], in_=pXh.rearrange("p (c q) -> p c q", q=128)), r=["PS2"], w=["uT"])
                for ch in range(8):
                    R.op("pe", lambda e, ch=ch: e.transpose(out=pXl[:, ch * 128:(ch + 1) * 128], in_=ul[:, ch * 128:(ch + 1) * 128], identity=idb[:]),
                         r=["ul", "idb"], w=["PS3"])
                R.op("dve", lambda e: e.tensor_copy(out=ulT[:], in_=pXl), r=["PS3"], w=["ulT"])
                if cut < 3:
                    continue
                for ch in range(8):
                    uhs = uT[:, ch, t * 128:(t + 1) * 128]
                    R.op("pe", lambda e, ch=ch, uhs=uhs: e.matmul(PS[4][:, 0:16], lhsT=uhs, rhs=rwh[:, ch, :], start=(ch == 0), stop=False), r=["uT", "rwh"], w=["PS4"])
                    R.op("pe", lambda e, ch=ch, uhs=uhs: e.matmul(PS[4][:, 0:16], lhsT=uhs, rhs=rwl[:, ch, :], start=False, stop=False), r=["uT", "rwl"], w=["PS4"])
                    R.op("pe", lambda e, ch=ch: e.matmul(PS[4][:, 0:16], lhsT=ulT[:, ch * 128:(ch + 1) * 128], rhs=rwh[:, ch, :], start=False, stop=(ch == 7)),
                         r=["ulT", "rwh"], w=["PS4"])
                v4 = lambda a: a[:].rearrange("p (g k) -> p g k", k=4)
                R.op("act", lambda e: e.activation(out=aff[:], in_=PS[4][:, 0:16], func=AF.Sigmoid), r=["PS4"], w=["aff"])
                if cut < 4:
                    continue
                R.op("dve", lambda e: e.tensor_tensor(out=sel[:], in0=aff[:], in1=rbb[:], op=ALU.add), r=["aff", "rbb"], w=["sel"])
                R.op("dve", lambda e: e.tensor_reduce(out=m1[:], in_=v4(sel), axis=AX.X, op=ALU.max), r=["sel"], w=["m1"])
                R.op("dve", lambda e: e.tensor_tensor(out=v4(eq), in0=v4(sel), in1=m1[:].unsqueeze(2).broadcast_to([128, 4, 4]), op=ALU.is_equal), r=["sel", "m1"], w=["eq"])
                R.op("dve", lambda e: e.scalar_tensor_tensor(out=s2[:], in0=eq[:], scalar=-BIGR, in1=sel[:], op0=ALU.mult, op1=ALU.add), r=["eq", "sel"], w=["s2"])
                R.op("dve", lambda e: e.tensor_reduce(out=m2[:], in_=v4(s2), axis=AX.X, op=ALU.max), r=["s2"], w=["m2"])
                R.op("dve", lambda e: e.tensor_tensor(out=gsum[:], in0=m1[:], in1=m2[:], op=ALU.add), r=["m1", "m2"], w=["gsum"])
                R.op("dve", lambda e: e.tensor_reduce(out=gmax[:], in_=gsum[:], axis=AX.X, op=ALU.max), r=["gsum"], w=["gmax"])
                R.op("dve", lambda e: e.tensor_scalar(out=pen[:], in0=gsum[:], scalar1=gmax[:, 0:1], scalar2=-BIGR, op0=ALU.is_lt, op1=ALU.mult), r=["gsum", "gmax"], w=["pen"])
                R.op("dve", lambda e: e.tensor_tensor(out=v4(selm), in0=v4(sel), in1=pen[:].unsqueeze(2).broadcast_to([128, 4, 4]), op=ALU.add), r=["sel", "pen"], w=["selm"])
                R.op("dve", lambda e: e.tensor_reduce(out=t1[:], in_=selm[:], axis=AX.X, op=ALU.max), r=["selm"], w=["t1"])
                R.op("dve", lambda e: e.tensor_scalar(out=e1[:], in0=selm[:], scalar1=t1[:, 0:1], scalar2=None, op0=ALU.is_equal), r=["selm", "t1"], w=["e1"])
                R.op("dve", lambda e: e.scalar_tensor_tensor(out=s2[:], in0=e1[:], scalar=-BIGR, in1=selm[:], op0=ALU.mult, op1=ALU.add), r=["e1", "selm"], w=["s2"])
                R.op("dve", lambda e: e.tensor_reduce(out=t2[:], in_=s2[:], axis=AX.X, op=ALU.max), r=["s2"], w=["t2"])
                R.op("dve", lambda e: e.tensor_scalar(out=e2[:], in0=s2[:], scalar1=t2[:, 0:1], scalar2=None, op0=ALU.is_equal), r=["s2", "t2"], w=["e2"])
                R.op("dve", lambda e: e.tensor_tensor(out=e1[:], in0=e1[:], in1=e2[:], op=ALU.add), r=["e1", "e2"], w=["e1"])
                R.op("dve", lambda e: e.tensor_tensor(out=e2[:], in0=e1[:], in1=aff[:], op=ALU.mult), r=["e1", "aff"], w=["e2"])
                R.op("dve", lambda e: e.tensor_reduce(out=den[:], in_=e2[:], axis=AX.X, op=ALU.add), r=["e2"], w=["den"])
                R.op("dve", lambda e: e.reciprocal(out=rden[:], in_=den[:]), r=["den"], w=["rden"])
                R.op("dve", lambda e, t=t: e.tensor_scalar(out=comb[:, t, :], in0=e2[:], scalar1=rden[:, 0:1], scalar2=None, op0=ALU.mult), r=["e2", "rden"], w=["comb"])
            R.final_wait = list(R.dma_count.keys())
            if "1" in phases:
                R.emit()
        nc.all_engine_barrier()

        R = Rec(nc)
        with ExitStack() as s1:
            sb = lambda n, s, d=F32: s1.enter_context(nc.sbuf_tensor(pfx + n, s, d))
            w1s = [sb("w1s%d" % i, [128, 8, FF], BF16) for i in range(2)]
            w3s = [sb("w3s%d" % i, [128, 8, FF], BF16) for i in range(2)]
            w2s = [sb("w2s%d" % i, [128, 4, D], BF16) for i in range(2)]
            sl = [sb("sl%d" % i, [128, 512]) for i in range(2)]
            hT = [sb("hT%d" % i, [128, 4, 512], BF16) for i in range(2)]
            kc = [0]

            def m_h(e_, tg, hb):
                wb = e_ % 2
                if tg == 0:
                    R.dma("pool", w1s[wb][:], w1.ap()[e_].rearrange("(ch p) f -> p ch f", p=128), "w1_%d" % wb, w=["w1s%d" % wb])
                    R.dma("pool", w3s[wb][:], w3.ap()[e_].rearrange("(ch p) f -> p ch f", p=128), "w1_%d" % wb, w=["w3s%d" % wb])
                    R.dma("pool", w2s[wb][:], w2.ap()[e_].rearrange("(ch p) n -> p ch n", p=128), "w1_%d" % wb, w=["w2s%d" % wb])
                ntl = min(4, nt - tg * 4)
                ncol = ntl * 128
                tsl = slice(tg * 512, tg * 512 + ncol)
                for fc in range(4):
                    pb = (kc[0] % 2) * 2
                    kc[0] += 1
                    for ch in range(8):
                        R.op("pe", lambda e, ch=ch, fc=fc, pb=pb: e.matmul(PS[pb][:, 0:ncol], lhsT=w1s[wb][:, ch, fc * 128:(fc + 1) * 128], rhs=uT[:, ch, tsl],
                                                                           start=(ch == 0), stop=(ch == 7)), r=["w1s%d" % wb], w=["PS%d" % pb])
                    for ch in range(8):
                        R.op("pe", lambda e, ch=ch, fc=fc, pb=pb: e.matmul(PS[pb + 1][:, 0:ncol], lhsT=w3s[wb][:, ch, fc * 128:(fc + 1) * 128], rhs=uT[:, ch, tsl],
                                                                           start=(ch == 0), stop=(ch == 7)), r=["w3s%d" % wb], w=["PS%d" % (pb + 1)])
                    sb_ = kc[0] % 2
                    R.op("act", lambda e, pb=pb, sb_=sb_: e.activation(out=sl[sb_][:, 0:ncol], in_=PS[pb][:, 0:ncol], func=AF.Silu), r=["PS%d" % pb], w=["sl%d" % sb_])
                    R.op("dve", lambda e, pb=pb, sb_=sb_, fc=fc: e.tensor_tensor(out=hT[hb][:, fc, 0:ncol], in0=PS[pb + 1][:, 0:ncol], in1=sl[sb_][:, 0:ncol], op=ALU.mult),
                         r=["PS%d" % (pb + 1), "sl%d" % sb_], w=["hT%d" % hb])

            def m_w(e_, tg, hb):
                wb = e_ % 2
                ntl = min(4, nt - tg * 4)
                for ti in range(ntl):
                    t = tg * 4 + ti
                    for hf in range(2):
                        ob = 4 + ((t * 2 + hf) % 4)
                        for fc in range(4):
                            R.op("pe", lambda e, fc=fc, ti=ti, hf=hf, ob=ob: e.matmul(PS[ob][:], lhsT=hT[hb][:, fc, ti * 128:(ti + 1) * 128], rhs=w2s[wb][:, fc, hf * 512:(hf + 1) * 512],
                                                                                   start=(fc == 0), stop=(fc == 3)), r=["hT%d" % hb, "w2s%d" % wb], w=["PS%d" % ob])
                        ysl = yacc[:, t, hf * 512:(hf + 1) * 512]
                        if e_ == 0:
                            R.op("dve", lambda e, ob=ob, ysl=ysl, t=t: e.tensor_scalar(out=ysl, in0=PS[ob][:], scalar1=comb[:, t, e_:e_ + 1], scalar2=None, op0=ALU.mult),
                                 r=["PS%d" % ob], w=["y%d_%d" % (t, hf)])
                        else:
                            R.op("dve", lambda e, ob=ob, ysl=ysl, t=t: e.scalar_tensor_tensor(out=ysl, in0=PS[ob][:], scalar=comb[:, t, e_:e_ + 1], in1=ysl, op0=ALU.mult, op1=ALU.add),
                                 r=["PS%d" % ob, "y%d_%d" % (t, hf)], w=["y%d_%d" % (t, hf)])

            mjobs = [(e_, tg) for e_ in range(ne) for tg in range(TG)]
            m_h(*mjobs[0], 0)
            for ji, job in enumerate(mjobs):
                if ji + 1 < len(mjobs):
                    m_h(*mjobs[ji + 1], (ji + 1) % 2)
                m_w(*job, ji % 2)
            R.final_wait = list(R.dma_count.keys())
            if "2" in phases:
                R.emit()
        nc.all_engine_barrier()

        R = Rec(nc)
        with ExitStack() as s2:
            sb = lambda n, s, d=F32: s2.enter_context(nc.sbuf_tensor(pfx + n, s, d))
            xt = [sb("f_xt%d" % i, [128, D]) for i in range(2)]
            xn = [sb("f_xn%d" % i, [128, D]) for i in range(2)]
            tmp = sb("f_tmp", [128, D]); junk = sb("f_junk", [128, D], BF16); ss = sb("f_ss", [128, 1]); rt = sb("f_rt", [128, 1]); rstd = sb("f_rstd", [128, 1])
            for t in range(nt):
                b = t % 2
                R.dma("sp", xt[b][:], x.ap()[t * 128:(t + 1) * 128, :], "x%d" % b, w=["xt%d" % b])
                R.op("dve", lambda e, t=t: e.tensor_tensor(out=tmp[:], in0=yacc[:, t, :], in1=modbc[:, 2 * D:3 * D], op=ALU.mult), r=[], w=["tmp"])
                R.op("dve", lambda e, b=b: e.tensor_tensor(out=xn[b][:], in0=tmp[:], in1=xt[b][:], op=ALU.add), r=["tmp", "xt%d" % b], w=["xn%d" % b])
                if final:
                    R.op("act", lambda e, b=b: e.activation(out=junk[:], in_=xn[b][:], func=AF.Square, accum_out=ss[:]), r=["xn%d" % b], w=["junk", "ss"])
                    R.op("act", lambda e: e.activation(out=rt[:], in_=ss[:], func=AF.Sqrt, scale=1.0 / D, bias=cst[:, 0:1]), r=["ss"], w=["rt"])
                    R.op("dve", lambda e: e.reciprocal(out=rstd[:], in_=rt[:]), r=["rt"], w=["rstd"])
                    R.op("dve", lambda e, b=b: e.scalar_tensor_tensor(out=xn[b][:], in0=xn[b][:], scalar=rstd[:, 0:1], in1=fgb[:], op0=ALU.mult, op1=ALU.mult),
                         r=["xn%d" % b, "rstd"], w=["xn%d" % b])
                R.dma("sp", xo.ap()[t * 128:(t + 1) * 128, :], xn[b][:], "o%d" % b, r=["xn%d" % b])
            R.final_wait = list(R.dma_count.keys())
            if "3" in phases:
                R.emit()


class H:
    def __init__(self, ap):
        self._ap = ap

    def ap(self):
        return self._ap


RBW = 2688
RG = [[0, 1, 2, 3], [4, 5, 6, 7]]


def build_fused(nt=16, stop=None):
    tok = nt * 128
    nc = bass.Bass("TRN2", target_bir_lowering=False)
    di = lambda n, s, d=F32: nc.dram_tensor(n, s, d, kind="ExternalInput")
    dn = lambda n, s, d=BF16: nc.dram_tensor(n, s, d)
    E = {}
    E["x"] = di("x", [tok, D]); E["pos"] = di("pos", [128, nt], I32); E["c"] = di("c", [128, 8])
    E["zmT"] = di("zmT", [128, 512], BF16); E["zq"] = di("zq", [128, 512]); E["cmaskT"] = di("cmaskT", [128, 1024])
    E["rbcore"] = di("rbcore", [2, 8, RBW])
    E["w_mod"] = di("w_mod", [2, D, 6144]); E["b_mod"] = di("b_mod", [2, 1, 6144])
    E["norm1_g"] = di("norm1_g", [2, 1, D]); E["norm2_g"] = di("norm2_g", [2, 1, D]); E["w_in"] = di("w_in", [2, D, INC])
    E["lam4"] = di("lam4", [2, 1, 256]); E["a_norm_g"] = di("a_norm_g", [2, 1, 128]); E["laminit"] = di("laminit", [2, 1, 2])
    E["wa"] = di("wa", [2, 512, D]); E["wb"] = di("wb", [2, 512, D]); E["wc"] = di("wc", [2, 512, D]); E["w_out"] = di("w_out", [2, D, D])
    E["router_w"] = di("router_w", [D, 16]); E["router_b"] = di("router_b", [1, 16])
    E["w1"] = di("w1", [2, NE, D, FF]); E["w3"] = di("w3", [2, NE, D, FF]); E["w2"] = di("w2", [2, NE, FF, D])
    E["final_g"] = di("final_g", [1, D]); E["ident"] = di("ident", [128, 128]); E["aident"] = di("aident", [128, 128])
    E["ropeinv"] = di("ropeinv", [1, 32]); E["p2tab"] = di("p2tab", [1, NIT])
    out = nc.dram_tensor("out", [tok, D], F32, kind="ExternalOutput")
    xin = E["x"]
    for layer in range(2):
        L = "L%d_" % layer
        aqt = dn(L + "aqt", [4, 64, 2 * tok]); bq_iq = dn(L + "bq_iq", [nt, 64, 1536]); iw = dn(L + "iw", [tok, 4], F32)
        cqt = dn(L + "cqt", [64, 8 * tok]); gates = dn(L + "gates", [tok, 3072], F32)
        akt_l = dn(L + "akt_l", [256, 2 * tok]); av_l = dn(L + "av_l", [tok, 516]); bkik_l = dn(L + "bkik_l", [64, 2 * tok])
        bv_l = dn(L + "bv_l", [tok, 65]); ck_l = dn(L + "ck_l", [64, 8 * tok]); cv_l = dn(L + "cv_l", [tok, 520])
        SL = min(4, nt); NCH = nt // SL
        akt_g = [dn(L + "akt_g%d" % i, [4 * 128, 2 * tok]) for i in range(2)]
        av_g = [dn(L + "av_g%d" % i, [4 * SL * 128, 516]) for i in range(NCH)]
        bkik_g = dn(L + "bkik_g", [4 * 64, 2 * tok]); bv_g = dn(L + "bv_g", [4 * tok, 65])
        ck_g = [dn(L + "ck_g%d" % i, [4 * 32, 8 * tok]) for i in range(2)]
        cv_g = [dn(L + "cv_g%d" % i, [4 * SL * 128, 520]) for i in range(NCH)]
        xmid = dn(L + "xmid", [tok, D], F32); xnext = dn(L + "xnext", [tok, D], F32) if layer == 0 else out
        wmod = H(E["w_mod"].ap()[layer]); bmod = H(E["b_mod"].ap()[layer])
        TP = {"x": xin, "pos": E["pos"], "c": E["c"], "w_mod": wmod, "b_mod": bmod, "norm_g": H(E["norm1_g"].ap()[layer]),
              "w_in": H(E["w_in"].ap()[layer]), "ident": E["ident"], "ropeinv": E["ropeinv"],
              "aqt": aqt, "akt": akt_l, "av": av_l, "bqt": bq_iq, "bkt": bkik_l, "bv": bv_l, "iw": iw,
              "cqt": cqt, "ckt": ck_l, "cv": cv_l, "gates": gates}
        emit_P(nc, TP, L + "P_", nt)
        nc.all_engine_barrier()
        if stop == "P":
            return nc
        pairs = [(bkik_l.ap(), bkik_g.ap()), (bv_l.ap(), bv_g.ap())]
        for i in range(2):
            pairs.append((akt_l.ap()[i * 128:(i + 1) * 128, :], akt_g[i].ap()))
            pairs.append((ck_l.ap()[i * 32:(i + 1) * 32, :], ck_g[i].ap()))
        for i in range(NCH):
            pairs.append((av_l.ap()[i * SL * 128:(i + 1) * SL * 128, :], av_g[i].ap()))
            pairs.append((cv_l.ap()[i * SL * 128:(i + 1) * SL * 128, :], cv_g[i].ap()))
        ccs = nc.alloc_semaphore(name=L + "ccs")
        with nc.Block() as blk:
            @blk.gpsimd
            def _(g):
                for (lo_, ga_) in pairs:
                    g.collective_compute("AllGather", mybir.AluOpType.bypass, replica_groups=RG,
                                         ins=[lo_], outs=[ga_]).then_inc(ccs, 1)
                g.wait_ge(ccs, len(pairs))
        nc.all_engine_barrier()
        nc.clear_and_free_semaphores([ccs])
        nc.all_engine_barrier()
        if stop == "AG":
            return nc
        TT = {"aqt": aqt, "bq_iq": bq_iq, "iw": iw, "cqt": cqt, "gates": gates, "x": xin,
              "akt": akt_g, "av": av_g, "bkik": bkik_g, "bv": bv_g, "ckb": ck_g, "cvb": cv_g,
              "zmT": E["zmT"], "zq": E["zq"], "cmaskT": E["cmaskT"],
              "wa": H(E["wa"].ap()[layer]), "wb": H(E["wb"].ap()[layer]), "wc": H(E["wc"].ap()[layer]), "w_out": H(E["w_out"].ap()[layer]),
              "w_mod": wmod, "b_mod": bmod, "c": E["c"], "lam4": H(E["lam4"].ap()[layer]), "a_norm_g": H(E["a_norm_g"].ap()[layer]),
              "rbext": E["rbcore"], "rb_off": layer * 8 * RBW, "rb_w": RBW, "ident": E["ident"], "aident": E["aident"],
              "laminit": H(E["laminit"].ap()[layer]), "p2tab": E["p2tab"], "xo": xmid}
        emit_T(nc, TT, L + "T_", nt, phases=(stop[1:] if (stop or "").startswith("T") else "0ABCM"))
        nc.all_engine_barrier()
        if (stop or "").startswith("T"):
            return nc
        TM = {"x": xmid, "c": E["c"], "w_mod": wmod, "b_mod": bmod, "norm_g": H(E["norm2_g"].ap()[layer]),
              "router_w": E["router_w"], "router_b": E["router_b"], "w1": H(E["w1"].ap()[layer]), "w3": H(E["w3"].ap()[layer]),
              "w2": H(E["w2"].ap()[layer]), "ident": E["ident"], "final_g": E["final_g"], "xo": xnext}
        emit_M(nc, TM, L + "M_", nt, final=(layer == 1))
        nc.all_engine_barrier()
        xin = xnext
    return nc


BF = ml_dtypes.bfloat16
_CACHE = {}


def _core_rows(r, nt=16):
    return np.concatenate([np.arange((4 * t + r) * 128, (4 * t + r + 1) * 128) for t in range(nt)])


def _masks(r):
    k = np.arange(128)[:, None]
    q = np.arange(128)[None, :]
    zmT = np.zeros((128, 4, 128), np.float32)
    zq = np.zeros((128, 4, 128), np.float32)
    for j in range(4):
        if j == r:
            m = (k >= 64) & (q < 64)
        elif j > r:
            m = np.ones((128, 128), bool)
        else:
            m = np.zeros((128, 128), bool)
        zmT[:, j, :] = np.where(m, -BIG, 0.0)
        zq[:, j, :] = np.where(m.T, -1e30, 0.0)
    cm = np.full((128, 8, 128), -BIG, np.float32)
    for i in range(8):
        j = i - r
        if 1 <= j <= 3:
            cm[:, i, :] = 0.0
        elif j == 0:
            cm[:, i, :] = np.where((k < 64) & (q >= 64), -BIG, 0.0)
        elif j == 4:
            cm[:, i, :] = np.where((k >= 64) & (q < 64), -BIG, 0.0)
    return zmT.reshape(128, 512).astype(BF), zq.reshape(128, 512), np.ascontiguousarray(cm[::-1]).reshape(128, 1024)


def kernel(x, c, positions, norm1_g, norm2_g, w_mod, b_mod, w_in, lambda_q1, lambda_k1, lambda_q2, lambda_k2,
           a_norm_g, c_rel_bias, w_branch_a, w_branch_b, w_branch_c, w_out, router_w, router_b,
           exp_w1, exp_w3, exp_w2, final_g, _nt=16, _runner=None, _stop=None):
    f32 = np.float32
    nt = _nt
    A = lambda a: np.ascontiguousarray(np.asarray(a, f32))
    x = A(x); c = A(c); positions = np.asarray(positions, np.int32)
    cores = [(b, r) for b in range(2) for r in range(4)]
    rows = [_core_rows(r, nt) for r in range(4)]
    ident = np.eye(128, dtype=f32)
    lam_init = [0.8 - 0.6 * math.exp(-0.3 * l) for l in range(2)]
    rb = A(c_rel_bias)
    rbext = np.concatenate([rb, np.repeat(rb[:, :, 512:513], 511, axis=2)], axis=2)
    rbbig = np.zeros((2, 8, 3072), f32); rbbig[:, :, 1024:2048] = rbext
    shared = {
        "w_mod": A(w_mod), "b_mod": A(b_mod)[:, None, :], "norm1_g": A(norm1_g)[:, None, :], "norm2_g": A(norm2_g)[:, None, :],
        "w_in": A(w_in), "lam4": np.concatenate([A(lambda_q1), A(lambda_k1), A(lambda_q2), A(lambda_k2)], axis=1)[:, None, :],
        "a_norm_g": A(a_norm_g)[:, None, :], "laminit": np.array([[[l, 1.0 - l]] for l in lam_init], f32),
        "wa": A(w_branch_a), "wb": A(w_branch_b), "wc": A(w_branch_c), "w_out": A(w_out),
        "router_w": A(router_w), "router_b": A(router_b)[None, :], "w1": A(exp_w1), "w3": A(exp_w3), "w2": A(exp_w2),
        "final_g": A(final_g)[None, :], "ident": ident, "aident": np.ascontiguousarray(ident[::-1]),
        "ropeinv": (np.float32(10000.0) ** (-np.arange(32, dtype=f32) / np.float32(32))).astype(f32)[None, :],
        "p2tab": (2.0 ** -(np.arange(NIT) + 1.0))[None, :].astype(f32),
    }
    shared = {k_: np.ascontiguousarray(v) for k_, v in shared.items()}
    ims = []
    for (b, r) in cores:
        zmT, zq, cm = _masks(r)
        im = dict(shared)
        im.update({"x": np.ascontiguousarray(x[b][rows[r]]), "pos": np.ascontiguousarray(positions[b][rows[r]].reshape(nt, 128).T),
                   "c": np.ascontiguousarray(c[b].reshape(8, 128).T), "zmT": zmT, "zq": np.ascontiguousarray(zq), "cmaskT": cm,
                   "rbcore": np.ascontiguousarray(rbbig[:, :, 128 * r:128 * r + RBW])})
        ims.append(im)
    if ("F", nt) not in _CACHE:
        _CACHE[("F", nt)] = build_fused(nt, stop=_stop)
    if _runner is None:
        results = run_bass_kernel_spmd(_CACHE[("F", nt)], ims, core_ids=list(range(8))).results
    else:
        results = _runner(_CACHE[("F", nt)], ims)
    out = np.zeros((2, 512 * nt, 1024), f32)
    for ci, (b, r) in enumerate(cores):
        out[b][rows[r]] = np.asarray(results[ci]["out"], f32)
    return out
```

```python
import math
from contextlib import ExitStack
import numpy as np
import ml_dtypes
import concourse.bass as bass
import concourse.mybir as mybir
from concourse.bass_utils import run_bass_kernel_spmd

F32 = mybir.dt.float32
BF16 = mybir.dt.bfloat16
I32 = mybir.dt.int32
ALU = mybir.AluOpType
AF = mybir.ActivationFunctionType
AX = mybir.AxisListType

ENGS = ("pe", "act", "dve", "pool", "sp")


class Op:
    __slots__ = ("eng", "fn", "deps", "is_dma", "sem", "ticket", "needs_inc", "idx")

    def __init__(self, eng, fn, is_dma, sem):
        self.eng = eng
        self.fn = fn
        self.deps = []
        self.is_dma = is_dma
        self.sem = sem
        self.ticket = None
        self.needs_inc = False


class Rec:
    def __init__(self, nc):
        self.nc = nc
        self.streams = {e: [] for e in ENGS}
        self.last_w = {}
        self.readers = {}
        self.dma_count = {}
        self.all_ops = []

    def _add(self, op, r, w):
        deps = []
        for k in r:
            lw = self.last_w.get(k)
            if lw is not None:
                deps.append(lw)
        for k in w:
            lw = self.last_w.get(k)
            if lw is not None:
                deps.append(lw)
            deps.extend(self.readers.get(k, ()))
        seen = set()
        for d in deps:
            if d is op or id(d) in seen:
                continue
            seen.add(id(d))
            if d.eng == "pe" and op.eng == "pe" and not d.is_dma and not op.is_dma:
                continue
            op.deps.append(d)
            d.needs_inc = True
        for k in w:
            self.last_w[k] = op
            self.readers[k] = []
        for k in r:
            self.readers.setdefault(k, []).append(op)
        self.streams[op.eng].append(op)
        self.all_ops.append(op)
        return op

    def op(self, eng, fn, r=(), w=()):
        return self._add(Op(eng, fn, False, None), r, w)

    def dma(self, eng, out, in_, sem, r=(), w=()):
        o = Op(eng, lambda e: e.dma_start(out=out, in_=in_), True, sem)
        self.dma_count[sem] = self.dma_count.get(sem, 0) + 1
        o.ticket = self.dma_count[sem]
        return self._add(o, r, w)

    def emit(self):
        nc = self.nc
        cnt = {e: 0 for e in ENGS}
        for e in ENGS:
            for o in self.streams[e]:
                if o.is_dma:
                    continue
                if o.needs_inc:
                    cnt[e] += 1
                    o.ticket = cnt[e]
        order = {id(o): i for i, o in enumerate(self.all_ops)}
        dma_hist = {}
        for i, o in enumerate(self.all_ops):
            if o.is_dma:
                dma_hist.setdefault(o.sem, []).append((i, o.ticket))
        import bisect
        dma_keys = sorted(self.dma_count.keys(), key=str)
        from contextlib import ExitStack
        esem = {e: nc.alloc_semaphore(name=nc.make_name("s_" + e, add_next_id=True)) for e in ENGS}
        dsem = {k: nc.alloc_semaphore(name=nc.make_name("d_%d" % i, add_next_id=True)) for i, k in enumerate(dma_keys)}
        with ExitStack() as st:
            block = st.enter_context(nc.Block())

            def run_stream(ename):
                def body(e):
                    waited = {}
                    for o in self.streams[ename]:
                        me = order[id(o)]
                        for d in o.deps:
                            if d.is_dma:
                                hist = dma_hist[d.sem]
                                j = bisect.bisect_left(hist, (me, 0)) - 1
                                val = 16 * hist[j][1]
                                sem = dsem[d.sem]
                                key = ("d", d.sem)
                            else:
                                val = d.ticket
                                sem = esem[d.eng]
                                key = ("e", d.eng)
                            if waited.get(key, 0) >= val:
                                continue
                            waited[key] = val
                            e.wait_ge(sem, val)
                        ins = o.fn(e)
                        if o.is_dma:
                            ins.then_inc(dsem[o.sem], 16)
                        elif o.needs_inc:
                            ins.then_inc(esem[ename], 1)
                    if ename == "sp":
                        for k in getattr(self, "final_wait", ()):
                            e.wait_ge(dsem[k], 16 * self.dma_count[k])
                return body

            block.tensor(run_stream("pe"))
            block.scalar(run_stream("act"))
            block.vector(run_stream("dve"))
            block.gpsimd(run_stream("pool"))
            block.sync(run_stream("sp"))
        nc.all_engine_barrier()
        nc.clear_and_free_semaphores(list(esem.values()) + list(dsem.values()))
        nc.all_engine_barrier()


def dram_ap(t, offset, pattern):
    return bass.AP(t, offset, pattern)


D = 1024
NT = 16
TOK = NT * 128
INC = 7108
C_AQ, C_AK, C_AV, C_BQ, C_BK, C_BV, C_IQ, C_IK, C_IW, C_CQ, C_CK, C_CV, C_G = (
    0, 512, 1024, 1536, 2048, 2112, 2176, 2432, 2496, 2500, 3012, 3524, 4036)
EPS = 1e-6
TWO_PI = 2.0 * math.pi


def emit_P(nc, T, pfx, nt=NT):
    tok = nt * 128
    di = lambda n, s, d=F32: T[n]
    do = lambda n, s, d=BF16: T[n]
    x = di("x", [tok, D]); pos = di("pos", [128, nt], I32); cvec = di("c", [128, 8])
    w_mod = di("w_mod", [D, 6144]); b_mod = di("b_mod", [1, 6144]); norm_g = di("norm_g", [1, D])
    w_in = di("w_in", [D, INC]); ident = di("ident", [128, 128]); ropeinv = di("ropeinv", [1, 32])
    aqt = do("aqt", [4, 128, tok]); akt = do("akt", [4, 128, tok]); av = do("av", [tok, 516])
    bqt = do("bqt", [nt, 64, 1024]); bkt = do("bkt", [64, tok]); bv = do("bv", [tok, 65])
    iw = do("iw", [tok, 4], F32)
    cqt = do("cqt", [4, 128, tok]); ckt = do("ckt", [4, 128, tok]); cv = do("cv", [tok, 520])
    gates = do("gates", [tok, 3072], F32)

    R = Rec(nc)
    with ExitStack() as st, nc.allow_low_precision("bf16 matmul operands, fp32 accumulation"):
        sb = lambda n, s, d=F32: st.enter_context(nc.sbuf_tensor(pfx + n, s, d))
        ps = lambda n, s, d=F32: st.enter_context(nc.psum_tensor(pfx + n, s, d))
        cs = sb("cs", [128, 8]); ca = sb("ca", [128, 8]); CA = sb("CA", [128, 8, 128])
        modbc = sb("modbc", [128, 2048]); gs = sb("gs", [128, D])
        wsb = sb("wsb", [128, 8, INC], BF16)
        idf = sb("idf", [128, 128]); idb = sb("idb", [128, 128], BF16)
        inv = sb("inv", [128, 32]); posi = sb("posi", [128, nt], I32); posf = sb("posf", [128, nt])
        xt = [sb("xt%d" % i, [128, D]) for i in range(2)]
        junk = sb("junk", [128, D], BF16); ss = sb("ss", [128, 1]); rstd = sb("rstd", [128, 1]); rt = sb("rt", [128, 1])
        tmp = sb("tmp", [128, D]); ub = sb("ub", [128, D], BF16); uT = sb("uT", [128, D], BF16)
        pj = sb("pj", [128, INC])
        ang = sb("ang", [128, nt, 32]); ang2 = sb("ang2", [128, nt, 32]); kf = sb("kf", [128, nt, 32]); ki = sb("ki", [128, nt, 32], I32)
        SN = sb("SN", [128, nt, 32]); CN = sb("CN", [128, nt, 32])
        t1 = sb("t1", [128, 512]); t2 = sb("t2", [128, 512])
        rb = sb("rb", [128, C_CV], BF16)
        avb = sb("avb", [128, 4, 129], BF16); bvb = sb("bvb", [128, 65], BF16); cvb = sb("cvb", [128, 8, 65], BF16)
        iwb = sb("iwb", [128, 4]); cst = sb("cst", [128, 2])
        wm = [pj[:, b * 2048:(b + 1) * 2048].rearrange("p (ch n) -> p ch n", n=256) for b in range(2)]
        WMK = [["pj%d" % i for i in range(4)], ["pj%d" % i for i in range(4, 8)]]
        bmb = pj[:, 4096:6144]; BMK = ["pj%d" % i for i in range(8, 12)]
        gbc = tmp
        tA = [sb("tA%d" % i, [128, 1024], BF16) for i in range(2)]
        pT = ps("pT", [128, 1024], BF16)
        pp = [ps("pp%d" % i, [128, 512]) for i in range(2)]
        pX = [ps("pX%d" % i, [128, 1024], BF16) for i in range(2)]

        R.dma("sp", cs[:], cvec.ap(), "c", w=["cs"])
        R.op("act", lambda e: e.activation(out=ca[:], in_=cs[:], func=AF.Silu), r=["cs"], w=["ca"])
        R.op("dve", lambda e: e.tensor_copy(out=CA[:], in_=ca[:].unsqueeze(2).broadcast_to([128, 8, 128])), r=["ca"], w=["CA"])
        R.dma("sp", bmb, b_mod.ap()[0:1, 0:2048].partition_broadcast(128), "c", w=BMK)
        R.dma("sp", gbc[:], norm_g.ap()[0:1, :].partition_broadcast(128), "c", w=["tmp"])
        R.dma("sp", idf[:], ident.ap(), "c", w=["idf"])
        R.dma("sp", inv[:], ropeinv.ap()[0:1, :].partition_broadcast(128), "c", w=["inv"])
        R.dma("sp", posi[:], pos.ap(), "c", w=["posi"])
        R.op("dve", lambda e: e.tensor_copy(out=idb[:], in_=idf[:]), r=["idf"], w=["idb"])
        R.op("dve", lambda e: e.tensor_copy(out=posf[:], in_=posi[:]), r=["posi"], w=["posf"])
        R.op("pool", lambda e: e.memset(cst[:, 0:1], EPS), w=["cst"])
        R.op("pool", lambda e: e.memset(cst[:, 1:2], math.pi), w=["cst"])
        R.op("pool", lambda e: e.memset(avb[:], 1.0), w=["avb"])
        R.op("pool", lambda e: e.memset(bvb[:], 1.0), w=["bvb"])
        R.op("pool", lambda e: e.memset(cvb[:], 1.0), w=["cvb"])
        R.op("dve", lambda e: e.tensor_tensor(out=ang[:], in0=inv[:].unsqueeze(1).broadcast_to([128, nt, 32]),
                                              in1=posf[:].unsqueeze(2).broadcast_to([128, nt, 32]), op=ALU.mult), r=["inv", "posf"], w=["ang"])
        R.op("dve", lambda e: e.tensor_scalar(out=ang2[:], in0=ang[:], scalar1=math.pi / 2, scalar2=None, op0=ALU.add), r=["ang"], w=["ang2"])
        for (src, dst, nm) in ((ang, SN, "SN"), (ang2, CN, "CN")):
            sk = "ang" if src is ang else "ang2"
            R.op("dve", lambda e, src=src: e.tensor_scalar(out=ki[:], in0=src[:], scalar1=1.0 / TWO_PI, scalar2=None, op0=ALU.mult), r=[sk], w=["ki"])
            R.op("dve", lambda e: e.tensor_copy(out=kf[:], in_=ki[:]), r=["ki"], w=["kf"])
            R.op("dve", lambda e, src=src: e.scalar_tensor_tensor(out=kf[:], in0=kf[:], scalar=-TWO_PI, in1=src[:], op0=ALU.mult, op1=ALU.add),
                 r=["kf", sk], w=["kf"])
            R.op("dve", lambda e: e.tensor_scalar(out=kf[:], in0=kf[:], scalar1=3.14159, scalar2=-3.14159, op0=ALU.min, op1=ALU.max), r=["kf"], w=["kf"])
            R.op("act", lambda e, dst=dst: e.activation(out=dst[:], in_=kf[:], func=AF.Sin), r=["kf"], w=[nm])
        wmv = w_mod.ap().rearrange("(ch p) n -> p ch n", p=128)
        for j in range(8):
            b = j % 2
            R.dma("sp", wm[b], wmv[:, :, j * 256:(j + 1) * 256], "wm%d" % b, w=WMK[b])
            for ch in range(8):
                R.op("pe", lambda e, ch=ch, b=b: e.matmul(pp[b][:, 0:256], lhsT=CA[:, ch, :], rhs=wm[b][:, ch, :],
                                                          start=(ch == 0), stop=(ch == 7)),
                     r=["CA"] + WMK[b], w=["pp%d" % b])
            R.op("dve", lambda e, j=j, b=b: e.tensor_tensor(out=modbc[:, j * 256:(j + 1) * 256], in0=pp[b][:, 0:256],
                                                            in1=bmb[:, j * 256:(j + 1) * 256], op=ALU.add),
                 r=["pp%d" % b] + BMK, w=["modbc"])
        R.op("dve", lambda e: e.scalar_tensor_tensor(out=gs[:], in0=modbc[:, 1024:2048], scalar=1.0, in1=gbc[:],
                                                     op0=ALU.add, op1=ALU.mult), r=["modbc", "tmp"], w=["gs"])
        wiv = w_in.ap().rearrange("(ch p) n -> p ch n", p=128)
        NCHK = (INC + 511) // 512
        for n_ in range(NCHK):
            c0_, c1_ = n_ * 512, min(INC, (n_ + 1) * 512)
            R.dma("pool", wsb[:, :, c0_:c1_], wiv[:, :, c0_:c1_], "wsb", w=["wsbn%d" % n_])

        for t in range(nt):
            xb = t % 2
            xk = "xt%d" % xb
            R.dma("sp", xt[xb][:], x.ap()[t * 128:(t + 1) * 128, :], xk, w=[xk])
            R.op("act", lambda e, xb=xb: e.activation(out=junk[:], in_=xt[xb][:], func=AF.Square, accum_out=ss[:]),
                 r=[xk], w=["junk", "ss"])
            R.op("act", lambda e: e.activation(out=rt[:], in_=ss[:], func=AF.Sqrt, scale=1.0 / D, bias=cst[:, 0:1]),
                 r=["ss", "cst"], w=["rt"])
            R.op("dve", lambda e: e.reciprocal(out=rstd[:], in_=rt[:]), r=["rt"], w=["rstd"])
            R.op("dve", lambda e, xb=xb: e.scalar_tensor_tensor(out=tmp[:], in0=xt[xb][:], scalar=rstd[:, 0:1], in1=gs[:],
                                                                op0=ALU.mult, op1=ALU.mult), r=[xk, "rstd", "gs"], w=["tmp"])
            R.op("dve", lambda e: e.tensor_tensor(out=ub[:], in0=tmp[:], in1=modbc[:, 0:1024], op=ALU.add),
                 r=["tmp", "modbc"], w=["ub"])
            for ch in range(8):
                R.op("pe", lambda e, ch=ch: e.transpose(out=pT[:, ch * 128:(ch + 1) * 128], in_=ub[:, ch * 128:(ch + 1) * 128],
                                                        identity=idb[:]), r=["ub", "idb"], w=["pT"])
            R.op("act", lambda e: e.copy(out=uT[:], in_=pT[:]), r=["pT"], w=["uT"])
            nchunks = (INC + 511) // 512
            for n in range(nchunks):
                n0, n1 = n * 512, min(INC, (n + 1) * 512)
                b = n % 2
                for ch in range(8):
                    R.op("pe", lambda e, ch=ch, b=b, n0=n0, n1=n1: e.matmul(pp[b][:, 0:n1 - n0], lhsT=uT[:, ch * 128:(ch + 1) * 128],
                                                                              rhs=wsb[:, ch, n0:n1], start=(ch == 0), stop=(ch == 7)),
                         r=["uT", "wsbn%d" % n], w=["pp%d" % b])
                eng = "act" if n % 2 == 0 else "dve"
                if eng == "act":
                    R.op("act", lambda e, b=b, n0=n0, n1=n1: e.copy(out=pj[:, n0:n1], in_=pp[b][:, 0:n1 - n0]),
                         r=["pp%d" % b], w=["pj%d" % n])
                else:
                    R.op("dve", lambda e, b=b, n0=n0, n1=n1: e.tensor_copy(out=pj[:, n0:n1], in_=pp[b][:, 0:n1 - n0]),
                         r=["pp%d" % b], w=["pj%d" % n])
            PJ = lambda c0, c1: ["pj%d" % n for n in range(c0 // 512, (c1 - 1) // 512 + 1)]
            for (c0, H) in ((C_AQ, 16), (C_BQ, 9), (C_IQ, 5)):
                c1 = c0 + 64 * H
                xv = pj[:, c0:c1].rearrange("p (h two d) -> p h two d", two=2, d=32)
                ov = rb[:, c0:c1].rearrange("p (h two d) -> p h two d", two=2, d=32)
                x1, x2 = xv[:, :, 0, :], xv[:, :, 1, :]
                o1, o2 = ov[:, :, 0, :], ov[:, :, 1, :]
                cb = CN[:, t:t + 1, :].broadcast_to([128, H, 32])
                sbv = SN[:, t:t + 1, :].broadcast_to([128, H, 32])
                a1 = t1[:, 0:32 * H].rearrange("p (h d) -> p h d", d=32)
                a2 = t2[:, 0:32 * H].rearrange("p (h d) -> p h d", d=32)
                rk = PJ(c0, c1)
                R.op("dve", lambda e, a1=a1, x1=x1, cb=cb: e.tensor_tensor(out=a1, in0=x1, in1=cb, op=ALU.mult), r=rk + ["CN"], w=["t1"])
                R.op("dve", lambda e, a2=a2, x2=x2, sbv=sbv: e.tensor_tensor(out=a2, in0=x2, in1=sbv, op=ALU.mult), r=rk + ["SN"], w=["t2"])
                R.op("dve", lambda e, o1=o1, a1=a1, a2=a2: e.tensor_tensor(out=o1, in0=a1, in1=a2, op=ALU.subtract), r=["t1", "t2"], w=["rb"])
                R.op("dve", lambda e, a1=a1, x2=x2, cb=cb: e.tensor_tensor(out=a1, in0=x2, in1=cb, op=ALU.mult), r=rk + ["CN"], w=["t1"])
                R.op("dve", lambda e, a2=a2, x1=x1, sbv=sbv: e.tensor_tensor(out=a2, in0=x1, in1=sbv, op=ALU.mult), r=rk + ["SN"], w=["t2"])
                R.op("dve", lambda e, o2=o2, a1=a1, a2=a2: e.tensor_tensor(out=o2, in0=a1, in1=a2, op=ALU.add), r=["t1", "t2"], w=["rb"])
            R.op("pool", lambda e: e.tensor_copy(out=rb[:, C_CQ:C_CV], in_=pj[:, C_CQ:C_CV]), r=PJ(C_CQ, C_CV), w=["rb"])
            R.op("pool", lambda e: e.tensor_copy(out=avb[:, :, 0:128], in_=pj[:, C_AV:C_BQ].rearrange("p (h d) -> p h d", d=128)),
                 r=PJ(C_AV, C_BQ), w=["avb"])
            R.op("pool", lambda e: e.tensor_copy(out=bvb[:, 0:64], in_=pj[:, C_BV:C_IQ]), r=PJ(C_BV, C_IQ), w=["bvb"])
            R.op("pool", lambda e: e.tensor_copy(out=cvb[:, :, 0:64], in_=pj[:, C_CV:C_G].rearrange("p (h d) -> p h d", d=64)),
                 r=PJ(C_CV, C_G), w=["cvb"])
            R.op("pool", lambda e: e.tensor_scalar(out=iwb[:], in0=pj[:, C_IW:C_CQ], scalar1=1.0 / 16.0, scalar2=None, op0=ALU.mult),
                 r=PJ(C_IW, C_CQ), w=["iwb"])
            R.op("act", lambda e: e.activation(out=pj[:, C_G:INC], in_=pj[:, C_G:INC], func=AF.Sigmoid), r=PJ(C_G, INC), w=PJ(C_G, INC))
            ts = slice(t * 128, (t + 1) * 128)
            R.dma("sp", av.ap()[ts, :], avb[:].rearrange("p h d -> p (h d)"), "out", r=["avb"])
            R.dma("sp", bv.ap()[ts, :], bvb[:], "out", r=["bvb"])
            R.dma("sp", cv.ap()[ts, :], cvb[:].rearrange("p h d -> p (h d)"), "out", r=["cvb"])
            R.dma("sp", iw.ap()[ts, :], iwb[:], "out", r=["iwb"])
            R.dma("sp", gates.ap()[ts, :], pj[:, C_G:INC], "out", r=PJ(C_G, INC))
            g = 0
            for blk in range(8):
                c0 = blk * 128
                R.op("pe", lambda e, blk=blk, c0=c0: e.transpose(out=pX[0][:, blk * 128:(blk + 1) * 128], in_=rb[:, c0:c0 + 128], identity=idb[:]),
                     r=["rb", "idb"], w=["pX0"])
            R.op("act", lambda e: e.copy(out=tA[0][:], in_=pX[0][:]), r=["pX0"], w=["tA0"])
            for m in range(2):
                R.dma("sp", bass.AP(aqt, m * tok + t * 128, [[2 * tok, 64], [64 * 2 * tok, 4], [1, 128]]),
                      tA[0][m * 64:(m + 1) * 64, 0:512].rearrange("p (h q) -> p h q", q=128), "out", r=["tA0"])
                R.dma("sp", bass.AP(akt, m * tok + t * 128, [[2 * tok, 64], [64 * 2 * tok, 4], [1, 128]]),
                      tA[0][m * 64:(m + 1) * 64, 512:1024].rearrange("p (h q) -> p h q", q=128), "out", r=["tA0"])
            for blk in range(8):
                c0 = C_CQ + blk * 128
                R.op("pe", lambda e, blk=blk, c0=c0: e.transpose(out=pX[1][:, blk * 128:(blk + 1) * 128], in_=rb[:, c0:c0 + 128], identity=idb[:]),
                     r=["rb", "idb"], w=["pX1"])
            R.op("dve", lambda e: e.tensor_copy(out=tA[1][:], in_=pX[1][:]), r=["pX1"], w=["tA1"])
            for hf in range(2):
                R.dma("sp", bass.AP(cqt, hf * tok + t * 128, [[8 * tok, 64], [2 * tok, 4], [1, 128]]),
                      tA[1][hf * 64:(hf + 1) * 64, 0:512].rearrange("p (h q) -> p h q", q=128), "out", r=["tA1"])
                R.dma("sp", bass.AP(ckt, t * 1024 + hf * 128, [[8 * tok, 64], [256, 4], [1, 128]]),
                      tA[1][hf * 64:(hf + 1) * 64, 512:1024].rearrange("p (h q) -> p h q", q=128), "out", r=["tA1"])
            for h in range(8):
                c0 = C_BQ + h * 64
                R.op("pe", lambda e, h=h, c0=c0: e.transpose(out=pX[0][0:64, h * 128:(h + 1) * 128], in_=rb[:, c0:c0 + 64], identity=idb[:]),
                     r=["rb", "idb"], w=["pX0"])
            R.op("act", lambda e: e.copy(out=tA[0][0:64, :], in_=pX[0][0:64, :]), r=["pX0"], w=["tA0"])
            R.dma("sp", bqt.ap()[t][:, 0:1024], tA[0][0:64, :], "out", r=["tA0"])
            srcs = [C_IQ + h * 64 for h in range(4)] + [C_BK, C_IK]
            for i, c0 in enumerate(srcs):
                R.op("pe", lambda e, i=i, c0=c0: e.transpose(out=pX[1][0:64, i * 128:(i + 1) * 128], in_=rb[:, c0:c0 + 64], identity=idb[:]),
                     r=["rb", "idb"], w=["pX1"])
            R.op("dve", lambda e: e.tensor_copy(out=tA[1][0:64, 0:768], in_=pX[1][0:64, 0:768]), r=["pX1"], w=["tA1"])
            R.dma("sp", bqt.ap()[t][:, 1024:1536], tA[1][0:64, 0:512], "out", r=["tA1"])
            R.dma("sp", bkt.ap()[:, t * 128:(t + 1) * 128], tA[1][0:64, 512:640], "out", r=["tA1"])
            R.dma("sp", bkt.ap()[:, tok + t * 128:tok + (t + 1) * 128], tA[1][0:64, 640:768], "out", r=["tA1"])
        R.final_wait = ["out"]
        R.emit()


D = 1024
BIG = 30000.0
NIT = 22
EPS = 1e-6


def emit_T(nc, T, pfx, nt=16, phases="0ABCM"):
    tok = nt * 128
    NKT = 4 * nt
    S = NKT * 128
    di = lambda n, s, d=F32: T[n]
    aqt = di("aqt", [4, 64, 2 * tok], BF16); bq_iq = di("bq_iq", [nt, 64, 1536], BF16); iw = di("iw", [tok, 4])
    cqt = di("cqt", [64, 8 * tok], BF16); gates = di("gates", [tok, 3072]); x = di("x", [tok, D])
    akt = di("akt", [4, 64, 2 * S], BF16); av = di("av", [4, 128, NKT * 129], BF16); bkik = di("bkik", [64, 2 * S], BF16)
    bv = di("bv", [128, NKT * 65], BF16); ckb = di("ckb", [nt, 64, 8 * 640], BF16); cvb = di("cvb", [nt, 128, 5 * 520], BF16)
    zmT = di("zmT", [128, 512], BF16); zq = di("zq", [128, 512]); cmaskT = di("cmaskT", [128, 8 * 128])
    wa = di("wa", [512, D]); wb = di("wb", [512, D]); wc = di("wc", [512, D]); w_out = di("w_out", [D, D])
    w_mod = di("w_mod", [D, 6144]); b_mod = di("b_mod", [1, 6144]); cvec = di("c", [128, 8])
    lam4 = di("lam4", [1, 256]); ang_in = di("a_norm_g", [1, 128]); rbext = di("rbext", [8, 1024])
    ident = di("ident", [128, 128]); aident = di("aident", [128, 128]); laminit = di("laminit", [1, 2]); p2tab = di("p2tab", [1, NIT])
    xo = T["xo"]

    with ExitStack() as st, nc.allow_low_precision("bf16 matmul operands, fp32 accumulation"):
        sbo = lambda n, s, d=F32: st.enter_context(nc.sbuf_tensor(pfx + n, s, d))
        pso = lambda n, s, d=F32: st.enter_context(nc.psum_tensor(pfx + n, s, d))
        ya = sbo("ya", [128, nt, 512], BF16); yb = sbo("yb", [128, nt, 512], BF16); yc = sbo("yc", [128, nt, 512], BF16)
        g1bc = sbo("g1bc", [128, D]); idf = sbo("idf", [128, 128]); idb = sbo("idb", [128, 128], BF16); jdb = sbo("jdb", [128, 128], BF16)
        bigi4 = sbo("bigi4", [128, 512], BF16); nlam = sbo("nlam", [128, 1]); gn = sbo("gn", [128, 128])
        cst = sbo("cst", [128, 2])
        psS = [pso("psS%d" % i, [128, 1024]) for i in range(2)]
        psO = [pso("psO%d" % i, [128, 1024]) for i in range(2)]

        R = Rec(nc)
        with ExitStack() as s0:
            sb = lambda n, s, d=F32: s0.enter_context(nc.sbuf_tensor(pfx + n, s, d))
            cs = sb("cs", [128, 8]); ca = sb("ca", [128, 8]); CA = sb("CA", [128, 8, 128])
            wm = [sb("wm%d" % i, [128, 8, 256]) for i in range(2)]
            bmb = sb("bmb", [128, D]); l4 = sb("l4", [128, 4, 64]); lt = sb("lt", [128, 2, 64]); ls = sb("ls", [128, 2]); le = sb("le", [128, 2])
            li = sb("li", [128, 2]); agb = sb("agb", [128, 128]); stg = sb("stg", [128, 8, 128])
            R.dma("sp", cs[:], cvec.ap(), "c", w=["cs"])
            R.dma("sp", idf[:], ident.ap(), "c", w=["idf"])
            R.dma("sp", bmb[:], b_mod.ap()[0:1, 2048:3072].partition_broadcast(128), "c", w=["bmb"])
            R.dma("sp", l4[:].rearrange("p a b -> p (a b)"), lam4.ap()[0:1, :].partition_broadcast(128), "c", w=["l4"])
            R.dma("sp", li[:], laminit.ap()[0:1, :].partition_broadcast(128), "c", w=["li"])
            R.dma("sp", agb[:], ang_in.ap()[0:1, :].partition_broadcast(128), "c", w=["agb"])
            R.op("pool", lambda e: e.memset(cst[:, 0:1], EPS), w=["cst"])
            R.op("act", lambda e: e.activation(out=ca[:], in_=cs[:], func=AF.Silu), r=["cs"], w=["ca"])
            R.op("dve", lambda e: e.tensor_copy(out=CA[:], in_=ca[:].unsqueeze(2).broadcast_to([128, 8, 128])), r=["ca"], w=["CA"])
            R.op("dve", lambda e: e.tensor_copy(out=idb[:], in_=idf[:]), r=["idf"], w=["idb"])
            R.dma("sp", stg[:, 0, :], aident.ap(), "c", w=["stg"])
            R.op("dve", lambda e: e.tensor_copy(out=jdb[:], in_=stg[:, 0, :]), r=["stg"], w=["jdb"])
            for k in range(4):
                R.op("dve", lambda e, k=k: e.tensor_scalar(out=bigi4[:, k * 128:(k + 1) * 128], in0=idf[:], scalar1=BIG, scalar2=None, op0=ALU.mult),
                     r=["idf"], w=["bigi4"])
            wmv = w_mod.ap().rearrange("(ch p) n -> p ch n", p=128)
            for j in range(4):
                b = j % 2
                R.dma("sp", wm[b][:], wmv[:, :, 2048 + j * 256:2048 + (j + 1) * 256], "wm%d" % b, w=["wm%d" % b])
                for ch in range(8):
                    R.op("pe", lambda e, ch=ch, b=b: e.matmul(psS[b][:, 0:256], lhsT=CA[:, ch, :], rhs=wm[b][:, ch, :], start=(ch == 0), stop=(ch == 7)),
                         r=["CA", "wm%d" % b], w=["psS%d" % b])
                R.op("dve", lambda e, j=j, b=b: e.tensor_tensor(out=g1bc[:, j * 256:(j + 1) * 256], in0=psS[b][:, 0:256], in1=bmb[:, j * 256:(j + 1) * 256], op=ALU.add),
                     r=["psS%d" % b, "bmb"], w=["g1bc"])
            R.op("dve", lambda e: e.tensor_tensor(out=lt[:, 0, :], in0=l4[:, 0, :], in1=l4[:, 1, :], op=ALU.mult), r=["l4"], w=["lt"])
            R.op("dve", lambda e: e.tensor_tensor(out=lt[:, 1, :], in0=l4[:, 2, :], in1=l4[:, 3, :], op=ALU.mult), r=["l4"], w=["lt"])
            R.op("dve", lambda e: e.tensor_reduce(out=ls[:], in_=lt[:], axis=AX.X, op=ALU.add), r=["lt"], w=["ls"])
            R.op("act", lambda e: e.activation(out=le[:], in_=ls[:], func=AF.Exp), r=["ls"], w=["le"])
            R.op("dve", lambda e: e.tensor_tensor(out=nlam[:], in0=le[:, 1:2], in1=le[:, 0:1], op=ALU.subtract), r=["le"], w=["nlam"])
            R.op("dve", lambda e: e.tensor_tensor(out=nlam[:], in0=nlam[:], in1=li[:, 0:1], op=ALU.subtract), r=["nlam", "li"], w=["nlam"])
            R.op("dve", lambda e: e.tensor_scalar(out=gn[:], in0=agb[:], scalar1=li[:, 1:2], scalar2=None, op0=ALU.mult), r=["agb", "li"], w=["gn"])
            R.final_wait = list(R.dma_count.keys())
            if "0" in phases:
                R.emit()
        nc.all_engine_barrier()

        def attn_epilogue_BC(R, po, pk, ydst, yk, rinv):
            pv = po[:].rearrange("p (a c) -> p a c", a=2)[:, :, 0:260].rearrange("p a (h e) -> p a h e", e=65)
            R.op("dve", lambda e: e.reciprocal(out=rinv[:].rearrange("p (a h) -> p a h", a=2), in_=pv[:, :, :, 64]), r=[pk], w=["rinv"])
            R.op("dve", lambda e: e.tensor_tensor(out=ydst.rearrange("p (a h e) -> p a h e", a=2, e=64), in0=pv[:, :, :, 0:64],
                                                  in1=rinv[:].rearrange("p (a h) -> p a h", a=2).unsqueeze(3).broadcast_to([128, 2, 4, 64]), op=ALU.mult),
                 r=[pk, "rinv"], w=[yk])

        def hoff(h):
            return (h // 4) * 512 + (h % 4) * 65

        R = Rec(nc)
        with ExitStack() as s1:
            sb = lambda n, s, d=F32: s1.enter_context(nc.sbuf_tensor(pfx + n, s, d))
            ktb = [sb("ktb%d" % i, [64, 2, S], BF16) for i in range(2)]
            avh = [sb("avh%d" % i, [128, NKT, 129], BF16) for i in range(2)]
            qh = [sb("qh%d" % i, [64, 2, tok], BF16) for i in range(2)]
            zm = sb("zm", [128, 4, 128], BF16)
            pt = [sb("pt%d" % i, [128, 4, 2, 128], BF16) for i in range(2)]
            r12 = sb("r12", [128, 2]); nr2 = sb("nr2", [128, 1]); d1 = sb("d1", [128, 128]); dd = sb("dd", [128, 128])
            jk = sb("jk", [128, 128]); ssq = sb("ssq", [128, 1]); lnv = sb("lnv", [128, 1]); rstd = sb("rstd", [128, 1])
            R.dma("sp", zm[:].rearrange("p a b -> p (a b)"), zmT.ap(), "c", w=["zm"])
            gi = 0
            for h in range(4):
                hb = h % 2
                for m_ in range(2):
                    for r_ in range(4):
                        R.dma("sp", ktb[hb][:, m_, :].rearrange("d (t r p) -> d t r p", r=4, p=128)[:, :, r_, :],
                              bass.AP(akt[h // 2], ((r_ * 2 + h % 2) * 64) * 2 * tok + m_ * tok, [[2 * tok, 64], [128, nt], [1, 128]]), "kt%d" % hb, w=["ktb%d" % hb])
                SL = min(4, nt)
                for c_ in range(nt // SL):
                    for r_ in range(4):
                        R.dma("sp", avh[hb][:].rearrange("p (t r) e -> p t r e", r=4)[:, c_ * SL:(c_ + 1) * SL, r_, :],
                              bass.AP(av[c_], r_ * SL * 128 * 516 + h * 129, [[516, 128], [128 * 516, SL], [1, 129]]), "av%d" % hb, w=["avh%d" % hb])
                R.dma("sp", qh[hb][:].rearrange("p a b -> p (a b)"), aqt.ap()[h], "qh%d" % hb, w=["qh%d" % hb])
                jobs = [(t, g) for t in range(nt) for g in range(t + 1)]

                def a_qk(t, g, b, hb=hb):
                    qs = slice(t * 128, (t + 1) * 128)
                    zone = (g == t)
                    for i in range(4):
                        kt = 4 * g + i
                        ks = slice(kt * 128, (kt + 1) * 128)
                        for m in range(2):
                            ps_out = psS[b][:, (i * 2 + m) * 128:(i * 2 + m + 1) * 128]
                            R.op("pe", lambda e, ps_out=ps_out, m=m, ks=ks, qs=qs, zone=zone: e.matmul(
                                ps_out, lhsT=ktb[hb][:, m, ks], rhs=qh[hb][:, m, qs], start=True, stop=not zone),
                                r=["ktb%d" % hb, "qh%d" % hb], w=["psS%d" % b])
                            if zone:
                                R.op("pe", lambda e, ps_out=ps_out, i=i: e.matmul(ps_out, lhsT=idb[:], rhs=zm[:, i, :], start=False, stop=True),
                                     r=["zm"], w=["psS%d" % b])

                def a_pv(t, g, b, hb=hb):
                    ob = t % 2
                    ok = "psO%d" % ob
                    R.op("act", lambda e: e.activation(out=pt[b][:].rearrange("p a m q -> p (a m q)"), in_=psS[b][:], func=AF.Exp, scale=0.125),
                         r=["psS%d" % b], w=["pt%d" % b])
                    for i in range(4):
                        kt = 4 * g + i
                        for m in range(2):
                            R.op("pe", lambda e, i=i, m=m, kt=kt: e.matmul(
                                psO[ob][:, m * 512:m * 512 + 129], lhsT=pt[b][:, i, m, :], rhs=avh[hb][:, kt, :],
                                start=(g == 0 and i == 0), stop=(g == t and i == 3)),
                                r=["pt%d" % b, "avh%d" % hb], w=[ok])
                    if g != t:
                        return
                    po = psO[ob]
                    R.op("dve", lambda e: e.reciprocal(out=r12[:, 0:1], in_=po[:, 128:129]), r=[ok], w=["r12"])
                    R.op("dve", lambda e: e.reciprocal(out=r12[:, 1:2], in_=po[:, 640:641]), r=[ok], w=["r12"])
                    R.op("dve", lambda e: e.tensor_tensor(out=nr2[:], in0=r12[:, 1:2], in1=nlam[:], op=ALU.mult), r=["r12"], w=["nr2"])
                    R.op("dve", lambda e: e.tensor_scalar(out=d1[:], in0=po[:, 0:128], scalar1=r12[:, 0:1], scalar2=None, op0=ALU.mult),
                         r=[ok, "r12"], w=["d1"])
                    R.op("dve", lambda e: e.scalar_tensor_tensor(out=dd[:], in0=po[:, 512:640], scalar=nr2[:, 0:1], in1=d1[:], op0=ALU.mult, op1=ALU.add),
                         r=[ok, "nr2", "d1"], w=["dd"])

                def a_norm(t, h=h):
                    R.op("act", lambda e: e.activation(out=jk[:], in_=dd[:], func=AF.Square, accum_out=ssq[:]), r=["dd"], w=["jk", "ssq"])
                    R.op("act", lambda e: e.activation(out=lnv[:], in_=ssq[:], func=AF.Ln, scale=1.0 / 128.0, bias=cst[:, 0:1]), r=["ssq"], w=["lnv"])
                    R.op("act", lambda e: e.activation(out=rstd[:], in_=lnv[:], func=AF.Exp, scale=-0.5), r=["lnv"], w=["rstd"])
                    R.op("dve", lambda e: e.scalar_tensor_tensor(out=ya[:, t, h * 128:(h + 1) * 128], in0=dd[:], scalar=rstd[:, 0:1], in1=gn[:],
                                                                 op0=ALU.mult, op1=ALU.mult), r=["dd", "rstd"], w=["ya"])

                pend = None
                a_qk(jobs[0][0], jobs[0][1], gi % 2)
                for ji, (t, g) in enumerate(jobs):
                    b = gi % 2
                    gi += 1
                    if ji + 1 < len(jobs):
                        a_qk(jobs[ji + 1][0], jobs[ji + 1][1], gi % 2)
                    a_pv(t, g, b)
                    if pend is not None:
                        a_norm(pend)
                        pend = None
                    if g == t:
                        pend = t
                if pend is not None:
                    a_norm(pend)
            R.final_wait = list(R.dma_count.keys())
            if "A" in phases:
                R.emit()
        nc.all_engine_barrier()

        R = Rec(nc)
        with ExitStack() as s2:
            sb = lambda n, s, d=F32: s2.enter_context(nc.sbuf_tensor(pfx + n, s, d))
            kk = sb("kk", [64, 2, S], BF16); bvs = sb("bvs", [128, NKT, 65], BF16)
            sc = sb("sc", [128, S]); cA = sb("cA", [128, S], BF16); cB = sb("cB", [128, S], BF16)
            mk = [sb("mk%d" % i, [128, S], BF16) for i in range(2)]
            bqi = [sb("bqi%d" % i, [64, 1536], BF16) for i in range(2)]
            iwt = [sb("iwt%d" % i, [128, 4]) for i in range(2)]
            pt = [sb("ptb%d" % i, [128, 1024], BF16) for i in range(2)]
            rl = [sb("rl%d" % i, [128, 512]) for i in range(2)] * 2
            zqs = sb("zqs", [128, 512]); p2 = sb("p2", [128, NIT]); WN = sb("WN", [128, NIT])
            rmin = sb("rmin", [128, 1]); rmax = sb("rmax", [128, 1]); w0 = sb("w0", [128, 1]); lo = sb("lo", [128, 1]); mid = sb("mid", [128, 1])
            cnt = sb("cnt", [128, 1]); dl = sb("dl", [128, 1]); hi = sb("hi", [128, 1]); chi = sb("chi", [128, 1]); mrem = sb("mrem", [128, 1])
            rinv = sb("rinv", [128, 8])
            for m_ in range(2):
                for r_ in range(4):
                    R.dma("sp", kk[:, m_, :].rearrange("d (t r p) -> d t r p", r=4, p=128)[:, :, r_, :],
                          bass.AP(bkik, r_ * 64 * 2 * tok + m_ * tok, [[2 * tok, 64], [128, nt], [1, 128]]), "c", w=["kk"])
            for r_ in range(4):
                R.dma("sp", bvs[:].rearrange("p (t r) e -> p t r e", r=4)[:, :, r_, :],
                      bass.AP(bv, r_ * tok * 65, [[65, 128], [128 * 65, nt], [1, 65]]), "c", w=["bvs"])
            R.dma("sp", zqs[:], zq.ap(), "c", w=["zqs"])
            R.dma("sp", p2[:], p2tab.ap()[0:1, :].partition_broadcast(128), "c", w=["p2"])
            psI = [psS[hh // 2][:, (hh % 2) * 512:(hh % 2) * 512 + 512] for hh in range(4)]
            def b_select(t):
                b = t % 2
                n = (4 * t + 4) * 128
                R.dma("sp", bqi[b][:], bq_iq.ap()[t], "bqi%d" % b, w=["bqi%d" % b])
                R.dma("sp", iwt[b][:], iw.ap()[t * 128:(t + 1) * 128, :], "bqi%d" % b, w=["iwt%d" % b])
                for g in range(t + 1):
                    gs_ = slice(g * 512, (g + 1) * 512)
                    for hh in range(4):
                        R.op("pe", lambda e, hh=hh, b=b, gs_=gs_: e.matmul(psI[hh], lhsT=bqi[b][:, 1024 + hh * 128:1024 + (hh + 1) * 128], rhs=kk[:, 1, gs_],
                                                                           start=True, stop=True), r=["bqi%d" % b, "kk"], w=["psS%d" % (hh // 2)])
                        R.op("act", lambda e, hh=hh: e.activation(out=rl[hh][:], in_=psI[hh], func=AF.Relu), r=["psS%d" % (hh // 2)], w=["rl%d" % (hh % 2)])
                        if hh == 0:
                            R.op("dve", lambda e, b=b, gs_=gs_: e.tensor_scalar(out=sc[:, gs_], in0=rl[0][:], scalar1=iwt[b][:, 0:1], scalar2=None, op0=ALU.mult),
                                 r=["rl0", "iwt%d" % b], w=["sc"])
                        else:
                            R.op("dve", lambda e, hh=hh, b=b, gs_=gs_: e.scalar_tensor_tensor(out=sc[:, gs_], in0=rl[hh][:], scalar=iwt[b][:, hh:hh + 1], in1=sc[:, gs_],
                                                                                               op0=ALU.mult, op1=ALU.add), r=["rl%d" % (hh % 2), "iwt%d" % b, "sc"], w=["sc"])
                R.op("dve", lambda e, n=n: e.tensor_reduce(out=rmin[:], in_=sc[:, 0:n], axis=AX.X, op=ALU.min), r=["sc"], w=["rmin"])
                R.op("dve", lambda e, t=t: e.tensor_tensor(out=sc[:, t * 512:(t + 1) * 512], in0=sc[:, t * 512:(t + 1) * 512], in1=zqs[:], op=ALU.add),
                     r=["sc", "zqs"], w=["sc"])
                R.op("dve", lambda e, n=n: e.tensor_reduce(out=rmax[:], in_=sc[:, 0:n], axis=AX.X, op=ALU.max), r=["sc"], w=["rmax"])
                R.op("dve", lambda e: e.tensor_tensor(out=w0[:], in0=rmax[:], in1=rmin[:], op=ALU.subtract), r=["rmax", "rmin"], w=["w0"])
                R.op("dve", lambda e: e.tensor_scalar(out=w0[:], in0=w0[:], scalar1=1.001, scalar2=1e-6, op0=ALU.mult, op1=ALU.add), r=["w0"], w=["w0"])
                R.op("dve", lambda e: e.tensor_scalar(out=WN[:], in0=p2[:], scalar1=w0[:, 0:1], scalar2=None, op0=ALU.mult), r=["p2", "w0"], w=["WN"])
                R.op("dve", lambda e: e.tensor_copy(out=lo[:], in_=rmin[:]), r=["rmin"], w=["lo"])
                for it in range(NIT):
                    R.op("dve", lambda e, it=it: e.tensor_tensor(out=mid[:], in0=lo[:], in1=WN[:, it:it + 1], op=ALU.add), r=["lo", "WN"], w=["mid"])
                    R.op("dve", lambda e, n=n: e.tensor_scalar(out=cA[:, 0:n], in0=sc[:, 0:n], scalar1=mid[:, 0:1], scalar2=None, op0=ALU.is_ge, op1=ALU.add,
                                                               accum_out=cnt[:]), r=["sc", "mid"], w=["cA", "cnt"])
                    R.op("dve", lambda e, it=it: e.tensor_scalar(out=dl[:], in0=cnt[:], scalar1=256.0, scalar2=WN[:, it:it + 1], op0=ALU.is_ge, op1=ALU.mult),
                         r=["cnt", "WN"], w=["dl"])
                    R.op("dve", lambda e: e.tensor_tensor(out=lo[:], in0=lo[:], in1=dl[:], op=ALU.add), r=["lo", "dl"], w=["lo"])
                R.op("dve", lambda e: e.tensor_tensor(out=hi[:], in0=lo[:], in1=WN[:, NIT - 1:NIT], op=ALU.add), r=["lo", "WN"], w=["hi"])
                R.op("dve", lambda e, n=n: e.tensor_scalar(out=cB[:, 0:n], in0=sc[:, 0:n], scalar1=hi[:, 0:1], scalar2=None, op0=ALU.is_ge, op1=ALU.add,
                                                           accum_out=chi[:]), r=["sc", "hi"], w=["cB", "chi"])
                R.op("dve", lambda e, n=n: e.tensor_scalar(out=cA[:, 0:n], in0=sc[:, 0:n], scalar1=lo[:, 0:1], scalar2=None, op0=ALU.is_ge), r=["sc", "lo"], w=["cA"])
                R.op("dve", lambda e, n=n: e.tensor_tensor(out=cA[:, 0:n], in0=cA[:, 0:n], in1=cB[:, 0:n], op=ALU.subtract), r=["cA", "cB"], w=["cA"])
                R.op("dve", lambda e: e.tensor_scalar(out=mrem[:], in0=chi[:], scalar1=-1.0, scalar2=256.0, op0=ALU.mult, op1=ALU.add), r=["chi"], w=["mrem"])
                R.op("dve", lambda e, n=n: e.tensor_tensor_scan(out=sc[:, 0:n], data0=cA[:, 0:n], data1=cA[:, 0:n], initial=0.0, op0=ALU.add, op1=ALU.max),
                     r=["cA"], w=["sc"])
                R.op("dve", lambda e, n=n: e.scalar_tensor_tensor(out=cA[:, 0:n], in0=sc[:, 0:n], scalar=mrem[:, 0:1], in1=cA[:, 0:n], op0=ALU.is_le, op1=ALU.mult),
                     r=["sc", "mrem", "cA"], w=["cA"])
                R.op("dve", lambda e, n=n, b=b: e.scalar_tensor_tensor(out=mk[b][:, 0:n], in0=cA[:, 0:n], scalar=-1.0, in1=cB[:, 0:n], op0=ALU.add, op1=ALU.add),
                     r=["cA", "cB"], w=["mk%d" % b])

            def b_attend(t):
                b = t % 2
                ob = t % 2
                ok = "psO%d" % ob
                for kt in range(4 * t + 4):
                    b2 = kt % 2
                    ks = slice(kt * 128, (kt + 1) * 128)
                    for half in range(2):
                        hs = slice(half * 512, (half + 1) * 512)
                        R.op("pe", lambda e, b2=b2, hs=hs, ks=ks, b=b: e.matmul(psS[b2][:, hs], lhsT=kk[:, 0, ks], rhs=bqi[b][:, hs], start=True, stop=False),
                             r=["kk", "bqi%d" % b], w=["psS%d" % b2])
                        R.op("pe", lambda e, b2=b2, hs=hs, ks=ks, b=b: e.matmul(psS[b2][:, hs], lhsT=mk[b][:, ks], rhs=bigi4[:], start=False, stop=True),
                             r=["mk%d" % b], w=["psS%d" % b2])
                    R.op("act", lambda e, b2=b2: e.activation(out=pt[b2][:], in_=psS[b2][:], func=AF.Exp, scale=0.125), r=["psS%d" % b2], w=["ptb%d" % b2])
                    for h in range(8):
                        R.op("pe", lambda e, h=h, b2=b2, kt=kt, ob=ob, t=t: e.matmul(psO[ob][:, hoff(h):hoff(h) + 65], lhsT=pt[b2][:, h * 128:(h + 1) * 128],
                                                                                    rhs=bvs[:, kt, :], start=(kt == 0 and h % 4 == 0), stop=(kt == 4 * t + 3 and h % 4 == 3)),
                             r=["ptb%d" % b2, "bvs"], w=[ok])
                attn_epilogue_BC(R, psO[ob], ok, yb[:, t, :], "yb", rinv)

            b_select(0)
            for t in range(1, nt):
                b_select(t)
                b_attend(t - 1)
            b_attend(nt - 1)
            R.final_wait = list(R.dma_count.keys())
            if "B" in phases:
                R.emit()
        nc.all_engine_barrier()

        R = Rec(nc)
        with ExitStack() as s3:
            sb = lambda n, s, d=F32: s3.enter_context(nc.sbuf_tensor(pfx + n, s, d))
            cqs = sb("cqs", [64, 8, tok], BF16)
            EBT = sb("EBT", [128, 8, 8, 128], BF16); stg = sb("stgc", [128, 8, 128]); cmk = sb("cmk", [128, 8, 128])
            R.dma("sp", cmk[:].rearrange("p a b -> p (a b)"), cmaskT.ap(), "c", w=["cmk"])
            for j in range(8):
                src = bass.AP(rbext, T["rb_off"] + 1665 - 128 * j, [[1, 128], [T["rb_w"], 8], [1, 128]])
                R.dma("sp", stg[:], src, "stg", w=["stg"])
                R.op("dve", lambda e, j=j: e.scalar_tensor_tensor(out=EBT[:, j, :, :], in0=stg[:], scalar=8.0,
                                                                   in1=cmk[:, j, :].unsqueeze(1).broadcast_to([128, 8, 128]),
                                                                   op0=ALU.mult, op1=ALU.add), r=["stg", "cmk"], w=["EBT"])
            ck = [sb("ck%d" % i, [64, 8, 8, 128], BF16) for i in range(2)]
            cvs = [sb("cvs%d" % i, [128, 8, 520], BF16) for i in range(2)]
            pt = [sb("ptc%d" % i, [128, 1024], BF16) for i in range(2)]
            rinv = sb("rinvc", [128, 8])
            R.dma("sp", cqs[:].rearrange("p a b -> p (a b)"), cqt.ap(), "c", w=["cqs"])
            gi = 0
            for t in range(nt):
                b = t % 2
                for si, s_ in enumerate((t - 1, t)):
                    if s_ < 0:
                        R.op("pool", lambda e, b=b: e.memset(ck[b][:, 0:4, :, :], 0.0), w=["ck%d" % b])
                        R.op("pool", lambda e, b=b: e.memset(cvs[b][:, 0:4, :], 0.0), w=["cvs%d" % b])
                        continue
                    for r_ in range(4):
                        for c_ in range(2):
                            R.dma("sp", ck[b][c_ * 32:(c_ + 1) * 32, si * 4 + r_, :, :].rearrange("p h k -> p (h k)"),
                                  bass.AP(ckb[c_], r_ * 32 * 8 * tok + s_ * 1024, [[8 * tok, 32], [1, 1024]]), "ck%d" % b, w=["ck%d" % b])
                    SLc = min(4, nt)
                    R.dma("sp", cvs[b][:, si * 4:(si + 1) * 4, :], bass.AP(cvb[s_ // SLc], (s_ % SLc) * 128 * 520, [[520, 128], [SLc * 128 * 520, 4], [1, 520]]),
                          "ck%d" % b, w=["cvs%d" % b])
                ob = t % 2
                ok = "psO%d" % ob
                for j in range(8):
                    b2 = gi % 2
                    gi += 1
                    for h in range(8):
                        R.op("pe", lambda e, h=h, b=b, b2=b2, j=j, t=t: e.matmul(
                            psS[b2][:, h * 128:(h + 1) * 128], lhsT=ck[b][:, j, h, :],
                            rhs=cqs[:, h, t * 128:(t + 1) * 128], start=(h % 4 == 0), stop=False), r=["ck%d" % b, "cqs"], w=["psS%d" % b2])
                    for half in range(2):
                        R.op("pe", lambda e, half=half, b2=b2, j=j: e.matmul(psS[b2][:, half * 512:(half + 1) * 512], lhsT=jdb[:],
                                                                             rhs=EBT[:, j, half * 4:(half + 1) * 4, :].rearrange("p h q -> p (h q)"),
                                                                             start=False, stop=True), r=["EBT"], w=["psS%d" % b2])
                    R.op("act", lambda e, b2=b2: e.activation(out=pt[b2][:], in_=psS[b2][:], func=AF.Exp, scale=0.125), r=["psS%d" % b2], w=["ptc%d" % b2])
                    for h in range(8):
                        R.op("pe", lambda e, h=h, b2=b2, j=j, b=b, ob=ob: e.matmul(psO[ob][:, hoff(h):hoff(h) + 65], lhsT=pt[b2][:, h * 128:(h + 1) * 128],
                                                                                  rhs=cvs[b][:, j, h * 65:(h + 1) * 65], start=(j == 0 and h % 4 == 0), stop=(j == 7 and h % 4 == 3)),
                             r=["ptc%d" % b2, "cvs%d" % b], w=[ok])
                attn_epilogue_BC(R, psO[ob], ok, yc[:, t, :], "yc", rinv)
            R.final_wait = list(R.dma_count.keys())
            if "C" in phases:
                R.emit()
        nc.all_engine_barrier()

        R = Rec(nc)
        with ExitStack() as s4:
            sb = lambda n, s, d=F32: s4.enter_context(nc.sbuf_tensor(pfx + n, s, d))
            wbr = [sb("wbr%d" % i, [128, 4, D], BF16) for i in range(3)]
            wo = sb("wo", [128, 8, D], BF16)
            gt = [sb("gt%d" % i, [128, 3072]) for i in range(2)]
            xt = [sb("xt%d" % i, [128, D]) for i in range(2)]
            yT = sb("yT", [128, 512], BF16); mg = sb("mg", [128, D]); tt = sb("tt", [128, 512]); mgb = sb("mgb", [128, D], BF16)
            mT = sb("mT", [128, D], BF16); xot = [sb("xot%d" % i, [128, D]) for i in range(2)]
            for i, wsrc in enumerate((wa, wb, wc)):
                R.dma("pool", wbr[i][:], wsrc.ap().rearrange("(ch p) n -> p ch n", p=128), "w", w=["wbr%d" % i])
            R.dma("pool", wo[:], w_out.ap().rearrange("(ch p) n -> p ch n", p=128), "w", w=["wo"])
            pXt = psO[0][:, 0:512].bitcast(BF16)
            assert tuple(pXt.shape) == (128, 1024), pXt.shape
            for t in range(nt):
                b = t % 2
                R.dma("sp", gt[b][:], gates.ap()[t * 128:(t + 1) * 128, :], "gx%d" % b, w=["gt%d" % b])
                R.dma("sp", xt[b][:], x.ap()[t * 128:(t + 1) * 128, :], "gx%d" % b, w=["xt%d" % b])
                for bi, (ysrc, yk) in enumerate(((ya, "ya"), (yb, "yb"), (yc, "yc"))):
                    for ch in range(4):
                        R.op("pe", lambda e, ysrc=ysrc, ch=ch, t=t: e.transpose(out=pXt[:, ch * 128:(ch + 1) * 128], in_=ysrc[:, t, ch * 128:(ch + 1) * 128], identity=idb[:]),
                             r=[], w=["pXt"])
                    R.op("act", lambda e: e.copy(out=yT[:], in_=pXt[:, 0:512]), r=["pXt"], w=["yT"])
                    for half in range(2):
                        hs = slice(half * 512, (half + 1) * 512)
                        for ch in range(4):
                            R.op("pe", lambda e, ch=ch, bi=bi, half=half, hs=hs: e.matmul(psS[half][:, 0:512], lhsT=yT[:, ch * 128:(ch + 1) * 128], rhs=wbr[bi][:, ch, hs],
                                                                                          start=(ch == 0), stop=(ch == 3)), r=["yT", "wbr%d" % bi], w=["psS%d" % half])
                        gsl = gt[b][:, bi * 1024 + half * 512: bi * 1024 + (half + 1) * 512]
                        if bi == 0:
                            R.op("dve", lambda e, half=half, hs=hs, gsl=gsl: e.tensor_tensor(out=mg[:, hs], in0=psS[half][:, 0:512], in1=gsl, op=ALU.mult),
                                 r=["psS%d" % half, "gt%d" % b], w=["mg%d" % half])
                        else:
                            R.op("dve", lambda e, half=half, gsl=gsl: e.tensor_tensor(out=tt[:], in0=psS[half][:, 0:512], in1=gsl, op=ALU.mult),
                                 r=["psS%d" % half, "gt%d" % b], w=["tt"])
                            dst = mg if bi == 1 else mgb
                            R.op("dve", lambda e, hs=hs, dst=dst: e.tensor_tensor(out=dst[:, hs], in0=mg[:, hs], in1=tt[:], op=ALU.add),
                                 r=["tt", "mg%d" % half], w=["mg%d" % half if bi == 1 else "mgb%d" % half])
                for ch in range(8):
                    R.op("pe", lambda e, ch=ch: e.transpose(out=pXt[:, ch * 128:(ch + 1) * 128], in_=mgb[:, ch * 128:(ch + 1) * 128], identity=idb[:]),
                         r=["mgb0", "mgb1"], w=["pXt"])
                R.op("act", lambda e: e.copy(out=mT[:], in_=pXt[:]), r=["pXt"], w=["mT"])
                for half in range(2):
                    hs = slice(half * 512, (half + 1) * 512)
                    for ch in range(8):
                        R.op("pe", lambda e, ch=ch, half=half, hs=hs: e.matmul(psS[half][:, 0:512], lhsT=mT[:, ch * 128:(ch + 1) * 128], rhs=wo[:, ch, hs],
                                                                               start=(ch == 0), stop=(ch == 7)), r=["mT", "wo"], w=["psS%d" % half])
                    R.op("dve", lambda e, half=half, hs=hs: e.tensor_tensor(out=tt[:], in0=psS[half][:, 0:512], in1=g1bc[:, hs], op=ALU.mult),
                         r=["psS%d" % half], w=["tt"])
                    R.op("dve", lambda e, hs=hs, b=b: e.tensor_tensor(out=xot[b][:, hs], in0=tt[:], in1=xt[b][:, hs], op=ALU.add),
                         r=["tt", "xt%d" % b], w=["xot%d_%d" % (b, half)])
                R.dma("sp", xo.ap()[t * 128:(t + 1) * 128, :], xot[b][:], "out%d" % b, r=["xot%d_0" % b, "xot%d_1" % b])
            R.final_wait = list(R.dma_count.keys())
            if "M" in phases:
                R.emit()


D = 1024
EPS = 1e-6
NE = 16
FF = 512
BIGR = 1.0e4


def emit_M(nc, T, pfx, nt=16, final=False, ne=NE, phases="123", cut=9):
    tok = nt * 128
    di = lambda n, s, d=F32: T[n]
    x = di("x", [tok, D]); cvec = di("c", [128, 8]); w_mod = di("w_mod", [D, 6144]); b_mod = di("b_mod", [1, 6144])
    norm_g = di("norm_g", [1, D]); router_w = di("router_w", [D, 16]); router_b = di("router_b", [1, 16])
    w1 = di("w1", [NE, D, FF]); w3 = di("w3", [NE, D, FF]); w2 = di("w2", [NE, FF, D]); ident = di("ident", [128, 128])
    final_g = di("final_g", [1, D])
    xo = T["xo"]
    TG = (nt + 3) // 4

    with ExitStack() as st, nc.allow_low_precision("bf16 matmul operands, fp32 accumulation"):
        sbo = lambda n, s, d=F32: st.enter_context(nc.sbuf_tensor(pfx + n, s, d))
        pso = lambda n, s, d=F32: st.enter_context(nc.psum_tensor(pfx + n, s, d))
        modbc = sbo("modbc", [128, 3 * D]); gs = sbo("gs", [128, D]); idf = sbo("idf", [128, 128])
        uT = sbo("uT", [128, 8, tok], BF16); comb = sbo("comb", [128, nt, 16]); yacc = sbo("yacc", [128, nt, D])
        cst = sbo("cst", [128, 2]); fgb = sbo("fgb", [128, D])
        PS = [pso("PS%d" % i, [128, 512]) for i in range(8)]

        R = Rec(nc)
        with ExitStack() as s0:
            sb = lambda n, s, d=F32: s0.enter_context(nc.sbuf_tensor(pfx + n, s, d))
            cs = sb("cs", [128, 8]); ca = sb("ca", [128, 8]); CA = sb("CA", [128, 8, 128])
            wm = [sb("wm%d" % i, [128, 8, 256]) for i in range(2)]
            bmb = sb("bmb", [128, 3 * D]); gbc = sb("gbc", [128, D]); rw = sb("rw", [128, 8, 16]); rbb = sb("rbb", [128, 16])
            xt = [sb("xt%d" % i, [128, D]) for i in range(2)]
            junk = sb("junk", [128, D], BF16); ss = sb("ss", [128, 1]); rt = sb("rt", [128, 1]); rstd = sb("rstd", [128, 1])
            tmp = sb("tmp", [128, D]); u2 = sb("u2", [128, D]); uh = sb("uh", [128, D], BF16); ul = sb("ul", [128, D], BF16)
            ulT = sb("ulT", [128, D], BF16); idb = sb("idb", [128, 128], BF16); rwh = sb("rwh", [128, 8, 16], BF16); rwl = sb("rwl", [128, 8, 16], BF16)
            pXh = PS[2][:].bitcast(BF16); pXl = PS[3][:].bitcast(BF16)
            aff = sb("aff", [128, 16]); sel = sb("sel", [128, 16]); m1 = sb("m1", [128, 4]); eq = sb("eq", [128, 16]); s2 = sb("s2", [128, 16])
            m2 = sb("m2", [128, 4]); gsum = sb("gsum", [128, 4]); gmax = sb("gmax", [128, 1]); ing = sb("ing", [128, 4]); pen = sb("pen", [128, 4])
            selm = sb("selm", [128, 16]); t1 = sb("t1", [128, 1]); e1 = sb("e1", [128, 16]); t2 = sb("t2", [128, 1]); e2 = sb("e2", [128, 16])
            den = sb("den", [128, 1]); rden = sb("rden", [128, 1])
            R.dma("sp", cs[:], cvec.ap(), "c", w=["cs"])
            R.dma("sp", idf[:], ident.ap(), "c", w=["idf"])
            R.dma("sp", bmb[:], b_mod.ap()[0:1, 3072:6144].partition_broadcast(128), "c", w=["bmb"])
            R.dma("sp", gbc[:], norm_g.ap()[0:1, :].partition_broadcast(128), "c", w=["gbc"])
            R.dma("sp", fgb[:], final_g.ap()[0:1, :].partition_broadcast(128), "c", w=["fgb"])
            R.dma("sp", rbb[:], router_b.ap()[0:1, :].partition_broadcast(128), "c", w=["rbb"])
            R.dma("sp", rw[:], router_w.ap().rearrange("(ch p) n -> p ch n", p=128), "c", w=["rw"])
            R.op("pool", lambda e: e.memset(cst[:, 0:1], EPS), w=["cst"])
            R.op("dve", lambda e: e.tensor_copy(out=idb[:], in_=idf[:]), r=["idf"], w=["idb"])
            R.op("dve", lambda e: e.tensor_copy(out=rwh[:], in_=rw[:]), r=["rw"], w=["rwh"])
            R.op("dve", lambda e: e.tensor_tensor(out=rwl[:], in0=rw[:], in1=rwh[:], op=ALU.subtract), r=["rw", "rwh"], w=["rwl"])
            R.op("act", lambda e: e.activation(out=ca[:], in_=cs[:], func=AF.Silu), r=["cs"], w=["ca"])
            R.op("dve", lambda e: e.tensor_copy(out=CA[:], in_=ca[:].unsqueeze(2).broadcast_to([128, 8, 128])), r=["ca"], w=["CA"])
            wmv = w_mod.ap().rearrange("(ch p) n -> p ch n", p=128)
            for j in range(12):
                b = j % 2
                R.dma("sp", wm[b][:], wmv[:, :, 3072 + j * 256:3072 + (j + 1) * 256], "wm%d" % b, w=["wm%d" % b])
                for ch in range(8):
                    R.op("pe", lambda e, ch=ch, b=b: e.matmul(PS[b][:, 0:256], lhsT=CA[:, ch, :], rhs=wm[b][:, ch, :], start=(ch == 0), stop=(ch == 7)),
                         r=["CA", "wm%d" % b], w=["PS%d" % b])
                R.op("dve", lambda e, j=j, b=b: e.tensor_tensor(out=modbc[:, j * 256:(j + 1) * 256], in0=PS[b][:, 0:256], in1=bmb[:, j * 256:(j + 1) * 256], op=ALU.add),
                     r=["PS%d" % b, "bmb"], w=["modbc"])
            R.op("dve", lambda e: e.scalar_tensor_tensor(out=gs[:], in0=modbc[:, D:2 * D], scalar=1.0, in1=gbc[:], op0=ALU.add, op1=ALU.mult),
                 r=["modbc", "gbc"], w=["gs"])
            for t in range(nt):
                b = t % 2
                xk = "xt%d" % b
                R.dma("sp", xt[b][:], x.ap()[t * 128:(t + 1) * 128, :], xk, w=[xk])
                R.op("act", lambda e, b=b: e.activation(out=junk[:], in_=xt[b][:], func=AF.Square, accum_out=ss[:]), r=[xk], w=["junk", "ss"])
                R.op("act", lambda e: e.activation(out=rt[:], in_=ss[:], func=AF.Sqrt, scale=1.0 / D, bias=cst[:, 0:1]), r=["ss", "cst"], w=["rt"])
                R.op("dve", lambda e: e.reciprocal(out=rstd[:], in_=rt[:]), r=["rt"], w=["rstd"])
                R.op("dve", lambda e, b=b: e.scalar_tensor_tensor(out=tmp[:], in0=xt[b][:], scalar=rstd[:, 0:1], in1=gs[:], op0=ALU.mult, op1=ALU.mult),
                     r=[xk, "rstd", "gs"], w=["tmp"])
                R.op("dve", lambda e: e.tensor_tensor(out=u2[:], in0=tmp[:], in1=modbc[:, 0:D], op=ALU.add), r=["tmp", "modbc"], w=["u2"])
                if cut < 2:
                    continue
                R.op("dve", lambda e: e.tensor_copy(out=uh[:], in_=u2[:]), r=["u2"], w=["uh"])
                R.op("dve", lambda e: e.tensor_tensor(out=ul[:], in0=u2[:], in1=uh[:], op=ALU.subtract), r=["u2", "uh"], w=["ul"])
                for ch in range(8):
                    R.op("pe", lambda e, ch=ch: e.transpose(out=pXh[:, ch * 128:(ch + 1) * 128], in_=uh[:, ch * 128:(ch + 1) * 128], identity=idb[:]),
                         r=["uh", "idb"], w=["PS2"])
                R.op("act", lambda e, t=t: e.copy(out=uT[:, :, t * 128:(t + 1) * 128], in_=pXh.rearrange("p (c q) -> p c q", q=128)), r=["PS2"], w=["uT"])
                for ch in range(8):
                    R.op("pe", lambda e, ch=ch: e.transpose(out=pXl[:, ch * 128:(ch + 1) * 128], in_=ul[:, ch * 128:(ch + 1) * 128], identity=idb[:]),
                         r=["ul", "idb"], w=["PS3"])
                R.op("dve", lambda e: e.tensor_copy(out=ulT[:], in_=pXl), r=["PS3"], w=["ulT"])
                if cut < 3:
                    continue
                for ch in range(8):
                    uhs = uT[:, ch, t * 128:(t + 1) * 128]
                    R.op("pe", lambda e, ch=ch, uhs=uhs: e.matmul(PS[4][:, 0:16], lhsT=uhs, rhs=rwh[:, ch, :], start=(ch == 0), stop=False), r=["uT", "rwh"], w=["PS4"])
                    R.op("pe", lambda e, ch=ch, uhs=uhs: e.matmul(PS[4][:, 0:16], lhsT=uhs, rhs=rwl[:, ch, :], start=False, stop=False), r=["uT", "rwl"], w=["PS4"])
                    R.op("pe", lambda e, ch=ch: e.matmul(PS[4][:, 0:16], lhsT=ulT[:, ch * 128:(ch + 1) * 128], rhs=rwh[:, ch, :], start=False, stop=(ch == 7)),
                         r=["ulT", "rwh"], w=["PS4"])
                v4 = lambda a: a[:].rearrange("p (g k) -> p g k", k=4)
                R.op("act", lambda e: e.activation(out=aff[:], in_=PS[4][:, 0:16], func=AF.Sigmoid), r=["PS4"], w=["aff"])
                if cut < 4:
                    continue
                R.op("dve", lambda e: e.tensor_tensor(out=sel[:], in0=aff[:], in1=rbb[:], op=ALU.add), r=["aff", "rbb"], w=["sel"])
                R.op("dve", lambda e: e.tensor_reduce(out=m1[:], in_=v4(sel), axis=AX.X, op=ALU.max), r=["sel"], w=["m1"])
                R.op("dve", lambda e: e.tensor_tensor(out=v4(eq), in0=v4(sel), in1=m1[:].unsqueeze(2).broadcast_to([128, 4, 4]), op=ALU.is_equal), r=["sel", "m1"], w=["eq"])
                R.op("dve", lambda e: e.scalar_tensor_tensor(out=s2[:], in0=eq[:], scalar=-BIGR, in1=sel[:], op0=ALU.mult, op1=ALU.add), r=["eq", "sel"], w=["s2"])
                R.op("dve", lambda e: e.tensor_reduce(out=m2[:], in_=v4(s2), axis=AX.X, op=ALU.max), r=["s2"], w=["m2"])
                R.op("dve", lambda e: e.tensor_tensor(out=gsum[:], in0=m1[:], in1=m2[:], op=ALU.add), r=["m1", "m2"], w=["gsum"])
                R.op("dve", lambda e: e.tensor_reduce(out=gmax[:], in_=gsum[:], axis=AX.X, op=ALU.max), r=["gsum"], w=["gmax"])
                R.op("dve", lambda e: e.tensor_scalar(out=pen[:], in0=gsum[:], scalar1=gmax[:, 0:1], scalar2=-BIGR, op0=ALU.is_lt, op1=ALU.mult), r=["gsum", "gmax"], w=["pen"])
                R.op("dve", lambda e: e.tensor_tensor(out=v4(selm), in0=v4(sel), in1=pen[:].unsqueeze(2).broadcast_to([128, 4, 4]), op=ALU.add), r=["sel", "pen"], w=["selm"])
                R.op("dve", lambda e: e.tensor_reduce(out=t1[:], in_=selm[:], axis=AX.X, op=ALU.max), r=["selm"], w=["t1"])
                R.op("dve", lambda e: e.tensor_scalar(out=e1[:], in0=selm[:], scalar1=t1[:, 0:1], scalar2=None, op0=ALU.is_equal), r=["selm", "t1"], w=["e1"])
                R.op("dve", lambda e: e.scalar_tensor_tensor(out=s2[:], in0=e1[:], scalar=-BIGR, in1=selm[:], op0=ALU.mult, op1=ALU.add), r=["e1", "selm"], w=["s2"])
                R.op("dve", lambda e: e.tensor_reduce(out=t2[:], in_=s2[:], axis=AX.X, op=ALU.max), r=["s2"], w=["t2"])
                R.op("dve", lambda e: e.tensor_scalar(out=e2[:], in0=s2[:], scalar1=t2[:, 0:1], scalar2=None, op0=ALU.is_equal), r=["s2", "t2"], w=["e2"])
                R.op("dve", lambda e: e.tensor_tensor(out=e1[:], in0=e1[:], in1=e2[:], op=ALU.add), r=["e1", "e2"], w=["e1"])
                R.op("dve", lambda e: e.tensor_tensor(out=e2[:], in0=e1[:], in1=aff[:], op=ALU.mult), r=["e1", "aff"], w=["e2"])
                R.op("dve", lambda e: e.tensor_reduce(out=den[:], in_=e2[:], axis=AX.X, op=ALU.add), r=["e2"], w=["den"])
                R.op("dve", lambda e: e.reciprocal(out=rden[:], in_=den[:]), r=["den"], w=["rden"])
                R.op("dve", lambda e, t=t: e.tensor_scalar(out=comb[:, t, :], in0=e2[:], scalar1=rden[:, 0:1], scalar2=None, op0=ALU.mult), r=["e2", "rden"], w=["comb"])
            R.final_wait = list(R.dma_count.keys())
            if "1" in phases:
                R.emit()
        nc.all_engine_barrier()

        R = Rec(nc)
        with ExitStack() as s1:
            sb = lambda n, s, d=F32: s1.enter_context(nc.sbuf_tensor(pfx + n, s, d))
            w1s = [sb("w1s%d" % i, [128, 8, FF], BF16) for i in range(2)]
            w3s = [sb("w3s%d" % i, [128, 8, FF], BF16) for i in range(2)]
            w2s = [sb("w2s%d" % i, [128, 4, D], BF16) for i in range(2)]
            sl = [sb("sl%d" % i, [128, 512]) for i in range(2)]
            hT = [sb("hT%d" % i, [128, 4, 512], BF16) for i in range(2)]
            kc = [0]

            def m_h(e_, tg, hb):
                wb = e_ % 2
                if tg == 0:
                    R.dma("pool", w1s[wb][:], w1.ap()[e_].rearrange("(ch p) f -> p ch f", p=128), "w1_%d" % wb, w=["w1s%d" % wb])
                    R.dma("pool", w3s[wb][:], w3.ap()[e_].rearrange("(ch p) f -> p ch f", p=128), "w1_%d" % wb, w=["w3s%d" % wb])
                    R.dma("pool", w2s[wb][:], w2.ap()[e_].rearrange("(ch p) n -> p ch n", p=128), "w1_%d" % wb, w=["w2s%d" % wb])
                ntl = min(4, nt - tg * 4)
                ncol = ntl * 128
                tsl = slice(tg * 512, tg * 512 + ncol)
                for fc in range(4):
                    pb = (kc[0] % 2) * 2
                    kc[0] += 1
                    for ch in range(8):
                        R.op("pe", lambda e, ch=ch, fc=fc, pb=pb: e.matmul(PS[pb][:, 0:ncol], lhsT=w1s[wb][:, ch, fc * 128:(fc + 1) * 128], rhs=uT[:, ch, tsl],
                                                                           start=(ch == 0), stop=(ch == 7)), r=["w1s%d" % wb], w=["PS%d" % pb])
                    for ch in range(8):
                        R.op("pe", lambda e, ch=ch, fc=fc, pb=pb: e.matmul(PS[pb + 1][:, 0:ncol], lhsT=w3s[wb][:, ch, fc * 128:(fc + 1) * 128], rhs=uT[:, ch, tsl],
                                                                           start=(ch == 0), stop=(ch == 7)), r=["w3s%d" % wb], w=["PS%d" % (pb + 1)])
                    sb_ = kc[0] % 2
                    R.op("act", lambda e, pb=pb, sb_=sb_: e.activation(out=sl[sb_][:, 0:ncol], in_=PS[pb][:, 0:ncol], func=AF.Silu), r=["PS%d" % pb], w=["sl%d" % sb_])
                    R.op("dve", lambda e, pb=pb, sb_=sb_, fc=fc: e.tensor_tensor(out=hT[hb][:, fc, 0:ncol], in0=PS[pb + 1][:, 0:ncol], in1=sl[sb_][:, 0:ncol], op=ALU.mult),
                         r=["PS%d" % (pb + 1), "sl%d" % sb_], w=["hT%d" % hb])

            def m_w(e_, tg, hb):
                wb = e_ % 2
                ntl = min(4, nt - tg * 4)
                for ti in range(ntl):
                    t = tg * 4 + ti
                    for hf in range(2):
                        ob = 4 + ((t * 2 + hf) % 4)
                        for fc in range(4):
                            R.op("pe", lambda e, fc=fc, ti=ti, hf=hf, ob=ob: e.matmul(PS[ob][:], lhsT=hT[hb][:, fc, ti * 128:(ti + 1) * 128], rhs=w2s[wb][:, fc, hf * 512:(hf + 1) * 512],
                                                                                   start=(fc == 0), stop=(fc == 3)), r=["hT%d" % hb, "w2s%d" % wb], w=["PS%d" % ob])
                        ysl = yacc[:, t, hf * 512:(hf + 1) * 512]
                        if e_ == 0:
                            R.op("dve", lambda e, ob=ob, ysl=ysl, t=t: e.tensor_scalar(out=ysl, in0=PS[ob][:], scalar1=comb[:, t, e_:e_ + 1], scalar2=None, op0=ALU.mult),
                                 r=["PS%d" % ob], w=["y%d_%d" % (t, hf)])
                        else:
                            R.op("dve", lambda e, ob=ob, ysl=ysl, t=t: e.scalar_tensor_tensor(out=ysl, in0=PS[ob][:], scalar=comb[:, t, e_:e_ + 1], in1=ysl, op0=ALU.mult, op1=ALU.add),
                                 r=["PS%d" % ob, "y%d_%d" % (t, hf)], w=["y%d_%d" % (t, hf)])

            mjobs = [(e_, tg) for e_ in range(ne) for tg in range(TG)]
            m_h(*mjobs[0], 0)
            for ji, job in enumerate(mjobs):
                if ji + 1 < len(mjobs):
                    m_h(*mjobs[ji + 1], (ji + 1) % 2)
                m_w(*job, ji % 2)
            R.final_wait = list(R.dma_count.keys())
            if "2" in phases:
                R.emit()
        nc.all_engine_barrier()

        R = Rec(nc)
        with ExitStack() as s2:
            sb = lambda n, s, d=F32: s2.enter_context(nc.sbuf_tensor(pfx + n, s, d))
            xt = [sb("f_xt%d" % i, [128, D]) for i in range(2)]
            xn = [sb("f_xn%d" % i, [128, D]) for i in range(2)]
            tmp = sb("f_tmp", [128, D]); junk = sb("f_junk", [128, D], BF16); ss = sb("f_ss", [128, 1]); rt = sb("f_rt", [128, 1]); rstd = sb("f_rstd", [128, 1])
            for t in range(nt):
                b = t % 2
                R.dma("sp", xt[b][:], x.ap()[t * 128:(t + 1) * 128, :], "x%d" % b, w=["xt%d" % b])
                R.op("dve", lambda e, t=t: e.tensor_tensor(out=tmp[:], in0=yacc[:, t, :], in1=modbc[:, 2 * D:3 * D], op=ALU.mult), r=[], w=["tmp"])
                R.op("dve", lambda e, b=b: e.tensor_tensor(out=xn[b][:], in0=tmp[:], in1=xt[b][:], op=ALU.add), r=["tmp", "xt%d" % b], w=["xn%d" % b])
                if final:
                    R.op("act", lambda e, b=b: e.activation(out=junk[:], in_=xn[b][:], func=AF.Square, accum_out=ss[:]), r=["xn%d" % b], w=["junk", "ss"])
                    R.op("act", lambda e: e.activation(out=rt[:], in_=ss[:], func=AF.Sqrt, scale=1.0 / D, bias=cst[:, 0:1]), r=["ss"], w=["rt"])
                    R.op("dve", lambda e: e.reciprocal(out=rstd[:], in_=rt[:]), r=["rt"], w=["rstd"])
                    R.op("dve", lambda e, b=b: e.scalar_tensor_tensor(out=xn[b][:], in0=xn[b][:], scalar=rstd[:, 0:1], in1=fgb[:], op0=ALU.mult, op1=ALU.mult),
                         r=["xn%d" % b, "rstd"], w=["xn%d" % b])
                R.dma("sp", xo.ap()[t * 128:(t + 1) * 128, :], xn[b][:], "o%d" % b, r=["xn%d" % b])
            R.final_wait = list(R.dma_count.keys())
            if "3" in phases:
                R.emit()


class H:
    def __init__(self, ap):
        self._ap = ap

    def ap(self):
        return self._ap


RBW = 2688
RG = [[0, 1, 2, 3], [4, 5, 6, 7]]


def build_fused(nt=16, stop=None):
    tok = nt * 128
    nc = bass.Bass("TRN2", target_bir_lowering=False)
    di = lambda n, s, d=F32: nc.dram_tensor(n, s, d, kind="ExternalInput")
    dn = lambda n, s, d=BF16: nc.dram_tensor(n, s, d)
    E = {}
    E["x"] = di("x", [tok, D]); E["pos"] = di("pos", [128, nt], I32); E["c"] = di("c", [128, 8])
    E["zmT"] = di("zmT", [128, 512], BF16); E["zq"] = di("zq", [128, 512]); E["cmaskT"] = di("cmaskT", [128, 1024])
    E["rbcore"] = di("rbcore", [2, 8, RBW])
    E["w_mod"] = di("w_mod", [2, D, 6144]); E["b_mod"] = di("b_mod", [2, 1, 6144])
    E["norm1_g"] = di("norm1_g", [2, 1, D]); E["norm2_g"] = di("norm2_g", [2, 1, D]); E["w_in"] = di("w_in", [2, D, INC])
    E["lam4"] = di("lam4", [2, 1, 256]); E["a_norm_g"] = di("a_norm_g", [2, 1, 128]); E["laminit"] = di("laminit", [2, 1, 2])
    E["wa"] = di("wa", [2, 512, D]); E["wb"] = di("wb", [2, 512, D]); E["wc"] = di("wc", [2, 512, D]); E["w_out"] = di("w_out", [2, D, D])
    E["router_w"] = di("router_w", [D, 16]); E["router_b"] = di("router_b", [1, 16])
    E["w1"] = di("w1", [2, NE, D, FF]); E["w3"] = di("w3", [2, NE, D, FF]); E["w2"] = di("w2", [2, NE, FF, D])
    E["final_g"] = di("final_g", [1, D]); E["ident"] = di("ident", [128, 128]); E["aident"] = di("aident", [128, 128])
    E["ropeinv"] = di("ropeinv", [1, 32]); E["p2tab"] = di("p2tab", [1, NIT])
    out = nc.dram_tensor("out", [tok, D], F32, kind="ExternalOutput")
    xin = E["x"]
    for layer in range(2):
        L = "L%d_" % layer
        aqt = dn(L + "aqt", [4, 64, 2 * tok]); bq_iq = dn(L + "bq_iq", [nt, 64, 1536]); iw = dn(L + "iw", [tok, 4], F32)
        cqt = dn(L + "cqt", [64, 8 * tok]); gates = dn(L + "gates", [tok, 3072], F32)
        akt_l = dn(L + "akt_l", [256, 2 * tok]); av_l = dn(L + "av_l", [tok, 516]); bkik_l = dn(L + "bkik_l", [64, 2 * tok])
        bv_l = dn(L + "bv_l", [tok, 65]); ck_l = dn(L + "ck_l", [64, 8 * tok]); cv_l = dn(L + "cv_l", [tok, 520])
        SL = min(4, nt); NCH = nt // SL
        akt_g = [dn(L + "akt_g%d" % i, [4 * 128, 2 * tok]) for i in range(2)]
        av_g = [dn(L + "av_g%d" % i, [4 * SL * 128, 516]) for i in range(NCH)]
        bkik_g = dn(L + "bkik_g", [4 * 64, 2 * tok]); bv_g = dn(L + "bv_g", [4 * tok, 65])
        ck_g = [dn(L + "ck_g%d" % i, [4 * 32, 8 * tok]) for i in range(2)]
        cv_g = [dn(L + "cv_g%d" % i, [4 * SL * 128, 520]) for i in range(NCH)]
        xmid = dn(L + "xmid", [tok, D], F32); xnext = dn(L + "xnext", [tok, D], F32) if layer == 0 else out
        wmod = H(E["w_mod"].ap()[layer]); bmod = H(E["b_mod"].ap()[layer])
        TP = {"x": xin, "pos": E["pos"], "c": E["c"], "w_mod": wmod, "b_mod": bmod, "norm_g": H(E["norm1_g"].ap()[layer]),
              "w_in": H(E["w_in"].ap()[layer]), "ident": E["ident"], "ropeinv": E["ropeinv"],
              "aqt": aqt, "akt": akt_l, "av": av_l, "bqt": bq_iq, "bkt": bkik_l, "bv": bv_l, "iw": iw,
              "cqt": cqt, "ckt": ck_l, "cv": cv_l, "gates": gates}
        emit_P(nc, TP, L + "P_", nt)
        nc.all_engine_barrier()
        if stop == "P":
            return nc
        pairs = [(bkik_l.ap(), bkik_g.ap()), (bv_l.ap(), bv_g.ap())]
        for i in range(2):
            pairs.append((akt_l.ap()[i * 128:(i + 1) * 128, :], akt_g[i].ap()))
            pairs.append((ck_l.ap()[i * 32:(i + 1) * 32, :], ck_g[i].ap()))
        for i in range(NCH):
            pairs.append((av_l.ap()[i * SL * 128:(i + 1) * SL * 128, :], av_g[i].ap()))
            pairs.append((cv_l.ap()[i * SL * 128:(i + 1) * SL * 128, :], cv_g[i].ap()))
        ccs = nc.alloc_semaphore(name=L + "ccs")
        with nc.Block() as blk:
            @blk.gpsimd
            def _(g):
                for (lo_, ga_) in pairs:
                    g.collective_compute("AllGather", mybir.AluOpType.bypass, replica_groups=RG,
                                         ins=[lo_], outs=[ga_]).then_inc(ccs, 1)
                g.wait_ge(ccs, len(pairs))
        nc.all_engine_barrier()
        nc.clear_and_free_semaphores([ccs])
        nc.all_engine_barrier()
        if stop == "AG":
            return nc
        TT = {"aqt": aqt, "bq_iq": bq_iq, "iw": iw, "cqt": cqt, "gates": gates, "x": xin,
              "akt": akt_g, "av": av_g, "bkik": bkik_g, "bv": bv_g, "ckb": ck_g, "cvb": cv_g,
              "zmT": E["zmT"], "zq": E["zq"], "cmaskT": E["cmaskT"],
              "wa": H(E["wa"].ap()[layer]), "wb": H(E["wb"].ap()[layer]), "wc": H(E["wc"].ap()[layer]), "w_out": H(E["w_out"].ap()[layer]),
              "w_mod": wmod, "b_mod": bmod, "c": E["c"], "lam4": H(E["lam4"].ap()[layer]), "a_norm_g": H(E["a_norm_g"].ap()[layer]),
              "rbext": E["rbcore"], "rb_off": layer * 8 * RBW, "rb_w": RBW, "ident": E["ident"], "aident": E["aident"],
              "laminit": H(E["laminit"].ap()[layer]), "p2tab": E["p2tab"], "xo": xmid}
        emit_T(nc, TT, L + "T_", nt, phases=(stop[1:] if (stop or "").startswith("T") else "0ABCM"))
        nc.all_engine_barrier()
        if (stop or "").startswith("T"):
            return nc
        TM = {"x": xmid, "c": E["c"], "w_mod": wmod, "b_mod": bmod, "norm_g": H(E["norm2_g"].ap()[layer]),
              "router_w": E["router_w"], "router_b": E["router_b"], "w1": H(E["w1"].ap()[layer]), "w3": H(E["w3"].ap()[layer]),
              "w2": H(E["w2"].ap()[layer]), "ident": E["ident"], "final_g": E["final_g"], "xo": xnext}
        emit_M(nc, TM, L + "M_", nt, final=(layer == 1))
        nc.all_engine_barrier()
        xin = xnext
    return nc


BF = ml_dtypes.bfloat16
_CACHE = {}


def _core_rows(r, nt=16):
    return np.concatenate([np.arange((4 * t + r) * 128, (4 * t + r + 1) * 128) for t in range(nt)])


def _masks(r):
    k = np.arange(128)[:, None]
    q = np.arange(128)[None, :]
    zmT = np.zeros((128, 4, 128), np.float32)
    zq = np.zeros((128, 4, 128), np.float32)
    for j in range(4):
        if j == r:
            m = (k >= 64) & (q < 64)
        elif j > r:
            m = np.ones((128, 128), bool)
        else:
            m = np.zeros((128, 128), bool)
        zmT[:, j, :] = np.where(m, -BIG, 0.0)
        zq[:, j, :] = np.where(m.T, -1e30, 0.0)
    cm = np.full((128, 8, 128), -BIG, np.float32)
    for i in range(8):
        j = i - r
        if 1 <= j <= 3:
            cm[:, i, :] = 0.0
        elif j == 0:
            cm[:, i, :] = np.where((k < 64) & (q >= 64), -BIG, 0.0)
        elif j == 4:
            cm[:, i, :] = np.where((k >= 64) & (q < 64), -BIG, 0.0)
    return zmT.reshape(128, 512).astype(BF), zq.reshape(128, 512), np.ascontiguousarray(cm[::-1]).reshape(128, 1024)


def kernel(x, c, positions, norm1_g, norm2_g, w_mod, b_mod, w_in, lambda_q1, lambda_k1, lambda_q2, lambda_k2,
           a_norm_g, c_rel_bias, w_branch_a, w_branch_b, w_branch_c, w_out, router_w, router_b,
           exp_w1, exp_w3, exp_w2, final_g, _nt=16, _runner=None, _stop=None):
    f32 = np.float32
    nt = _nt
    A = lambda a: np.ascontiguousarray(np.asarray(a, f32))
    x = A(x); c = A(c); positions = np.asarray(positions, np.int32)
    cores = [(b, r) for b in range(2) for r in range(4)]
    rows = [_core_rows(r, nt) for r in range(4)]
    ident = np.eye(128, dtype=f32)
    lam_init = [0.8 - 0.6 * math.exp(-0.3 * l) for l in range(2)]
    rb = A(c_rel_bias)
    rbext = np.concatenate([rb, np.repeat(rb[:, :, 512:513], 511, axis=2)], axis=2)
    rbbig = np.zeros((2, 8, 3072), f32); rbbig[:, :, 1024:2048] = rbext
    shared = {
        "w_mod": A(w_mod), "b_mod": A(b_mod)[:, None, :], "norm1_g": A(norm1_g)[:, None, :], "norm2_g": A(norm2_g)[:, None, :],
        "w_in": A(w_in), "lam4": np.concatenate([A(lambda_q1), A(lambda_k1), A(lambda_q2), A(lambda_k2)], axis=1)[:, None, :],
        "a_norm_g": A(a_norm_g)[:, None, :], "laminit": np.array([[[l, 1.0 - l]] for l in lam_init], f32),
        "wa": A(w_branch_a), "wb": A(w_branch_b), "wc": A(w_branch_c), "w_out": A(w_out),
        "router_w": A(router_w), "router_b": A(router_b)[None, :], "w1": A(exp_w1), "w3": A(exp_w3), "w2": A(exp_w2),
        "final_g": A(final_g)[None, :], "ident": ident, "aident": np.ascontiguousarray(ident[::-1]),
        "ropeinv": (np.float32(10000.0) ** (-np.arange(32, dtype=f32) / np.float32(32))).astype(f32)[None, :],
        "p2tab": (2.0 ** -(np.arange(NIT) + 1.0))[None, :].astype(f32),
    }
    shared = {k_: np.ascontiguousarray(v) for k_, v in shared.items()}
    ims = []
    for (b, r) in cores:
        zmT, zq, cm = _masks(r)
        im = dict(shared)
        im.update({"x": np.ascontiguousarray(x[b][rows[r]]), "pos": np.ascontiguousarray(positions[b][rows[r]].reshape(nt, 128).T),
                   "c": np.ascontiguousarray(c[b].reshape(8, 128).T), "zmT": zmT, "zq": np.ascontiguousarray(zq), "cmaskT": cm,
                   "rbcore": np.ascontiguousarray(rbbig[:, :, 128 * r:128 * r + RBW])})
        ims.append(im)
    if ("F", nt) not in _CACHE:
        _CACHE[("F", nt)] = build_fused(nt, stop=_stop)
    if _runner is None:
        results = run_bass_kernel_spmd(_CACHE[("F", nt)], ims, core_ids=list(range(8))).results
    else:
        results = _runner(_CACHE[("F", nt)], ims)
    out = np.zeros((2, 512 * nt, 1024), f32)
    for ci, (b, r) in enumerate(cores):
        out[b][rows[r]] = np.asarray(results[ci]["out"], f32)
    return out
```

```python
import math
from contextlib import ExitStack
import numpy as np
import ml_dtypes
import concourse.bass as bass
import concourse.mybir as mybir
from concourse.bass_utils import run_bass_kernel_spmd

F32 = mybir.dt.float32
BF16 = mybir.dt.bfloat16
I32 = mybir.dt.int32
ALU = mybir.AluOpType
AF = mybir.ActivationFunctionType
AX = mybir.AxisListType

ENGS = ("pe", "act", "dve", "pool", "sp")


class Op:
    __slots__ = ("eng", "fn", "deps", "is_dma", "sem", "ticket", "needs_inc", "idx")

    def __init__(self, eng, fn, is_dma, sem):
        self.eng = eng
        self.fn = fn
        self.deps = []
        self.is_dma = is_dma
        self.sem = sem
        self.ticket = None
        self.needs_inc = False


class Rec:
    def __init__(self, nc):
        self.nc = nc
        self.streams = {e: [] for e in ENGS}
        self.last_w = {}
        self.readers = {}
        self.dma_count = {}
        self.all_ops = []

    def _add(self, op, r, w):
        deps = []
        for k in r:
            lw = self.last_w.get(k)
            if lw is not None:
                deps.append(lw)
        for k in w:
            lw = self.last_w.get(k)
            if lw is not None:
                deps.append(lw)
            deps.extend(self.readers.get(k, ()))
        seen = set()
        for d in deps:
            if d is op or id(d) in seen:
                continue
            seen.add(id(d))
            if d.eng == "pe" and op.eng == "pe" and not d.is_dma and not op.is_dma:
                continue
            op.deps.append(d)
            d.needs_inc = True
        for k in w:
            self.last_w[k] = op
            self.readers[k] = []
        for k in r:
            self.readers.setdefault(k, []).append(op)
        self.streams[op.eng].append(op)
        self.all_ops.append(op)
        return op

    def op(self, eng, fn, r=(), w=()):
        return self._add(Op(eng, fn, False, None), r, w)

    def dma(self, eng, out, in_, sem, r=(), w=()):
        o = Op(eng, lambda e: e.dma_start(out=out, in_=in_), True, sem)
        self.dma_count[sem] = self.dma_count.get(sem, 0) + 1
        o.ticket = self.dma_count[sem]
        return self._add(o, r, w)

    def emit(self):
        nc = self.nc
        cnt = {e: 0 for e in ENGS}
        for e in ENGS:
            for o in self.streams[e]:
                if o.is_dma:
                    continue
                if o.needs_inc:
                    cnt[e] += 1
                    o.ticket = cnt[e]
        order = {id(o): i for i, o in enumerate(self.all_ops)}
        dma_hist = {}
        for i, o in enumerate(self.all_ops):
            if o.is_dma:
                dma_hist.setdefault(o.sem, []).append((i, o.ticket))
        import bisect
        dma_keys = sorted(self.dma_count.keys(), key=str)
        from contextlib import ExitStack
        esem = {e: nc.alloc_semaphore(name=nc.make_name("s_" + e, add_next_id=True)) for e in ENGS}
        dsem = {k: nc.alloc_semaphore(name=nc.make_name("d_%d" % i, add_next_id=True)) for i, k in enumerate(dma_keys)}
        with ExitStack() as st:
            block = st.enter_context(nc.Block())

            def run_stream(ename):
                def body(e):
                    waited = {}
                    for o in self.streams[ename]:
                        me = order[id(o)]
                        for d in o.deps:
                            if d.is_dma:
                                hist = dma_hist[d.sem]
                                j = bisect.bisect_left(hist, (me, 0)) - 1
                                val = 16 * hist[j][1]
                                sem = dsem[d.sem]
                                key = ("d", d.sem)
                            else:
                                val = d.ticket
                                sem = esem[d.eng]
                                key = ("e", d.eng)
                            if waited.get(key, 0) >= val:
                                continue
                            waited[key] = val
                            e.wait_ge(sem, val)
                        ins = o.fn(e)
                        if o.is_dma:
                            ins.then_inc(dsem[o.sem], 16)
                        elif o.needs_inc:
                            ins.then_inc(esem[ename], 1)
                    if ename == "sp":
                        for k in getattr(self, "final_wait", ()):
                            e.wait_ge(dsem[k], 16 * self.dma_count[k])
                return body

            block.tensor(run_stream("pe"))
            block.scalar(run_stream("act"))
            block.vector(run_stream("dve"))
            block.gpsimd(run_stream("pool"))
            block.sync(run_stream("sp"))
        nc.all_engine_barrier()
        nc.clear_and_free_semaphores(list(esem.values()) + list(dsem.values()))
        nc.all_engine_barrier()


def dram_ap(t, offset, pattern):
    return bass.AP(t, offset, pattern)


D = 1024
NT = 16
TOK = NT * 128
INC = 7108
C_AQ, C_AK, C_AV, C_BQ, C_BK, C_BV, C_IQ, C_IK, C_IW, C_CQ, C_CK, C_CV, C_G = (
    0, 512, 1024, 1536, 2048, 2112, 2176, 2432, 2496, 2500, 3012, 3524, 4036)
EPS = 1e-6
TWO_PI = 2.0 * math.pi


def emit_P(nc, T, pfx, nt=NT):
    tok = nt * 128
    di = lambda n, s, d=F32: T[n]
    do = lambda n, s, d=BF16: T[n]
    x = di("x", [tok, D]); pos = di("pos", [128, nt], I32); cvec = di("c", [128, 8])
    w_mod = di("w_mod", [D, 6144]); b_mod = di("b_mod", [1, 6144]); norm_g = di("norm_g", [1, D])
    w_in = di("w_in", [D, INC]); ident = di("ident", [128, 128]); ropeinv = di("ropeinv", [1, 32])
    aqt = do("aqt", [4, 128, tok]); akt = do("akt", [4, 128, tok]); av = do("av", [tok, 516])
    bqt = do("bqt", [nt, 64, 1024]); bkt = do("bkt", [64, tok]); bv = do("bv", [tok, 65])
    iw = do("iw", [tok, 4], F32)
    cqt = do("cqt", [4, 128, tok]); ckt = do("ckt", [4, 128, tok]); cv = do("cv", [tok, 520])
    gates = do("gates", [tok, 3072], F32)

    R = Rec(nc)
    with ExitStack() as st, nc.allow_low_precision("bf16 matmul operands, fp32 accumulation"):
        sb = lambda n, s, d=F32: st.enter_context(nc.sbuf_tensor(pfx + n, s, d))
        ps = lambda n, s, d=F32: st.enter_context(nc.psum_tensor(pfx + n, s, d))
        cs = sb("cs", [128, 8]); ca = sb("ca", [128, 8]); CA = sb("CA", [128, 8, 128])
        modbc = sb("modbc", [128, 2048]); gs = sb("gs", [128, D])
        wsb = sb("wsb", [128, 8, INC], BF16)
        idf = sb("idf", [128, 128]); idb = sb("idb", [128, 128], BF16)
        inv = sb("inv", [128, 32]); posi = sb("posi", [128, nt], I32); posf = sb("posf", [128, nt])
        xt = [sb("xt%d" % i, [128, D]) for i in range(2)]
        junk = sb("junk", [128, D], BF16); ss = sb("ss", [128, 1]); rstd = sb("rstd", [128, 1]); rt = sb("rt", [128, 1])
        tmp = sb("tmp", [128, D]); ub = sb("ub", [128, D], BF16); uT = sb("uT", [128, D], BF16)
        pj = sb("pj", [128, INC])
        ang = sb("ang", [128, nt, 32]); ang2 = sb("ang2", [128, nt, 32]); kf = sb("kf", [128, nt, 32]); ki = sb("ki", [128, nt, 32], I32)
        SN = sb("SN", [128, nt, 32]); CN = sb("CN", [128, nt, 32])
        t1 = sb("t1", [128, 512]); t2 = sb("t2", [128, 512])
        rb = sb("rb", [128, C_CV], BF16)
        avb = sb("avb", [128, 4, 129], BF16); bvb = sb("bvb", [128, 65], BF16); cvb = sb("cvb", [128, 8, 65], BF16)
        iwb = sb("iwb", [128, 4]); cst = sb("cst", [128, 2])
        wm = [pj[:, b * 2048:(b + 1) * 2048].rearrange("p (ch n) -> p ch n", n=256) for b in range(2)]
        WMK = [["pj%d" % i for i in range(4)], ["pj%d" % i for i in range(4, 8)]]
        bmb = pj[:, 4096:6144]; BMK = ["pj%d" % i for i in range(8, 12)]
        gbc = tmp
        tA = [sb("tA%d" % i, [128, 1024], BF16) for i in range(2)]
        pT = ps("pT", [128, 1024], BF16)
        pp = [ps("pp%d" % i, [128, 512]) for i in range(2)]
        pX = [ps("pX%d" % i, [128, 1024], BF16) for i in range(2)]

        R.dma("sp", cs[:], cvec.ap(), "c", w=["cs"])
        R.op("act", lambda e: e.activation(out=ca[:], in_=cs[:], func=AF.Silu), r=["cs"], w=["ca"])
        R.op("dve", lambda e: e.tensor_copy(out=CA[:], in_=ca[:].unsqueeze(2).broadcast_to([128, 8, 128])), r=["ca"], w=["CA"])
        R.dma("sp", bmb, b_mod.ap()[0:1, 0:2048].partition_broadcast(128), "c", w=BMK)
        R.dma("sp", gbc[:], norm_g.ap()[0:1, :].partition_broadcast(128), "c", w=["tmp"])
        R.dma("sp", idf[:], ident.ap(), "c", w=["idf"])
        R.dma("sp", inv[:], ropeinv.ap()[0:1, :].partition_broadcast(128), "c", w=["inv"])
        R.dma("sp", posi[:], pos.ap(), "c", w=["posi"])
        R.op("dve", lambda e: e.tensor_copy(out=idb[:], in_=idf[:]), r=["idf"], w=["idb"])
        R.op("dve", lambda e: e.tensor_copy(out=posf[:], in_=posi[:]), r=["posi"], w=["posf"])
        R.op("pool", lambda e: e.memset(cst[:, 0:1], EPS), w=["cst"])
        R.op("pool", lambda e: e.memset(cst[:, 1:2], math.pi), w=["cst"])
        R.op("pool", lambda e: e.memset(avb[:], 1.0), w=["avb"])
        R.op("pool", lambda e: e.memset(bvb[:], 1.0), w=["bvb"])
        R.op("pool", lambda e: e.memset(cvb[:], 1.0), w=["cvb"])
        R.op("dve", lambda e: e.tensor_tensor(out=ang[:], in0=inv[:].unsqueeze(1).broadcast_to([128, nt, 32]),
                                              in1=posf[:].unsqueeze(2).broadcast_to([128, nt, 32]), op=ALU.mult), r=["inv", "posf"], w=["ang"])
        R.op("dve", lambda e: e.tensor_scalar(out=ang2[:], in0=ang[:], scalar1=math.pi / 2, scalar2=None, op0=ALU.add), r=["ang"], w=["ang2"])
        for (src, dst, nm) in ((ang, SN, "SN"), (ang2, CN, "CN")):
            sk = "ang" if src is ang else "ang2"
            R.op("dve", lambda e, src=src: e.tensor_scalar(out=ki[:], in0=src[:], scalar1=1.0 / TWO_PI, scalar2=None, op0=ALU.mult), r=[sk], w=["ki"])
            R.op("dve", lambda e: e.tensor_copy(out=kf[:], in_=ki[:]), r=["ki"], w=["kf"])
            R.op("dve", lambda e, src=src: e.scalar_tensor_tensor(out=kf[:], in0=kf[:], scalar=-TWO_PI, in1=src[:], op0=ALU.mult, op1=ALU.add),
                 r=["kf", sk], w=["kf"])
            R.op("dve", lambda e: e.tensor_scalar(out=kf[:], in0=kf[:], scalar1=3.14159, scalar2=-3.14159, op0=ALU.min, op1=ALU.max), r=["kf"], w=["kf"])
            R.op("act", lambda e, dst=dst: e.activation(out=dst[:], in_=kf[:], func=AF.Sin), r=["kf"], w=[nm])
        wmv = w_mod.ap().rearrange("(ch p) n -> p ch n", p=128)
        for j in range(8):
            b = j % 2
            R.dma("sp", wm[b], wmv[:, :, j * 256:(j + 1) * 256], "wm%d" % b, w=WMK[b])
            for ch in range(8):
                R.op("pe", lambda e, ch=ch, b=b: e.matmul(pp[b][:, 0:256], lhsT=CA[:, ch, :], rhs=wm[b][:, ch, :],
                                                          start=(ch == 0), stop=(ch == 7)),
                     r=["CA"] + WMK[b], w=["pp%d" % b])
            R.op("dve", lambda e, j=j, b=b: e.tensor_tensor(out=modbc[:, j * 256:(j + 1) * 256], in0=pp[b][:, 0:256],
                                                            in1=bmb[:, j * 256:(j + 1) * 256], op=ALU.add),
                 r=["pp%d" % b] + BMK, w=["modbc"])
        R.op("dve", lambda e: e.scalar_tensor_tensor(out=gs[:], in0=modbc[:, 1024:2048], scalar=1.0, in1=gbc[:],
                                                     op0=ALU.add, op1=ALU.mult), r=["modbc", "tmp"], w=["gs"])
        wiv = w_in.ap().rearrange("(ch p) n -> p ch n", p=128)
        NCHK = (INC + 511) // 512
        for n_ in range(NCHK):
            c0_, c1_ = n_ * 512, min(INC, (n_ + 1) * 512)
            R.dma("pool", wsb[:, :, c0_:c1_], wiv[:, :, c0_:c1_], "wsb", w=["wsbn%d" % n_])

        for t in range(nt):
            xb = t % 2
            xk = "xt%d" % xb
            R.dma("sp", xt[xb][:], x.ap()[t * 128:(t + 1) * 128, :], xk, w=[xk])
            R.op("act", lambda e, xb=xb: e.activation(out=junk[:], in_=xt[xb][:], func=AF.Square, accum_out=ss[:]),
                 r=[xk], w=["junk", "ss"])
            R.op("act", lambda e: e.activation(out=rt[:], in_=ss[:], func=AF.Sqrt, scale=1.0 / D, bias=cst[:, 0:1]),
                 r=["ss", "cst"], w=["rt"])
            R.op("dve", lambda e: e.reciprocal(out=rstd[:], in_=rt[:]), r=["rt"], w=["rstd"])
            R.op("dve", lambda e, xb=xb: e.scalar_tensor_tensor(out=tmp[:], in0=xt[xb][:], scalar=rstd[:, 0:1], in1=gs[:],
                                                                op0=ALU.mult, op1=ALU.mult), r=[xk, "rstd", "gs"], w=["tmp"])
            R.op("dve", lambda e: e.tensor_tensor(out=ub[:], in0=tmp[:], in1=modbc[:, 0:1024], op=ALU.add),
                 r=["tmp", "modbc"], w=["ub"])
            for ch in range(8):
                R.op("pe", lambda e, ch=ch: e.transpose(out=pT[:, ch * 128:(ch + 1) * 128], in_=ub[:, ch * 128:(ch + 1) * 128],
                                                        identity=idb[:]), r=["ub", "idb"], w=["pT"])
            R.op("act", lambda e: e.copy(out=uT[:], in_=pT[:]), r=["pT"], w=["uT"])
            nchunks = (INC + 511) // 512
            for n in range(nchunks):
                n0, n1 = n * 512, min(INC, (n + 1) * 512)
                b = n % 2
                for ch in range(8):
                    R.op("pe", lambda e, ch=ch, b=b, n0=n0, n1=n1: e.matmul(pp[b][:, 0:n1 - n0], lhsT=uT[:, ch * 128:(ch + 1) * 128],
                                                                              rhs=wsb[:, ch, n0:n1], start=(ch == 0), stop=(ch == 7)),
                         r=["uT", "wsbn%d" % n], w=["pp%d" % b])
                eng = "act" if n % 2 == 0 else "dve"
                if eng == "act":
                    R.op("act", lambda e, b=b, n0=n0, n1=n1: e.copy(out=pj[:, n0:n1], in_=pp[b][:, 0:n1 - n0]),
                         r=["pp%d" % b], w=["pj%d" % n])
                else:
                    R.op("dve", lambda e, b=b, n0=n0, n1=n1: e.tensor_copy(out=pj[:, n0:n1], in_=pp[b][:, 0:n1 - n0]),
                         r=["pp%d" % b], w=["pj%d" % n])
            PJ = lambda c0, c1: ["pj%d" % n for n in range(c0 // 512, (c1 - 1) // 512 + 1)]
            for (c0, H) in ((C_AQ, 16), (C_BQ, 9), (C_IQ, 5)):
                c1 = c0 + 64 * H
                xv = pj[:, c0:c1].rearrange("p (h two d) -> p h two d", two=2, d=32)
                ov = rb[:, c0:c1].rearrange("p (h two d) -> p h two d", two=2, d=32)
                x1, x2 = xv[:, :, 0, :], xv[:, :, 1, :]
                o1, o2 = ov[:, :, 0, :], ov[:, :, 1, :]
                cb = CN[:, t:t + 1, :].broadcast_to([128, H, 32])
                sbv = SN[:, t:t + 1, :].broadcast_to([128, H, 32])
                a1 = t1[:, 0:32 * H].rearrange("p (h d) -> p h d", d=32)
                a2 = t2[:, 0:32 * H].rearrange("p (h d) -> p h d", d=32)
                rk = PJ(c0, c1)
                R.op("dve", lambda e, a1=a1, x1=x1, cb=cb: e.tensor_tensor(out=a1, in0=x1, in1=cb, op=ALU.mult), r=rk + ["CN"], w=["t1"])
                R.op("dve", lambda e, a2=a2, x2=x2, sbv=sbv: e.tensor_tensor(out=a2, in0=x2, in1=sbv, op=ALU.mult), r=rk + ["SN"], w=["t2"])
                R.op("dve", lambda e, o1=o1, a1=a1, a2=a2: e.tensor_tensor(out=o1, in0=a1, in1=a2, op=ALU.subtract), r=["t1", "t2"], w=["rb"])
                R.op("dve", lambda e, a1=a1, x2=x2, cb=cb: e.tensor_tensor(out=a1, in0=x2, in1=cb, op=ALU.mult), r=rk + ["CN"], w=["t1"])
                R.op("dve", lambda e, a2=a2, x1=x1, sbv=sbv: e.tensor_tensor(out=a2, in0=x1, in1=sbv, op=ALU.mult), r=rk + ["SN"], w=["t2"])
                R.op("dve", lambda e, o2=o2, a1=a1, a2=a2: e.tensor_tensor(out=o2, in0=a1, in1=a2, op=ALU.add), r=["t1", "t2"], w=["rb"])
            R.op("pool", lambda e: e.tensor_copy(out=rb[:, C_CQ:C_CV], in_=pj[:, C_CQ:C_CV]), r=PJ(C_CQ, C_CV), w=["rb"])
            R.op("pool", lambda e: e.tensor_copy(out=avb[:, :, 0:128], in_=pj[:, C_AV:C_BQ].rearrange("p (h d) -> p h d", d=128)),
                 r=PJ(C_AV, C_BQ), w=["avb"])
            R.op("pool", lambda e: e.tensor_copy(out=bvb[:, 0:64], in_=pj[:, C_BV:C_IQ]), r=PJ(C_BV, C_IQ), w=["bvb"])
            R.op("pool", lambda e: e.tensor_copy(out=cvb[:, :, 0:64], in_=pj[:, C_CV:C_G].rearrange("p (h d) -> p h d", d=64)),
                 r=PJ(C_CV, C_G), w=["cvb"])
            R.op("pool", lambda e: e.tensor_scalar(out=iwb[:], in0=pj[:, C_IW:C_CQ], scalar1=1.0 / 16.0, scalar2=None, op0=ALU.mult),
                 r=PJ(C_IW, C_CQ), w=["iwb"])
            R.op("act", lambda e: e.activation(out=pj[:, C_G:INC], in_=pj[:, C_G:INC], func=AF.Sigmoid), r=PJ(C_G, INC), w=PJ(C_G, INC))
            ts = slice(t * 128, (t + 1) * 128)
            R.dma("sp", av.ap()[ts, :], avb[:].rearrange("p h d -> p (h d)"), "out", r=["avb"])
            R.dma("sp", bv.ap()[ts, :], bvb[:], "out", r=["bvb"])
            R.dma("sp", cv.ap()[ts, :], cvb[:].rearrange("p h d -> p (h d)"), "out", r=["cvb"])
            R.dma("sp", iw.ap()[ts, :], iwb[:], "out", r=["iwb"])
            R.dma("sp", gates.ap()[ts, :], pj[:, C_G:INC], "out", r=PJ(C_G, INC))
            g = 0
            for blk in range(8):
                c0 = blk * 128
                R.op("pe", lambda e, blk=blk, c0=c0: e.transpose(out=pX[0][:, blk * 128:(blk + 1) * 128], in_=rb[:, c0:c0 + 128], identity=idb[:]),
                     r=["rb", "idb"], w=["pX0"])
            R.op("act", lambda e: e.copy(out=tA[0][:], in_=pX[0][:]), r=["pX0"], w=["tA0"])
            for m in range(2):
                R.dma("sp", bass.AP(aqt, m * tok + t * 128, [[2 * tok, 64], [64 * 2 * tok, 4], [1, 128]]),
                      tA[0][m * 64:(m + 1) * 64, 0:512].rearrange("p (h q) -> p h q", q=128), "out", r=["tA0"])
                R.dma("sp", bass.AP(akt, m * tok + t * 128, [[2 * tok, 64], [64 * 2 * tok, 4], [1, 128]]),
                      tA[0][m * 64:(m + 1) * 64, 512:1024].rearrange("p (h q) -> p h q", q=128), "out", r=["tA0"])
            for blk in range(8):
                c0 = C_CQ + blk * 128
                R.op("pe", lambda e, blk=blk, c0=c0: e.transpose(out=pX[1][:, blk * 128:(blk + 1) * 128], in_=rb[:, c0:c0 + 128], identity=idb[:]),
                     r=["rb", "idb"], w=["pX1"])
            R.op("dve", lambda e: e.tensor_copy(out=tA[1][:], in_=pX[1][:]), r=["pX1"], w=["tA1"])
            for hf in range(2):
                R.dma("sp", bass.AP(cqt, hf * tok + t * 128, [[8 * tok, 64], [2 * tok, 4], [1, 128]]),
                      tA[1][hf * 64:(hf + 1) * 64, 0:512].rearrange("p (h q) -> p h q", q=128), "out", r=["tA1"])
                R.dma("sp", bass.AP(ckt, t * 1024 + hf * 128, [[8 * tok, 64], [256, 4], [1, 128]]),
                      tA[1][hf * 64:(hf + 1) * 64, 512:1024].rearrange("p (h q) -> p h q", q=128), "out", r=["tA1"])
            for h in range(8):
                c0 = C_BQ + h * 64
                R.op("pe", lambda e, h=h, c0=c0: e.transpose(out=pX[0][0:64, h * 128:(h + 1) * 128], in_=rb[:, c0:c0 + 64], identity=idb[:]),
                     r=["rb", "idb"], w=["pX0"])
            R.op("act", lambda e: e.copy(out=tA[0][0:64, :], in_=pX[0][0:64, :]), r=["pX0"], w=["tA0"])
            R.dma("sp", bqt.ap()[t][:, 0:1024], tA[0][0:64, :], "out", r=["tA0"])
            srcs = [C_IQ + h * 64 for h in range(4)] + [C_BK, C_IK]
            for i, c0 in enumerate(srcs):
                R.op("pe", lambda e, i=i, c0=c0: e.transpose(out=pX[1][0:64, i * 128:(i + 1) * 128], in_=rb[:, c0:c0 + 64], identity=idb[:]),
                     r=["rb", "idb"], w=["pX1"])
            R.op("dve", lambda e: e.tensor_copy(out=tA[1][0:64, 0:768], in_=pX[1][0:64, 0:768]), r=["pX1"], w=["tA1"])
            R.dma("sp", bqt.ap()[t][:, 1024:1536], tA[1][0:64, 0:512], "out", r=["tA1"])
            R.dma("sp", bkt.ap()[:, t * 128:(t + 1) * 128], tA[1][0:64, 512:640], "out", r=["tA1"])
            R.dma("sp", bkt.ap()[:, tok + t * 128:tok + (t + 1) * 128], tA[1][0:64, 640:768], "out", r=["tA1"])
        R.final_wait = ["out"]
        R.emit()


D = 1024
BIG = 30000.0
NIT = 22
EPS = 1e-6


def emit_T(nc, T, pfx, nt=16, phases="0ABCM"):
    tok = nt * 128
    NKT = 4 * nt
    S = NKT * 128
    di = lambda n, s, d=F32: T[n]
    aqt = di("aqt", [4, 64, 2 * tok], BF16); bq_iq = di("bq_iq", [nt, 64, 1536], BF16); iw = di("iw", [tok, 4])
    cqt = di("cqt", [64, 8 * tok], BF16); gates = di("gates", [tok, 3072]); x = di("x", [tok, D])
    akt = di("akt", [4, 64, 2 * S], BF16); av = di("av", [4, 128, NKT * 129], BF16); bkik = di("bkik", [64, 2 * S], BF16)
    bv = di("bv", [128, NKT * 65], BF16); ckb = di("ckb", [nt, 64, 8 * 640], BF16); cvb = di("cvb", [nt, 128, 5 * 520], BF16)
    zmT = di("zmT", [128, 512], BF16); zq = di("zq", [128, 512]); cmaskT = di("cmaskT", [128, 8 * 128])
    wa = di("wa", [512, D]); wb = di("wb", [512, D]); wc = di("wc", [512, D]); w_out = di("w_out", [D, D])
    w_mod = di("w_mod", [D, 6144]); b_mod = di("b_mod", [1, 6144]); cvec = di("c", [128, 8])
    lam4 = di("lam4", [1, 256]); ang_in = di("a_norm_g", [1, 128]); rbext = di("rbext", [8, 1024])
    ident = di("ident", [128, 128]); aident = di("aident", [128, 128]); laminit = di("laminit", [1, 2]); p2tab = di("p2tab", [1, NIT])
    xo = T["xo"]

    with ExitStack() as st, nc.allow_low_precision("bf16 matmul operands, fp32 accumulation"):
        sbo = lambda n, s, d=F32: st.enter_context(nc.sbuf_tensor(pfx + n, s, d))
        pso = lambda n, s, d=F32: st.enter_context(nc.psum_tensor(pfx + n, s, d))
        ya = sbo("ya", [128, nt, 512], BF16); yb = sbo("yb", [128, nt, 512], BF16); yc = sbo("yc", [128, nt, 512], BF16)
        g1bc = sbo("g1bc", [128, D]); idf = sbo("idf", [128, 128]); idb = sbo("idb", [128, 128], BF16); jdb = sbo("jdb", [128, 128], BF16)
        bigi4 = sbo("bigi4", [128, 512], BF16); nlam = sbo("nlam", [128, 1]); gn = sbo("gn", [128, 128])
        cst = sbo("cst", [128, 2])
        psS = [pso("psS%d" % i, [128, 1024]) for i in range(2)]
        psO = [pso("psO%d" % i, [128, 1024]) for i in range(2)]

        R = Rec(nc)
        with ExitStack() as s0:
            sb = lambda n, s, d=F32: s0.enter_context(nc.sbuf_tensor(pfx + n, s, d))
            cs = sb("cs", [128, 8]); ca = sb("ca", [128, 8]); CA = sb("CA", [128, 8, 128])
            wm = [sb("wm%d" % i, [128, 8, 256]) for i in range(2)]
            bmb = sb("bmb", [128, D]); l4 = sb("l4", [128, 4, 64]); lt = sb("lt", [128, 2, 64]); ls = sb("ls", [128, 2]); le = sb("le", [128, 2])
            li = sb("li", [128, 2]); agb = sb("agb", [128, 128]); stg = sb("stg", [128, 8, 128])
            R.dma("sp", cs[:], cvec.ap(), "c", w=["cs"])
            R.dma("sp", idf[:], ident.ap(), "c", w=["idf"])
            R.dma("sp", bmb[:], b_mod.ap()[0:1, 2048:3072].partition_broadcast(128), "c", w=["bmb"])
            R.dma("sp", l4[:].rearrange("p a b -> p (a b)"), lam4.ap()[0:1, :].partition_broadcast(128), "c", w=["l4"])
            R.dma("sp", li[:], laminit.ap()[0:1, :].partition_broadcast(128), "c", w=["li"])
            R.dma("sp", agb[:], ang_in.ap()[0:1, :].partition_broadcast(128), "c", w=["agb"])
            R.op("pool", lambda e: e.memset(cst[:, 0:1], EPS), w=["cst"])
            R.op("act", lambda e: e.activation(out=ca[:], in_=cs[:], func=AF.Silu), r=["cs"], w=["ca"])
            R.op("dve", lambda e: e.tensor_copy(out=CA[:], in_=ca[:].unsqueeze(2).broadcast_to([128, 8, 128])), r=["ca"], w=["CA"])
            R.op("dve", lambda e: e.tensor_copy(out=idb[:], in_=idf[:]), r=["idf"], w=["idb"])
            R.dma("sp", stg[:, 0, :], aident.ap(), "c", w=["stg"])
            R.op("dve", lambda e: e.tensor_copy(out=jdb[:], in_=stg[:, 0, :]), r=["stg"], w=["jdb"])
            for k in range(4):
                R.op("dve", lambda e, k=k: e.tensor_scalar(out=bigi4[:, k * 128:(k + 1) * 128], in0=idf[:], scalar1=BIG, scalar2=None, op0=ALU.mult),
                     r=["idf"], w=["bigi4"])
            wmv = w_mod.ap().rearrange("(ch p) n -> p ch n", p=128)
            for j in range(4):
                b = j % 2
                R.dma("sp", wm[b][:], wmv[:, :, 2048 + j * 256:2048 + (j + 1) * 256], "wm%d" % b, w=["wm%d" % b])
                for ch in range(8):
                    R.op("pe", lambda e, ch=ch, b=b: e.matmul(psS[b][:, 0:256], lhsT=CA[:, ch, :], rhs=wm[b][:, ch, :], start=(ch == 0), stop=(ch == 7)),
                         r=["CA", "wm%d" % b], w=["psS%d" % b])
                R.op("dve", lambda e, j=j, b=b: e.tensor_tensor(out=g1bc[:, j * 256:(j + 1) * 256], in0=psS[b][:, 0:256], in1=bmb[:, j * 256:(j + 1) * 256], op=ALU.add),
                     r=["psS%d" % b, "bmb"], w=["g1bc"])
            R.op("dve", lambda e: e.tensor_tensor(out=lt[:, 0, :], in0=l4[:, 0, :], in1=l4[:, 1, :], op=ALU.mult), r=["l4"], w=["lt"])
            R.op("dve", lambda e: e.tensor_tensor(out=lt[:, 1, :], in0=l4[:, 2, :], in1=l4[:, 3, :], op=ALU.mult), r=["l4"], w=["lt"])
            R.op("dve", lambda e: e.tensor_reduce(out=ls[:], in_=lt[:], axis=AX.X, op=ALU.add), r=["lt"], w=["ls"])
            R.op("act", lambda e: e.activation(out=le[:], in_=ls[:], func=AF.Exp), r=["ls"], w=["le"])
            R.op("dve", lambda e: e.tensor_tensor(out=nlam[:], in0=le[:, 1:2], in1=le[:, 0:1], op=ALU.subtract), r=["le"], w=["nlam"])
            R.op("dve", lambda e: e.tensor_tensor(out=nlam[:], in0=nlam[:], in1=li[:, 0:1], op=ALU.subtract), r=["nlam", "li"], w=["nlam"])
            R.op("dve", lambda e: e.tensor_scalar(out=gn[:], in0=agb[:], scalar1=li[:, 1:2], scalar2=None, op0=ALU.mult), r=["agb", "li"], w=["gn"])
            R.final_wait = list(R.dma_count.keys())
            if "0" in phases:
                R.emit()
        nc.all_engine_barrier()

        def attn_epilogue_BC(R, po, pk, ydst, yk, rinv):
            pv = po[:].rearrange("p (a c) -> p a c", a=2)[:, :, 0:260].rearrange("p a (h e) -> p a h e", e=65)
            R.op("dve", lambda e: e.reciprocal(out=rinv[:].rearrange("p (a h) -> p a h", a=2), in_=pv[:, :, :, 64]), r=[pk], w=["rinv"])
            R.op("dve", lambda e: e.tensor_tensor(out=ydst.rearrange("p (a h e) -> p a h e", a=2, e=64), in0=pv[:, :, :, 0:64],
                                                  in1=rinv[:].rearrange("p (a h) -> p a h", a=2).unsqueeze(3).broadcast_to([128, 2, 4, 64]), op=ALU.mult),
                 r=[pk, "rinv"], w=[yk])

        def hoff(h):
            return (h // 4) * 512 + (h % 4) * 65

        R = Rec(nc)
        with ExitStack() as s1:
            sb = lambda n, s, d=F32: s1.enter_context(nc.sbuf_tensor(pfx + n, s, d))
            ktb = [sb("ktb%d" % i, [64, 2, S], BF16) for i in range(2)]
            avh = [sb("avh%d" % i, [128, NKT, 129], BF16) for i in range(2)]
            qh = [sb("qh%d" % i, [64, 2, tok], BF16) for i in range(2)]
            zm = sb("zm", [128, 4, 128], BF16)
            pt = [sb("pt%d" % i, [128, 4, 2, 128], BF16) for i in range(2)]
            r12 = sb("r12", [128, 2]); nr2 = sb("nr2", [128, 1]); d1 = sb("d1", [128, 128]); dd = sb("dd", [128, 128])
            jk = sb("jk", [128, 128]); ssq = sb("ssq", [128, 1]); lnv = sb("lnv", [128, 1]); rstd = sb("rstd", [128, 1])
            R.dma("sp", zm[:].rearrange("p a b -> p (a b)"), zmT.ap(), "c", w=["zm"])
            gi = 0
            for h in range(4):
                hb = h % 2
                for m_ in range(2):
                    for r_ in range(4):
                        R.dma("sp", ktb[hb][:, m_, :].rearrange("d (t r p) -> d t r p", r=4, p=128)[:, :, r_, :],
                              bass.AP(akt[h // 2], ((r_ * 2 + h % 2) * 64) * 2 * tok + m_ * tok, [[2 * tok, 64], [128, nt], [1, 128]]), "kt%d" % hb, w=["ktb%d" % hb])
                SL = min(4, nt)
                for c_ in range(nt // SL):
                    for r_ in range(4):
                        R.dma("sp", avh[hb][:].rearrange("p (t r) e -> p t r e", r=4)[:, c_ * SL:(c_ + 1) * SL, r_, :],
                              bass.AP(av[c_], r_ * SL * 128 * 516 + h * 129, [[516, 128], [128 * 516, SL], [1, 129]]), "av%d" % hb, w=["avh%d" % hb])
                R.dma("sp", qh[hb][:].rearrange("p a b -> p (a b)"), aqt.ap()[h], "qh%d" % hb, w=["qh%d" % hb])
                jobs = [(t, g) for t in range(nt) for g in range(t + 1)]

                def a_qk(t, g, b, hb=hb):
                    qs = slice(t * 128, (t + 1) * 128)
                    zone = (g == t)
                    for i in range(4):
                        kt = 4 * g + i
                        ks = slice(kt * 128, (kt + 1) * 128)
                        for m in range(2):
                            ps_out = psS[b][:, (i * 2 + m) * 128:(i * 2 + m + 1) * 128]
                            R.op("pe", lambda e, ps_out=ps_out, m=m, ks=ks, qs=qs, zone=zone: e.matmul(
                                ps_out, lhsT=ktb[hb][:, m, ks], rhs=qh[hb][:, m, qs], start=True, stop=not zone),
                                r=["ktb%d" % hb, "qh%d" % hb], w=["psS%d" % b])
                            if zone:
                                R.op("pe", lambda e, ps_out=ps_out, i=i: e.matmul(ps_out, lhsT=idb[:], rhs=zm[:, i, :], start=False, stop=True),
                                     r=["zm"], w=["psS%d" % b])

                def a_pv(t, g, b, hb=hb):
                    ob = t % 2
                    ok = "psO%d" % ob
                    R.op("act", lambda e: e.activation(out=pt[b][:].rearrange("p a m q -> p (a m q)"), in_=psS[b][:], func=AF.Exp, scale=0.125),
                         r=["psS%d" % b], w=["pt%d" % b])
                    for i in range(4):
                        kt = 4 * g + i
                        for m in range(2):
                            R.op("pe", lambda e, i=i, m=m, kt=kt: e.matmul(
                                psO[ob][:, m * 512:m * 512 + 129], lhsT=pt[b][:, i, m, :], rhs=avh[hb][:, kt, :],
                                start=(g == 0 and i == 0), stop=(g == t and i == 3)),
                                r=["pt%d" % b, "avh%d" % hb], w=[ok])
                    if g != t:
                        return
                    po = psO[ob]
                    R.op("dve", lambda e: e.reciprocal(out=r12[:, 0:1], in_=po[:, 128:129]), r=[ok], w=["r12"])
                    R.op("dve", lambda e: e.reciprocal(out=r12[:, 1:2], in_=po[:, 640:641]), r=[ok], w=["r12"])
                    R.op("dve", lambda e: e.tensor_tensor(out=nr2[:], in0=r12[:, 1:2], in1=nlam[:], op=ALU.mult), r=["r12"], w=["nr2"])
                    R.op("dve", lambda e: e.tensor_scalar(out=d1[:], in0=po[:, 0:128], scalar1=r12[:, 0:1], scalar2=None, op0=ALU.mult),
                         r=[ok, "r12"], w=["d1"])
                    R.op("dve", lambda e: e.scalar_tensor_tensor(out=dd[:], in0=po[:, 512:640], scalar=nr2[:, 0:1], in1=d1[:], op0=ALU.mult, op1=ALU.add),
                         r=[ok, "nr2", "d1"], w=["dd"])

                def a_norm(t, h=h):
                    R.op("act", lambda e: e.activation(out=jk[:], in_=dd[:], func=AF.Square, accum_out=ssq[:]), r=["dd"], w=["jk", "ssq"])
                    R.op("act", lambda e: e.activation(out=lnv[:], in_=ssq[:], func=AF.Ln, scale=1.0 / 128.0, bias=cst[:, 0:1]), r=["ssq"], w=["lnv"])
                    R.op("act", lambda e: e.activation(out=rstd[:], in_=lnv[:], func=AF.Exp, scale=-0.5), r=["lnv"], w=["rstd"])
                    R.op("dve", lambda e: e.scalar_tensor_tensor(out=ya[:, t, h * 128:(h + 1) * 128], in0=dd[:], scalar=rstd[:, 0:1], in1=gn[:],
                                                                 op0=ALU.mult, op1=ALU.mult), r=["dd", "rstd"], w=["ya"])

                pend = None
                a_qk(jobs[0][0], jobs[0][1], gi % 2)
                for ji, (t, g) in enumerate(jobs):
                    b = gi % 2
                    gi += 1
                    if ji + 1 < len(jobs):
                        a_qk(jobs[ji + 1][0], jobs[ji + 1][1], gi % 2)
                    a_pv(t, g, b)
                    if pend is not None:
                        a_norm(pend)
                        pend = None
                    if g == t:
                        pend = t
                if pend is not None:
                    a_norm(pend)
            R.final_wait = list(R.dma_count.keys())
            if "A" in phases:
                R.emit()
        nc.all_engine_barrier()

        R = Rec(nc)
        with ExitStack() as s2:
            sb = lambda n, s, d=F32: s2.enter_context(nc.sbuf_tensor(pfx + n, s, d))
            kk = sb("kk", [64, 2, S], BF16); bvs = sb("bvs", [128, NKT, 65], BF16)
            sc = sb("sc", [128, S]); cA = sb("cA", [128, S], BF16); cB = sb("cB", [128, S], BF16)
            mk = [sb("mk%d" % i, [128, S], BF16) for i in range(2)]
            bqi = [sb("bqi%d" % i, [64, 1536], BF16) for i in range(2)]
            iwt = [sb("iwt%d" % i, [128, 4]) for i in range(2)]
            pt = [sb("ptb%d" % i, [128, 1024], BF16) for i in range(2)]
            rl = [sb("rl%d" % i, [128, 512]) for i in range(2)] * 2
            zqs = sb("zqs", [128, 512]); p2 = sb("p2", [128, NIT]); WN = sb("WN", [128, NIT]); NWN = sb("NWN", [128, NIT])
            rmin = sb("rmin", [128, 1]); rmax = sb("rmax", [128, 1]); w0 = sb("w0", [128, 1]); lo = sb("lo", [128, 1]); mid = sb("mid", [128, 1])
            cnt = sb("cnt", [128, 1]); dl = sb("dl", [128, 1]); hi = sb("hi", [128, 1]); chi = sb("chi", [128, 1]); mrem = sb("mrem", [128, 1])
            rinv = sb("rinv", [128, 8])
            for m_ in range(2):
                for r_ in range(4):
                    R.dma("sp", kk[:, m_, :].rearrange("d (t r p) -> d t r p", r=4, p=128)[:, :, r_, :],
                          bass.AP(bkik, r_ * 64 * 2 * tok + m_ * tok, [[2 * tok, 64], [128, nt], [1, 128]]), "c", w=["kk"])
            for r_ in range(4):
                R.dma("sp", bvs[:].rearrange("p (t r) e -> p t r e", r=4)[:, :, r_, :],
                      bass.AP(bv, r_ * tok * 65, [[65, 128], [128 * 65, nt], [1, 65]]), "c", w=["bvs"])
            R.dma("sp", zqs[:], zq.ap(), "c", w=["zqs"])
            R.dma("sp", p2[:], p2tab.ap()[0:1, :].partition_broadcast(128), "c", w=["p2"])
            psI = [psS[hh // 2][:, (hh % 2) * 512:(hh % 2) * 512 + 512] for hh in range(4)]
            def b_select(t):
                b = t % 2
                n = (4 * t + 4) * 128
                R.dma("sp", bqi[b][:], bq_iq.ap()[t], "bqi%d" % b, w=["bqi%d" % b])
                R.dma("sp", iwt[b][:], iw.ap()[t * 128:(t + 1) * 128, :], "bqi%d" % b, w=["iwt%d" % b])
                for g in range(t + 1):
                    gs_ = slice(g * 512, (g + 1) * 512)
                    for hh in range(4):
                        R.op("pe", lambda e, hh=hh, b=b, gs_=gs_: e.matmul(psI[hh], lhsT=bqi[b][:, 1024 + hh * 128:1024 + (hh + 1) * 128], rhs=kk[:, 1, gs_],
                                                                           start=True, stop=True), r=["bqi%d" % b, "kk"], w=["psS%d" % (hh // 2)])
                        R.op("act", lambda e, hh=hh: e.activation(out=rl[hh][:], in_=psI[hh], func=AF.Relu), r=["psS%d" % (hh // 2)], w=["rl%d" % (hh % 2)])
                        if hh == 0:
                            R.op("dve", lambda e, b=b, gs_=gs_: e.tensor_scalar(out=sc[:, gs_], in0=rl[0][:], scalar1=iwt[b][:, 0:1], scalar2=None, op0=ALU.mult),
                                 r=["rl0", "iwt%d" % b], w=["sc"])
                        else:
                            R.op("dve", lambda e, hh=hh, b=b, gs_=gs_: e.scalar_tensor_tensor(out=sc[:, gs_], in0=rl[hh][:], scalar=iwt[b][:, hh:hh + 1], in1=sc[:, gs_],
                                                                                               op0=ALU.mult, op1=ALU.add), r=["rl%d" % (hh % 2), "iwt%d" % b, "sc"], w=["sc"])
                R.op("dve", lambda e, n=n: e.tensor_reduce(out=rmin[:], in_=sc[:, 0:n], axis=AX.X, op=ALU.min), r=["sc"], w=["rmin"])
                R.op("dve", lambda e, t=t: e.tensor_tensor(out=sc[:, t * 512:(t + 1) * 512], in0=sc[:, t * 512:(t + 1) * 512], in1=zqs[:], op=ALU.add),
                     r=["sc", "zqs"], w=["sc"])
                R.op("dve", lambda e, n=n: e.tensor_reduce(out=rmax[:], in_=sc[:, 0:n], axis=AX.X, op=ALU.max), r=["sc"], w=["rmax"])
                R.op("dve", lambda e: e.tensor_tensor(out=w0[:], in0=rmax[:], in1=rmin[:], op=ALU.subtract), r=["rmax", "rmin"], w=["w0"])
                R.op("dve", lambda e: e.tensor_scalar(out=w0[:], in0=w0[:], scalar1=1.001, scalar2=1e-6, op0=ALU.mult, op1=ALU.add), r=["w0"], w=["w0"])
                R.op("dve", lambda e: e.tensor_scalar(out=WN[:], in0=p2[:], scalar1=w0[:, 0:1], scalar2=None, op0=ALU.mult), r=["p2", "w0"], w=["WN"])
                R.op("dve", lambda e: e.tensor_scalar(out=NWN[:], in0=WN[:], scalar1=-1.0, scalar2=None, op0=ALU.mult), r=["WN"], w=["NWN"])
                R.op("dve", lambda e: e.tensor_tensor(out=mid[:], in0=rmin[:], in1=WN[:, 0:1], op=ALU.add), r=["rmin", "WN"], w=["mid"])
                for it in range(NIT):
                    R.op("dve", lambda e, n=n: e.tensor_scalar(out=cA[:, 0:n], in0=sc[:, 0:n], scalar1=mid[:, 0:1], scalar2=None, op0=ALU.is_ge, op1=ALU.add,
                                                               accum_out=cnt[:]), r=["sc", "mid"], w=["cA", "cnt"])
                    R.op("dve", lambda e, it=it: e.tensor_scalar(out=dl[:], in0=cnt[:], scalar1=256.0, scalar2=WN[:, it:it + 1], op0=ALU.is_ge, op1=ALU.mult),
                         r=["cnt", "WN"], w=["dl"])
                    if it + 1 < NIT:
                        R.op("dve", lambda e, it=it: e.scalar_tensor_tensor(out=mid[:], in0=mid[:], scalar=NWN[:, it + 1:it + 2], in1=dl[:], op0=ALU.add, op1=ALU.add),
                             r=["mid", "NWN", "dl"], w=["mid"])
                    else:
                        R.op("dve", lambda e, it=it: e.scalar_tensor_tensor(out=lo[:], in0=mid[:], scalar=NWN[:, it:it + 1], in1=dl[:], op0=ALU.add, op1=ALU.add),
                             r=["mid", "NWN", "dl"], w=["lo"])
                R.op("dve", lambda e: e.tensor_tensor(out=hi[:], in0=lo[:], in1=WN[:, NIT - 1:NIT], op=ALU.add), r=["lo", "WN"], w=["hi"])
                R.op("dve", lambda e, n=n: e.tensor_scalar(out=cB[:, 0:n], in0=sc[:, 0:n], scalar1=hi[:, 0:1], scalar2=None, op0=ALU.is_ge, op1=ALU.add,
                                                           accum_out=chi[:]), r=["sc", "hi"], w=["cB", "chi"])
                R.op("dve", lambda e, n=n: e.tensor_scalar(out=cA[:, 0:n], in0=sc[:, 0:n], scalar1=lo[:, 0:1], scalar2=None, op0=ALU.is_ge), r=["sc", "lo"], w=["cA"])
                R.op("dve", lambda e, n=n: e.tensor_tensor(out=cA[:, 0:n], in0=cA[:, 0:n], in1=cB[:, 0:n], op=ALU.subtract), r=["cA", "cB"], w=["cA"])
                R.op("dve", lambda e: e.tensor_scalar(out=mrem[:], in0=chi[:], scalar1=-1.0, scalar2=256.0, op0=ALU.mult, op1=ALU.add), r=["chi"], w=["mrem"])
                R.op("dve", lambda e, n=n: e.tensor_tensor_scan(out=sc[:, 0:n], data0=cA[:, 0:n], data1=cA[:, 0:n], initial=0.0, op0=ALU.add, op1=ALU.max),
                     r=["cA"], w=["sc"])
                R.op("dve", lambda e, n=n: e.scalar_tensor_tensor(out=cA[:, 0:n], in0=sc[:, 0:n], scalar=mrem[:, 0:1], in1=cA[:, 0:n], op0=ALU.is_le, op1=ALU.mult),
                     r=["sc", "mrem", "cA"], w=["cA"])
                R.op("dve", lambda e, n=n, b=b: e.scalar_tensor_tensor(out=mk[b][:, 0:n], in0=cA[:, 0:n], scalar=-1.0, in1=cB[:, 0:n], op0=ALU.add, op1=ALU.add),
                     r=["cA", "cB"], w=["mk%d" % b])

            def b_attend(t):
                b = t % 2
                ob = t % 2
                ok = "psO%d" % ob
                for kt in range(4 * t + 4):
                    b2 = kt % 2
                    ks = slice(kt * 128, (kt + 1) * 128)
                    for half in range(2):
                        hs = slice(half * 512, (half + 1) * 512)
                        R.op("pe", lambda e, b2=b2, hs=hs, ks=ks, b=b: e.matmul(psS[b2][:, hs], lhsT=kk[:, 0, ks], rhs=bqi[b][:, hs], start=True, stop=False),
                             r=["kk", "bqi%d" % b], w=["psS%d" % b2])
                        R.op("pe", lambda e, b2=b2, hs=hs, ks=ks, b=b: e.matmul(psS[b2][:, hs], lhsT=mk[b][:, ks], rhs=bigi4[:], start=False, stop=True),
                             r=["mk%d" % b], w=["psS%d" % b2])
                    R.op("act", lambda e, b2=b2: e.activation(out=pt[b2][:], in_=psS[b2][:], func=AF.Exp, scale=0.125), r=["psS%d" % b2], w=["ptb%d" % b2])
                    for h in range(8):
                        R.op("pe", lambda e, h=h, b2=b2, kt=kt, ob=ob, t=t: e.matmul(psO[ob][:, hoff(h):hoff(h) + 65], lhsT=pt[b2][:, h * 128:(h + 1) * 128],
                                                                                    rhs=bvs[:, kt, :], start=(kt == 0 and h % 4 == 0), stop=(kt == 4 * t + 3 and h % 4 == 3)),
                             r=["ptb%d" % b2, "bvs"], w=[ok])
                attn_epilogue_BC(R, psO[ob], ok, yb[:, t, :], "yb", rinv)

            b_select(0)
            for t in range(1, nt):
                b_select(t)
                b_attend(t - 1)
            b_attend(nt - 1)
            R.final_wait = list(R.dma_count.keys())
            if "B" in phases:
                R.emit()
        nc.all_engine_barrier()

        R = Rec(nc)
        with ExitStack() as s3:
            sb = lambda n, s, d=F32: s3.enter_context(nc.sbuf_tensor(pfx + n, s, d))
            cqs = sb("cqs", [64, 8, tok], BF16)
            EBT = sb("EBT", [128, 8, 8, 128], BF16); stg = sb("stgc", [128, 8, 128]); cmk = sb("cmk", [128, 8, 128])
            R.dma("sp", cmk[:].rearrange("p a b -> p (a b)"), cmaskT.ap(), "c", w=["cmk"])
            for j in range(8):
                src = bass.AP(rbext, T["rb_off"] + 1665 - 128 * j, [[1, 128], [T["rb_w"], 8], [1, 128]])
                R.dma("sp", stg[:], src, "stg", w=["stg"])
                R.op("dve", lambda e, j=j: e.scalar_tensor_tensor(out=EBT[:, j, :, :], in0=stg[:], scalar=8.0,
                                                                   in1=cmk[:, j, :].unsqueeze(1).broadcast_to([128, 8, 128]),
                                                                   op0=ALU.mult, op1=ALU.add), r=["stg", "cmk"], w=["EBT"])
            ck = [sb("ck%d" % i, [64, 8, 8, 128], BF16) for i in range(2)]
            cvs = [sb("cvs%d" % i, [128, 8, 520], BF16) for i in range(2)]
            pt = [sb("ptc%d" % i, [128, 1024], BF16) for i in range(2)]
            rinv = sb("rinvc", [128, 8])
            R.dma("sp", cqs[:].rearrange("p a b -> p (a b)"), cqt.ap(), "c", w=["cqs"])
            gi = 0
            for t in range(nt):
                b = t % 2
                for si, s_ in enumerate((t - 1, t)):
                    if s_ < 0:
                        R.op("pool", lambda e, b=b: e.memset(ck[b][:, 0:4, :, :], 0.0), w=["ck%d" % b])
                        R.op("pool", lambda e, b=b: e.memset(cvs[b][:, 0:4, :], 0.0), w=["cvs%d" % b])
                        continue
                    for r_ in range(4):
                        for c_ in range(2):
                            R.dma("sp", ck[b][c_ * 32:(c_ + 1) * 32, si * 4 + r_, :, :].rearrange("p h k -> p (h k)"),
                                  bass.AP(ckb[c_], r_ * 32 * 8 * tok + s_ * 1024, [[8 * tok, 32], [1, 1024]]), "ck%d" % b, w=["ck%d" % b])
                    SLc = min(4, nt)
                    R.dma("sp", cvs[b][:, si * 4:(si + 1) * 4, :], bass.AP(cvb[s_ // SLc], (s_ % SLc) * 128 * 520, [[520, 128], [SLc * 128 * 520, 4], [1, 520]]),
                          "ck%d" % b, w=["cvs%d" % b])
                ob = t % 2
                ok = "psO%d" % ob
                for j in range(8):
                    b2 = gi % 2
                    gi += 1
                    for h in range(8):
                        R.op("pe", lambda e, h=h, b=b, b2=b2, j=j, t=t: e.matmul(
                            psS[b2][:, h * 128:(h + 1) * 128], lhsT=ck[b][:, j, h, :],
                            rhs=cqs[:, h, t * 128:(t + 1) * 128], start=(h % 4 == 0), stop=False), r=["ck%d" % b, "cqs"], w=["psS%d" % b2])
                    for half in range(2):
                        R.op("pe", lambda e, half=half, b2=b2, j=j: e.matmul(psS[b2][:, half * 512:(half + 1) * 512], lhsT=jdb[:],
                                                                             rhs=EBT[:, j, half * 4:(half + 1) * 4, :].rearrange("p h q -> p (h q)"),
                                                                             start=False, stop=True), r=["EBT"], w=["psS%d" % b2])
                    R.op("act", lambda e, b2=b2: e.activation(out=pt[b2][:], in_=psS[b2][:], func=AF.Exp, scale=0.125), r=["psS%d" % b2], w=["ptc%d" % b2])
                    for h in range(8):
                        R.op("pe", lambda e, h=h, b2=b2, j=j, b=b, ob=ob: e.matmul(psO[ob][:, hoff(h):hoff(h) + 65], lhsT=pt[b2][:, h * 128:(h + 1) * 128],
                                                                                  rhs=cvs[b][:, j, h * 65:(h + 1) * 65], start=(j == 0 and h % 4 == 0), stop=(j == 7 and h % 4 == 3)),
                             r=["ptc%d" % b2, "cvs%d" % b], w=[ok])
                attn_epilogue_BC(R, psO[ob], ok, yc[:, t, :], "yc", rinv)
            R.final_wait = list(R.dma_count.keys())
            if "C" in phases:
                R.emit()
        nc.all_engine_barrier()

        R = Rec(nc)
        with ExitStack() as s4:
            sb = lambda n, s, d=F32: s4.enter_context(nc.sbuf_tensor(pfx + n, s, d))
            wbr = [sb("wbr%d" % i, [128, 4, D], BF16) for i in range(3)]
            wo = sb("wo", [128, 8, D], BF16)
            gt = [sb("gt%d" % i, [128, 3072]) for i in range(2)]
            xt = [sb("xt%d" % i, [128, D]) for i in range(2)]
            yT = sb("yT", [128, 512], BF16); mg = sb("mg", [128, D]); tt = sb("tt", [128, 512]); mgb = sb("mgb", [128, D], BF16)
            mT = sb("mT", [128, D], BF16); xot = [sb("xot%d" % i, [128, D]) for i in range(2)]
            for i, wsrc in enumerate((wa, wb, wc)):
                R.dma("pool", wbr[i][:], wsrc.ap().rearrange("(ch p) n -> p ch n", p=128), "w", w=["wbr%d" % i])
            R.dma("pool", wo[:], w_out.ap().rearrange("(ch p) n -> p ch n", p=128), "w", w=["wo"])
            pXt = psO[0][:, 0:512].bitcast(BF16)
            assert tuple(pXt.shape) == (128, 1024), pXt.shape
            for t in range(nt):
                b = t % 2
                R.dma("sp", gt[b][:], gates.ap()[t * 128:(t + 1) * 128, :], "gx%d" % b, w=["gt%d" % b])
                R.dma("sp", xt[b][:], x.ap()[t * 128:(t + 1) * 128, :], "gx%d" % b, w=["xt%d" % b])
                for bi, (ysrc, yk) in enumerate(((ya, "ya"), (yb, "yb"), (yc, "yc"))):
                    for ch in range(4):
                        R.op("pe", lambda e, ysrc=ysrc, ch=ch, t=t: e.transpose(out=pXt[:, ch * 128:(ch + 1) * 128], in_=ysrc[:, t, ch * 128:(ch + 1) * 128], identity=idb[:]),
                             r=[], w=["pXt"])
                    R.op("act", lambda e: e.copy(out=yT[:], in_=pXt[:, 0:512]), r=["pXt"], w=["yT"])
                    for half in range(2):
                        hs = slice(half * 512, (half + 1) * 512)
                        for ch in range(4):
                            R.op("pe", lambda e, ch=ch, bi=bi, half=half, hs=hs: e.matmul(psS[half][:, 0:512], lhsT=yT[:, ch * 128:(ch + 1) * 128], rhs=wbr[bi][:, ch, hs],
                                                                                          start=(ch == 0), stop=(ch == 3)), r=["yT", "wbr%d" % bi], w=["psS%d" % half])
                        gsl = gt[b][:, bi * 1024 + half * 512: bi * 1024 + (half + 1) * 512]
                        if bi == 0:
                            R.op("dve", lambda e, half=half, hs=hs, gsl=gsl: e.tensor_tensor(out=mg[:, hs], in0=psS[half][:, 0:512], in1=gsl, op=ALU.mult),
                                 r=["psS%d" % half, "gt%d" % b], w=["mg%d" % half])
                        else:
                            R.op("dve", lambda e, half=half, gsl=gsl: e.tensor_tensor(out=tt[:], in0=psS[half][:, 0:512], in1=gsl, op=ALU.mult),
                                 r=["psS%d" % half, "gt%d" % b], w=["tt"])
                            dst = mg if bi == 1 else mgb
                            R.op("dve", lambda e, hs=hs, dst=dst: e.tensor_tensor(out=dst[:, hs], in0=mg[:, hs], in1=tt[:], op=ALU.add),
                                 r=["tt", "mg%d" % half], w=["mg%d" % half if bi == 1 else "mgb%d" % half])
                for ch in range(8):
                    R.op("pe", lambda e, ch=ch: e.transpose(out=pXt[:, ch * 128:(ch + 1) * 128], in_=mgb[:, ch * 128:(ch + 1) * 128], identity=idb[:]),
                         r=["mgb0", "mgb1"], w=["pXt"])
                R.op("act", lambda e: e.copy(out=mT[:], in_=pXt[:]), r=["pXt"], w=["mT"])
                for half in range(2):
                    hs = slice(half * 512, (half + 1) * 512)
                    for ch in range(8):
                        R.op("pe", lambda e, ch=ch, half=half, hs=hs: e.matmul(psS[half][:, 0:512], lhsT=mT[:, ch * 128:(ch + 1) * 128], rhs=wo[:, ch, hs],
                                                                               start=(ch == 0), stop=(ch == 7)), r=["mT", "wo"], w=["psS%d" % half])
                    R.op("dve", lambda e, half=half, hs=hs: e.tensor_tensor(out=tt[:], in0=psS[half][:, 0:512], in1=g1bc[:, hs], op=ALU.mult),
                         r=["psS%d" % half], w=["tt"])
                    R.op("dve", lambda e, hs=hs, b=b: e.tensor_tensor(out=xot[b][:, hs], in0=tt[:], in1=xt[b][:, hs], op=ALU.add),
                         r=["tt", "xt%d" % b], w=["xot%d_%d" % (b, half)])
                R.dma("sp", xo.ap()[t * 128:(t + 1) * 128, :], xot[b][:], "out%d" % b, r=["xot%d_0" % b, "xot%d_1" % b])
            R.final_wait = list(R.dma_count.keys())
            if "M" in phases:
                R.emit()


D = 1024
EPS = 1e-6
NE = 16
FF = 512
BIGR = 1.0e4


def emit_M(nc, T, pfx, nt=16, final=False, ne=NE, phases="123", cut=9):
    tok = nt * 128
    di = lambda n, s, d=F32: T[n]
    x = di("x", [tok, D]); cvec = di("c", [128, 8]); w_mod = di("w_mod", [D, 6144]); b_mod = di("b_mod", [1, 6144])
    norm_g = di("norm_g", [1, D]); router_w = di("router_w", [D, 16]); router_b = di("router_b", [1, 16])
    w1 = di("w1", [NE, D, FF]); w3 = di("w3", [NE, D, FF]); w2 = di("w2", [NE, FF, D]); ident = di("ident", [128, 128])
    final_g = di("final_g", [1, D])
    xo = T["xo"]
    TG = (nt + 3) // 4

    with ExitStack() as st, nc.allow_low_precision("bf16 matmul operands, fp32 accumulation"):
        sbo = lambda n, s, d=F32: st.enter_context(nc.sbuf_tensor(pfx + n, s, d))
        pso = lambda n, s, d=F32: st.enter_context(nc.psum_tensor(pfx + n, s, d))
        modbc = sbo("modbc", [128, 3 * D]); gs = sbo("gs", [128, D]); idf = sbo("idf", [128, 128])
        uT = sbo("uT", [128, 8, tok], BF16); comb = sbo("comb", [128, nt, 16]); yacc = sbo("yacc", [128, nt, D])
        cst = sbo("cst", [128, 2]); fgb = sbo("fgb", [128, D])
        PS = [pso("PS%d" % i, [128, 512]) for i in range(8)]

        R = Rec(nc)
        with ExitStack() as s0:
            sb = lambda n, s, d=F32: s0.enter_context(nc.sbuf_tensor(pfx + n, s, d))
            cs = sb("cs", [128, 8]); ca = sb("ca", [128, 8]); CA = sb("CA", [128, 8, 128])
            wm = [sb("wm%d" % i, [128, 8, 256]) for i in range(2)]
            bmb = sb("bmb", [128, 3 * D]); gbc = sb("gbc", [128, D]); rw = sb("rw", [128, 8, 16]); rbb = sb("rbb", [128, 16])
            xt = [sb("xt%d" % i, [128, D]) for i in range(2)]
            junk = sb("junk", [128, D], BF16); ss = sb("ss", [128, 1]); rt = sb("rt", [128, 1]); rstd = sb("rstd", [128, 1])
            tmp = sb("tmp", [128, D]); u2 = sb("u2", [128, D]); uh = sb("uh", [128, D], BF16); ul = sb("ul", [128, D], BF16)
            ulT = sb("ulT", [128, D], BF16); idb = sb("idb", [128, 128], BF16); rwh = sb("rwh", [128, 8, 16], BF16); rwl = sb("rwl", [128, 8, 16], BF16)
            pXh = PS[2][:].bitcast(BF16); pXl = PS[3][:].bitcast(BF16)
            aff = sb("aff", [128, 16]); sel = sb("sel", [128, 16]); m1 = sb("m1", [128, 4]); eq = sb("eq", [128, 16]); s2 = sb("s2", [128, 16])
            m2 = sb("m2", [128, 4]); gsum = sb("gsum", [128, 4]); gmax = sb("gmax", [128, 1]); ing = sb("ing", [128, 4]); pen = sb("pen", [128, 4])
            selm = sb("selm", [128, 16]); t1 = sb("t1", [128, 1]); e1 = sb("e1", [128, 16]); t2 = sb("t2", [128, 1]); e2 = sb("e2", [128, 16])
            den = sb("den", [128, 1]); rden = sb("rden", [128, 1])
            R.dma("sp", cs[:], cvec.ap(), "c", w=["cs"])
            R.dma("sp", idf[:], ident.ap(), "c", w=["idf"])
            R.dma("sp", bmb[:], b_mod.ap()[0:1, 3072:6144].partition_broadcast(128), "c", w=["bmb"])
            R.dma("sp", gbc[:], norm_g.ap()[0:1, :].partition_broadcast(128), "c", w=["gbc"])
            R.dma("sp", fgb[:], final_g.ap()[0:1, :].partition_broadcast(128), "c", w=["fgb"])
            R.dma("sp", rbb[:], router_b.ap()[0:1, :].partition_broadcast(128), "c", w=["rbb"])
            R.dma("sp", rw[:], router_w.ap().rearrange("(ch p) n -> p ch n", p=128), "c", w=["rw"])
            R.op("pool", lambda e: e.memset(cst[:, 0:1], EPS), w=["cst"])
            R.op("dve", lambda e: e.tensor_copy(out=idb[:], in_=idf[:]), r=["idf"], w=["idb"])
            R.op("dve", lambda e: e.tensor_copy(out=rwh[:], in_=rw[:]), r=["rw"], w=["rwh"])
            R.op("dve", lambda e: e.tensor_tensor(out=rwl[:], in0=rw[:], in1=rwh[:], op=ALU.subtract), r=["rw", "rwh"], w=["rwl"])
            R.op("act", lambda e: e.activation(out=ca[:], in_=cs[:], func=AF.Silu), r=["cs"], w=["ca"])
            R.op("dve", lambda e: e.tensor_copy(out=CA[:], in_=ca[:].unsqueeze(2).broadcast_to([128, 8, 128])), r=["ca"], w=["CA"])
            wmv = w_mod.ap().rearrange("(ch p) n -> p ch n", p=128)
            for j in range(12):
                b = j % 2
                R.dma("sp", wm[b][:], wmv[:, :, 3072 + j * 256:3072 + (j + 1) * 256], "wm%d" % b, w=["wm%d" % b])
                for ch in range(8):
                    R.op("pe", lambda e, ch=ch, b=b: e.matmul(PS[b][:, 0:256], lhsT=CA[:, ch, :], rhs=wm[b][:, ch, :], start=(ch == 0), stop=(ch == 7)),
                         r=["CA", "wm%d" % b], w=["PS%d" % b])
                R.op("dve", lambda e, j=j, b=b: e.tensor_tensor(out=modbc[:, j * 256:(j + 1) * 256], in0=PS[b][:, 0:256], in1=bmb[:, j * 256:(j + 1) * 256], op=ALU.add),
                     r=["PS%d" % b, "bmb"], w=["modbc"])
            R.op("dve", lambda e: e.scalar_tensor_tensor(out=gs[:], in0=modbc[:, D:2 * D], scalar=1.0, in1=gbc[:], op0=ALU.add, op1=ALU.mult),
                 r=["modbc", "gbc"], w=["gs"])
            for t in range(nt):
                b = t % 2
                xk = "xt%d" % b
                R.dma("sp", xt[b][:], x.ap()[t * 128:(t + 1) * 128, :], xk, w=[xk])
                R.op("act", lambda e, b=b: e.activation(out=junk[:], in_=xt[b][:], func=AF.Square, accum_out=ss[:]), r=[xk], w=["junk", "ss"])
                R.op("act", lambda e: e.activation(out=rt[:], in_=ss[:], func=AF.Sqrt, scale=1.0 / D, bias=cst[:, 0:1]), r=["ss", "cst"], w=["rt"])
                R.op("dve", lambda e: e.reciprocal(out=rstd[:], in_=rt[:]), r=["rt"], w=["rstd"])
                R.op("dve", lambda e, b=b: e.scalar_tensor_tensor(out=tmp[:], in0=xt[b][:], scalar=rstd[:, 0:1], in1=gs[:], op0=ALU.mult, op1=ALU.mult),
                     r=[xk, "rstd", "gs"], w=["tmp"])
                R.op("dve", lambda e: e.tensor_tensor(out=u2[:], in0=tmp[:], in1=modbc[:, 0:D], op=ALU.add), r=["tmp", "modbc"], w=["u2"])
                if cut < 2:
                    continue
                R.op("dve", lambda e: e.tensor_copy(out=uh[:], in_=u2[:]), r=["u2"], w=["uh"])
                R.op("dve", lambda e: e.tensor_tensor(out=ul[:], in0=u2[:], in1=uh[:], op=ALU.subtract), r=["u2", "uh"], w=["ul"])
                for ch in range(8):
                    R.op("pe", lambda e, ch=ch: e.transpose(out=pXh[:, ch * 128:(ch + 1) * 128], in_=uh[:, ch * 128:(ch + 1) * 128], identity=idb[:]),
                         r=["uh", "idb"], w=["PS2"])
                R.op("act", lambda e, t=t: e.copy(out=uT[:, :, t * 128:(t + 1) * 128], in_=pXh.rearrange("p (c q) -> p c q", q=128)), r=["PS2"], w=["uT"])
                for ch in range(8):
                    R.op("pe", lambda e, ch=ch: e.transpose(out=pXl[:, ch * 128:(ch + 1) * 128], in_=ul[:, ch * 128:(ch + 1) * 128], identity=idb[:]),
                         r=["ul", "idb"], w=["PS3"])
                R.op("dve", lambda e: e.tensor_copy(out=ulT[:], in_=pXl), r=["PS3"], w=["ulT"])
                if cut < 3:
                    continue
                for ch in range(8):
                    uhs = uT[:, ch, t * 128:(t + 1) * 128]
                    R.op("pe", lambda e, ch=ch, uhs=uhs: e.matmul(PS[4][:, 0:16], lhsT=uhs, rhs=rwh[:, ch, :], start=(ch == 0), stop=False), r=["uT", "rwh"], w=["PS4"])
                    R.op("pe", lambda e, ch=ch, uhs=uhs: e.matmul(PS[4][:, 0:16], lhsT=uhs, rhs=rwl[:, ch, :], start=False, stop=False), r=["uT", "rwl"], w=["PS4"])
                    R.op("pe", lambda e, ch=ch: e.matmul(PS[4][:, 0:16], lhsT=ulT[:, ch * 128:(ch + 1) * 128], rhs=rwh[:, ch, :], start=False, stop=(ch == 7)),
                         r=["ulT", "rwh"], w=["PS4"])
                v4 = lambda a: a[:].rearrange("p (g k) -> p g k", k=4)
                R.op("act", lambda e: e.activation(out=aff[:], in_=PS[4][:, 0:16], func=AF.Sigmoid), r=["PS4"], w=["aff"])
                if cut < 4:
                    continue
                R.op("dve", lambda e: e.tensor_tensor(out=sel[:], in0=aff[:], in1=rbb[:], op=ALU.add), r=["aff", "rbb"], w=["sel"])
                R.op("dve", lambda e: e.tensor_reduce(out=m1[:], in_=v4(sel), axis=AX.X, op=ALU.max), r=["sel"], w=["m1"])
                R.op("dve", lambda e: e.tensor_tensor(out=v4(eq), in0=v4(sel), in1=m1[:].unsqueeze(2).broadcast_to([128, 4, 4]), op=ALU.is_equal), r=["sel", "m1"], w=["eq"])
                R.op("dve", lambda e: e.scalar_tensor_tensor(out=s2[:], in0=eq[:], scalar=-BIGR, in1=sel[:], op0=ALU.mult, op1=ALU.add), r=["eq", "sel"], w=["s2"])
                R.op("dve", lambda e: e.tensor_reduce(out=m2[:], in_=v4(s2), axis=AX.X, op=ALU.max), r=["s2"], w=["m2"])
                R.op("dve", lambda e: e.tensor_tensor(out=gsum[:], in0=m1[:], in1=m2[:], op=ALU.add), r=["m1", "m2"], w=["gsum"])
                R.op("dve", lambda e: e.tensor_reduce(out=gmax[:], in_=gsum[:], axis=AX.X, op=ALU.max), r=["gsum"], w=["gmax"])
                R.op("dve", lambda e: e.tensor_scalar(out=pen[:], in0=gsum[:], scalar1=gmax[:, 0:1], scalar2=-BIGR, op0=ALU.is_lt, op1=ALU.mult), r=["gsum", "gmax"], w=["pen"])
                R.op("dve", lambda e: e.tensor_tensor(out=v4(selm), in0=v4(sel), in1=pen[:].unsqueeze(2).broadcast_to([128, 4, 4]), op=ALU.add), r=["sel", "pen"], w=["selm"])
                R.op("dve", lambda e: e.tensor_reduce(out=t1[:], in_=selm[:], axis=AX.X, op=ALU.max), r=["selm"], w=["t1"])
                R.op("dve", lambda e: e.tensor_scalar(out=e1[:], in0=selm[:], scalar1=t1[:, 0:1], scalar2=None, op0=ALU.is_equal), r=["selm", "t1"], w=["e1"])
                R.op("dve", lambda e: e.scalar_tensor_tensor(out=s2[:], in0=e1[:], scalar=-BIGR, in1=selm[:], op0=ALU.mult, op1=ALU.add), r=["e1", "selm"], w=["s2"])
                R.op("dve", lambda e: e.tensor_reduce(out=t2[:], in_=s2[:], axis=AX.X, op=ALU.max), r=["s2"], w=["t2"])
                R.op("dve", lambda e: e.tensor_scalar(out=e2[:], in0=s2[:], scalar1=t2[:, 0:1], scalar2=None, op0=ALU.is_equal), r=["s2", "t2"], w=["e2"])
                R.op("dve", lambda e: e.tensor_tensor(out=e1[:], in0=e1[:], in1=e2[:], op=ALU.add), r=["e1", "e2"], w=["e1"])
                R.op("dve", lambda e: e.tensor_tensor(out=e2[:], in0=e1[:], in1=aff[:], op=ALU.mult), r=["e1", "aff"], w=["e2"])
                R.op("dve", lambda e: e.tensor_reduce(out=den[:], in_=e2[:], axis=AX.X, op=ALU.add), r=["e2"], w=["den"])
                R.op("dve", lambda e: e.reciprocal(out=rden[:], in_=den[:]), r=["den"], w=["rden"])
                R.op("dve", lambda e, t=t: e.tensor_scalar(out=comb[:, t, :], in0=e2[:], scalar1=rden[:, 0:1], scalar2=None, op0=ALU.mult), r=["e2", "rden"], w=["comb"])
            R.final_wait = list(R.dma_count.keys())
            if "1" in phases:
                R.emit()
        nc.all_engine_barrier()

        R = Rec(nc)
        with ExitStack() as s1:
            sb = lambda n, s, d=F32: s1.enter_context(nc.sbuf_tensor(pfx + n, s, d))
            w1s = [sb("w1s%d" % i, [128, 8, FF], BF16) for i in range(2)]
            w3s = [sb("w3s%d" % i, [128, 8, FF], BF16) for i in range(2)]
            w2s = [sb("w2s%d" % i, [128, 4, D], BF16) for i in range(2)]
            sl = [sb("sl%d" % i, [128, 512]) for i in range(2)]
            hT = [sb("hT%d" % i, [128, 4, 512], BF16) for i in range(2)]
            kc = [0]

            def m_h(e_, tg, hb):
                wb = e_ % 2
                if tg == 0:
                    R.dma("pool", w1s[wb][:], w1.ap()[e_].rearrange("(ch p) f -> p ch f", p=128), "w1_%d" % wb, w=["w1s%d" % wb])
                    R.dma("pool", w3s[wb][:], w3.ap()[e_].rearrange("(ch p) f -> p ch f", p=128), "w1_%d" % wb, w=["w3s%d" % wb])
                    R.dma("pool", w2s[wb][:], w2.ap()[e_].rearrange("(ch p) n -> p ch n", p=128), "w1_%d" % wb, w=["w2s%d" % wb])
                ntl = min(4, nt - tg * 4)
                ncol = ntl * 128
                tsl = slice(tg * 512, tg * 512 + ncol)
                for fc in range(4):
                    pb = (kc[0] % 2) * 2
                    kc[0] += 1
                    for ch in range(8):
                        R.op("pe", lambda e, ch=ch, fc=fc, pb=pb: e.matmul(PS[pb][:, 0:ncol], lhsT=w1s[wb][:, ch, fc * 128:(fc + 1) * 128], rhs=uT[:, ch, tsl],
                                                                           start=(ch == 0), stop=(ch == 7)), r=["w1s%d" % wb], w=["PS%d" % pb])
                    for ch in range(8):
                        R.op("pe", lambda e, ch=ch, fc=fc, pb=pb: e.matmul(PS[pb + 1][:, 0:ncol], lhsT=w3s[wb][:, ch, fc * 128:(fc + 1) * 128], rhs=uT[:, ch, tsl],
                                                                           start=(ch == 0), stop=(ch == 7)), r=["w3s%d" % wb], w=["PS%d" % (pb + 1)])
                    sb_ = kc[0] % 2
                    R.op("act", lambda e, pb=pb, sb_=sb_: e.activation(out=sl[sb_][:, 0:ncol], in_=PS[pb][:, 0:ncol], func=AF.Silu), r=["PS%d" % pb], w=["sl%d" % sb_])
                    R.op("dve", lambda e, pb=pb, sb_=sb_, fc=fc: e.tensor_tensor(out=hT[hb][:, fc, 0:ncol], in0=PS[pb + 1][:, 0:ncol], in1=sl[sb_][:, 0:ncol], op=ALU.mult),
                         r=["PS%d" % (pb + 1), "sl%d" % sb_], w=["hT%d" % hb])

            def m_w(e_, tg, hb):
                wb = e_ % 2
                ntl = min(4, nt - tg * 4)
                for ti in range(ntl):
                    t = tg * 4 + ti
                    for hf in range(2):
                        ob = 4 + ((t * 2 + hf) % 4)
                        for fc in range(4):
                            R.op("pe", lambda e, fc=fc, ti=ti, hf=hf, ob=ob: e.matmul(PS[ob][:], lhsT=hT[hb][:, fc, ti * 128:(ti + 1) * 128], rhs=w2s[wb][:, fc, hf * 512:(hf + 1) * 512],
                                                                                   start=(fc == 0), stop=(fc == 3)), r=["hT%d" % hb, "w2s%d" % wb], w=["PS%d" % ob])
                        ysl = yacc[:, t, hf * 512:(hf + 1) * 512]
                        if e_ == 0:
                            R.op("dve", lambda e, ob=ob, ysl=ysl, t=t: e.tensor_scalar(out=ysl, in0=PS[ob][:], scalar1=comb[:, t, e_:e_ + 1], scalar2=None, op0=ALU.mult),
                                 r=["PS%d" % ob], w=["y%d_%d" % (t, hf)])
                        else:
                            R.op("dve", lambda e, ob=ob, ysl=ysl, t=t: e.scalar_tensor_tensor(out=ysl, in0=PS[ob][:], scalar=comb[:, t, e_:e_ + 1], in1=ysl, op0=ALU.mult, op1=ALU.add),
                                 r=["PS%d" % ob, "y%d_%d" % (t, hf)], w=["y%d_%d" % (t, hf)])

            mjobs = [(e_, tg) for e_ in range(ne) for tg in range(TG)]
            m_h(*mjobs[0], 0)
            for ji, job in enumerate(mjobs):
                if ji + 1 < len(mjobs):
                    m_h(*mjobs[ji + 1], (ji + 1) % 2)
                m_w(*job, ji % 2)
            R.final_wait = list(R.dma_count.keys())
            if "2" in phases:
                R.emit()
        nc.all_engine_barrier()

        R = Rec(nc)
        with ExitStack() as s2:
            sb = lambda n, s, d=F32: s2.enter_context(nc.sbuf_tensor(pfx + n, s, d))
            xt = [sb("f_xt%d" % i, [128, D]) for i in range(2)]
            xn = [sb("f_xn%d" % i, [128, D]) for i in range(2)]
            tmp = sb("f_tmp", [128, D]); junk = sb("f_junk", [128, D], BF16); ss = sb("f_ss", [128, 1]); rt = sb("f_rt", [128, 1]); rstd = sb("f_rstd", [128, 1])
            for t in range(nt):
                b = t % 2
                R.dma("sp", xt[b][:], x.ap()[t * 128:(t + 1) * 128, :], "x%d" % b, w=["xt%d" % b])
                R.op("dve", lambda e, t=t: e.tensor_tensor(out=tmp[:], in0=yacc[:, t, :], in1=modbc[:, 2 * D:3 * D], op=ALU.mult), r=[], w=["tmp"])
                R.op("dve", lambda e, b=b: e.tensor_tensor(out=xn[b][:], in0=tmp[:], in1=xt[b][:], op=ALU.add), r=["tmp", "xt%d" % b], w=["xn%d" % b])
                if final:
                    R.op("act", lambda e, b=b: e.activation(out=junk[:], in_=xn[b][:], func=AF.Square, accum_out=ss[:]), r=["xn%d" % b], w=["junk", "ss"])
                    R.op("act", lambda e: e.activation(out=rt[:], in_=ss[:], func=AF.Sqrt, scale=1.0 / D, bias=cst[:, 0:1]), r=["ss"], w=["rt"])
                    R.op("dve", lambda e: e.reciprocal(out=rstd[:], in_=rt[:]), r=["rt"], w=["rstd"])
                    R.op("dve", lambda e, b=b: e.scalar_tensor_tensor(out=xn[b][:], in0=xn[b][:], scalar=rstd[:, 0:1], in1=fgb[:], op0=ALU.mult, op1=ALU.mult),
                         r=["xn%d" % b, "rstd"], w=["xn%d" % b])
                R.dma("sp", xo.ap()[t * 128:(t + 1) * 128, :], xn[b][:], "o%d" % b, r=["xn%d" % b])
            R.final_wait = list(R.dma_count.keys())
            if "3" in phases:
                R.emit()


class H:
    def __init__(self, ap):
        self._ap = ap

    def ap(self):
        return self._ap


RBW = 2688
RG = [[0, 1, 2, 3], [4, 5, 6, 7]]


def build_fused(nt=16, stop=None):
    tok = nt * 128
    nc = bass.Bass("TRN2", target_bir_lowering=False)
    di = lambda n, s, d=F32: nc.dram_tensor(n, s, d, kind="ExternalInput")
    dn = lambda n, s, d=BF16: nc.dram_tensor(n, s, d)
    E = {}
    E["x"] = di("x", [tok, D]); E["pos"] = di("pos", [128, nt], I32); E["c"] = di("c", [128, 8])
    E["zmT"] = di("zmT", [128, 512], BF16); E["zq"] = di("zq", [128, 512]); E["cmaskT"] = di("cmaskT", [128, 1024])
    E["rbcore"] = di("rbcore", [2, 8, RBW])
    E["w_mod"] = di("w_mod", [2, D, 6144]); E["b_mod"] = di("b_mod", [2, 1, 6144])
    E["norm1_g"] = di("norm1_g", [2, 1, D]); E["norm2_g"] = di("norm2_g", [2, 1, D]); E["w_in"] = di("w_in", [2, D, INC])
    E["lam4"] = di("lam4", [2, 1, 256]); E["a_norm_g"] = di("a_norm_g", [2, 1, 128]); E["laminit"] = di("laminit", [2, 1, 2])
    E["wa"] = di("wa", [2, 512, D]); E["wb"] = di("wb", [2, 512, D]); E["wc"] = di("wc", [2, 512, D]); E["w_out"] = di("w_out", [2, D, D])
    E["router_w"] = di("router_w", [D, 16]); E["router_b"] = di("router_b", [1, 16])
    E["w1"] = di("w1", [2, NE, D, FF]); E["w3"] = di("w3", [2, NE, D, FF]); E["w2"] = di("w2", [2, NE, FF, D])
    E["final_g"] = di("final_g", [1, D]); E["ident"] = di("ident", [128, 128]); E["aident"] = di("aident", [128, 128])
    E["ropeinv"] = di("ropeinv", [1, 32]); E["p2tab"] = di("p2tab", [1, NIT])
    out = nc.dram_tensor("out", [tok, D], F32, kind="ExternalOutput")
    xin = E["x"]
    for layer in range(2):
        L = "L%d_" % layer
        aqt = dn(L + "aqt", [4, 64, 2 * tok]); bq_iq = dn(L + "bq_iq", [nt, 64, 1536]); iw = dn(L + "iw", [tok, 4], F32)
        cqt = dn(L + "cqt", [64, 8 * tok]); gates = dn(L + "gates", [tok, 3072], F32)
        akt_l = dn(L + "akt_l", [256, 2 * tok]); av_l = dn(L + "av_l", [tok, 516]); bkik_l = dn(L + "bkik_l", [64, 2 * tok])
        bv_l = dn(L + "bv_l", [tok, 65]); ck_l = dn(L + "ck_l", [64, 8 * tok]); cv_l = dn(L + "cv_l", [tok, 520])
        SL = min(4, nt); NCH = nt // SL
        akt_g = [dn(L + "akt_g%d" % i, [4 * 128, 2 * tok]) for i in range(2)]
        av_g = [dn(L + "av_g%d" % i, [4 * SL * 128, 516]) for i in range(NCH)]
        bkik_g = dn(L + "bkik_g", [4 * 64, 2 * tok]); bv_g = dn(L + "bv_g", [4 * tok, 65])
        ck_g = [dn(L + "ck_g%d" % i, [4 * 32, 8 * tok]) for i in range(2)]
        cv_g = [dn(L + "cv_g%d" % i, [4 * SL * 128, 520]) for i in range(NCH)]
        xmid = dn(L + "xmid", [tok, D], F32); xnext = dn(L + "xnext", [tok, D], F32) if layer == 0 else out
        wmod = H(E["w_mod"].ap()[layer]); bmod = H(E["b_mod"].ap()[layer])
        TP = {"x": xin, "pos": E["pos"], "c": E["c"], "w_mod": wmod, "b_mod": bmod, "norm_g": H(E["norm1_g"].ap()[layer]),
              "w_in": H(E["w_in"].ap()[layer]), "ident": E["ident"], "ropeinv": E["ropeinv"],
              "aqt": aqt, "akt": akt_l, "av": av_l, "bqt": bq_iq, "bkt": bkik_l, "bv": bv_l, "iw": iw,
              "cqt": cqt, "ckt": ck_l, "cv": cv_l, "gates": gates}
        emit_P(nc, TP, L + "P_", nt)
        nc.all_engine_barrier()
        if stop == "P":
            return nc
        pairs = [(bkik_l.ap(), bkik_g.ap()), (bv_l.ap(), bv_g.ap())]
        for i in range(2):
            pairs.append((akt_l.ap()[i * 128:(i + 1) * 128, :], akt_g[i].ap()))
            pairs.append((ck_l.ap()[i * 32:(i + 1) * 32, :], ck_g[i].ap()))
        for i in range(NCH):
            pairs.append((av_l.ap()[i * SL * 128:(i + 1) * SL * 128, :], av_g[i].ap()))
            pairs.append((cv_l.ap()[i * SL * 128:(i + 1) * SL * 128, :], cv_g[i].ap()))
        ccs = nc.alloc_semaphore(name=L + "ccs")
        with nc.Block() as blk:
            @blk.gpsimd
            def _(g):
                for (lo_, ga_) in pairs:
                    g.collective_compute("AllGather", mybir.AluOpType.bypass, replica_groups=RG,
                                         ins=[lo_], outs=[ga_]).then_inc(ccs, 1)
                g.wait_ge(ccs, len(pairs))
        nc.all_engine_barrier()
        nc.clear_and_free_semaphores([ccs])
        nc.all_engine_barrier()
        if stop == "AG":
            return nc
        TT = {"aqt": aqt, "bq_iq": bq_iq, "iw": iw, "cqt": cqt, "gates": gates, "x": xin,
              "akt": akt_g, "av": av_g, "bkik": bkik_g, "bv": bv_g, "ckb": ck_g, "cvb": cv_g,
              "zmT": E["zmT"], "zq": E["zq"], "cmaskT": E["cmaskT"],
              "wa": H(E["wa"].ap()[layer]), "wb": H(E["wb"].ap()[layer]), "wc": H(E["wc"].ap()[layer]), "w_out": H(E["w_out"].ap()[layer]),
              "w_mod": wmod, "b_mod": bmod, "c": E["c"], "lam4": H(E["lam4"].ap()[layer]), "a_norm_g": H(E["a_norm_g"].ap()[layer]),
              "rbext": E["rbcore"], "rb_off": layer * 8 * RBW, "rb_w": RBW, "ident": E["ident"], "aident": E["aident"],
              "laminit": H(E["laminit"].ap()[layer]), "p2tab": E["p2tab"], "xo": xmid}
        emit_T(nc, TT, L + "T_", nt, phases=(stop[1:] if (stop or "").startswith("T") else "0ABCM"))
        nc.all_engine_barrier()
        if (stop or "").startswith("T"):
            return nc
        TM = {"x": xmid, "c": E["c"], "w_mod": wmod, "b_mod": bmod, "norm_g": H(E["norm2_g"].ap()[layer]),
              "router_w": E["router_w"], "router_b": E["router_b"], "w1": H(E["w1"].ap()[layer]), "w3": H(E["w3"].ap()[layer]),
              "w2": H(E["w2"].ap()[layer]), "ident": E["ident"], "final_g": E["final_g"], "xo": xnext}
        emit_M(nc, TM, L + "M_", nt, final=(layer == 1))
        nc.all_engine_barrier()
        xin = xnext
    return nc


BF = ml_dtypes.bfloat16
_CACHE = {}


def _core_rows(r, nt=16):
    return np.concatenate([np.arange((4 * t + r) * 128, (4 * t + r + 1) * 128) for t in range(nt)])


def _masks(r):
    k = np.arange(128)[:, None]
    q = np.arange(128)[None, :]
    zmT = np.zeros((128, 4, 128), np.float32)
    zq = np.zeros((128, 4, 128), np.float32)
    for j in range(4):
        if j == r:
            m = (k >= 64) & (q < 64)
        elif j > r:
            m = np.ones((128, 128), bool)
        else:
            m = np.zeros((128, 128), bool)
        zmT[:, j, :] = np.where(m, -BIG, 0.0)
        zq[:, j, :] = np.where(m.T, -1e30, 0.0)
    cm = np.full((128, 8, 128), -BIG, np.float32)
    for i in range(8):
        j = i - r
        if 1 <= j <= 3:
            cm[:, i, :] = 0.0
        elif j == 0:
            cm[:, i, :] = np.where((k < 64) & (q >= 64), -BIG, 0.0)
        elif j == 4:
            cm[:, i, :] = np.where((k >= 64) & (q < 64), -BIG, 0.0)
    return zmT.reshape(128, 512).astype(BF), zq.reshape(128, 512), np.ascontiguousarray(cm[::-1]).reshape(128, 1024)


def kernel(x, c, positions, norm1_g, norm2_g, w_mod, b_mod, w_in, lambda_q1, lambda_k1, lambda_q2, lambda_k2,
           a_norm_g, c_rel_bias, w_branch_a, w_branch_b, w_branch_c, w_out, router_w, router_b,
           exp_w1, exp_w3, exp_w2, final_g, _nt=16, _runner=None, _stop=None):
    f32 = np.float32
    nt = _nt
    A = lambda a: np.ascontiguousarray(np.asarray(a, f32))
    x = A(x); c = A(c); positions = np.asarray(positions, np.int32)
    cores = [(b, r) for b in range(2) for r in range(4)]
    rows = [_core_rows(r, nt) for r in range(4)]
    ident = np.eye(128, dtype=f32)
    lam_init = [0.8 - 0.6 * math.exp(-0.3 * l) for l in range(2)]
    rb = A(c_rel_bias)
    rbext = np.concatenate([rb, np.repeat(rb[:, :, 512:513], 511, axis=2)], axis=2)
    rbbig = np.zeros((2, 8, 3072), f32); rbbig[:, :, 1024:2048] = rbext
    shared = {
        "w_mod": A(w_mod), "b_mod": A(b_mod)[:, None, :], "norm1_g": A(norm1_g)[:, None, :], "norm2_g": A(norm2_g)[:, None, :],
        "w_in": A(w_in), "lam4": np.concatenate([A(lambda_q1), A(lambda_k1), A(lambda_q2), A(lambda_k2)], axis=1)[:, None, :],
        "a_norm_g": A(a_norm_g)[:, None, :], "laminit": np.array([[[l, 1.0 - l]] for l in lam_init], f32),
        "wa": A(w_branch_a), "wb": A(w_branch_b), "wc": A(w_branch_c), "w_out": A(w_out),
        "router_w": A(router_w), "router_b": A(router_b)[None, :], "w1": A(exp_w1), "w3": A(exp_w3), "w2": A(exp_w2),
        "final_g": A(final_g)[None, :], "ident": ident, "aident": np.ascontiguousarray(ident[::-1]),
        "ropeinv": (np.float32(10000.0) ** (-np.arange(32, dtype=f32) / np.float32(32))).astype(f32)[None, :],
        "p2tab": (2.0 ** -(np.arange(NIT) + 1.0))[None, :].astype(f32),
    }
    shared = {k_: np.ascontiguousarray(v) for k_, v in shared.items()}
    ims = []
    for (b, r) in cores:
        zmT, zq, cm = _masks(r)
        im = dict(shared)
        im.update({"x": np.ascontiguousarray(x[b][rows[r]]), "pos": np.ascontiguousarray(positions[b][rows[r]].reshape(nt, 128).T),
                   "c": np.ascontiguousarray(c[b].reshape(8, 128).T), "zmT": zmT, "zq": np.ascontiguousarray(zq), "cmaskT": cm,
                   "rbcore": np.ascontiguousarray(rbbig[:, :, 128 * r:128 * r + RBW])})
        ims.append(im)
    if ("F", nt) not in _CACHE:
        _CACHE[("F", nt)] = build_fused(nt, stop=_stop)
    if _runner is None:
        results = run_bass_kernel_spmd(_CACHE[("F", nt)], ims, core_ids=list(range(8))).results
    else:
        results = _runner(_CACHE[("F", nt)], ims)
    out = np.zeros((2, 512 * nt, 1024), f32)
    for ci, (b, r) in enumerate(cores):
        out[b][rows[r]] = np.asarray(results[ci]["out"], f32)
    return out
```
